# Optimizing a Trainium2 kernel written in Bass

```python
import jax, jax.numpy as jnp
from jax import lax
import numpy as np

D_MODEL = 1024
BATCH = 4
SEQ = 8192
DEPTH = 4

N_MIXERS = 2
N_EVEN = (DEPTH + 1) // 2
N_ODD = DEPTH // 2
POOL_WINDOWS = (2, 4, 8, 16)
N_POOL_GROUPS = len(POOL_WINDOWS)
POOL_GROUP_DIM = D_MODEL // N_POOL_GROUPS
DIL_PATTERNS = ((128, 1), (512, 4), (2048, 16))
N_DIL_GROUPS = len(DIL_PATTERNS)
HEADS_PER_GROUP = 8
HEAD_DIM = D_MODEL // HEADS_PER_GROUP
GROUP_WIDTH = HEADS_PER_GROUP * HEAD_DIM
QKV_WIDTH = N_DIL_GROUPS * 3 * GROUP_WIDTH
ROPE_DIM = HEAD_DIM // 4
ROPE_THETA = 500000.0
D_FF = 2816
N_EXPERTS = 8
TOP_K = 2
D_FF_EXPERT = 3584
NORM_EPS = 1e-6

kernel_name = "hybrid_pool_dilated_attn_moe_adaln"


def _rmsnorm(t, g):
    t32 = t.astype(jnp.float32)
    y = t32 * lax.rsqrt(jnp.mean(t32 * t32, axis=-1, keepdims=True) + NORM_EPS)
    return (y * g.astype(jnp.float32)).astype(t.dtype)


def _rope_tables(positions):
    half = ROPE_DIM // 2
    inv_freq = ROPE_THETA ** (-jnp.arange(half, dtype=jnp.float32) * 2.0 / ROPE_DIM)
    ang = positions.astype(jnp.float32)[..., None] * inv_freq
    return jnp.cos(ang)[:, :, None, :], jnp.sin(ang)[:, :, None, :]


def _partial_rope(t, cos, sin):
    t32 = t.astype(jnp.float32)
    half = ROPE_DIM // 2
    x1, x2, rest = t32[..., :half], t32[..., half:ROPE_DIM], t32[..., ROPE_DIM:]
    out = jnp.concatenate([x1 * cos - x2 * sin, x2 * cos + x1 * sin, rest], axis=-1)
    return out.astype(t.dtype)


def _pool_mixer(h, w_pool, scale):
    B, S, D = h.shape
    hg = h.astype(jnp.float32).reshape(B, S, N_POOL_GROUPS, POOL_GROUP_DIM)
    cs = jnp.cumsum(hg, axis=1)
    t = jnp.arange(S)
    pooled = []
    for gi, w in enumerate(POOL_WINDOWS):
        c = cs[:, :, gi]
        lag = jnp.pad(c[:, :S - w], ((0, 0), (w, 0), (0, 0)))
        cnt = jnp.minimum(t + 1, w).astype(jnp.float32)[None, :, None]
        pooled.append((c - lag) / cnt)
    pooled = jnp.stack(pooled, axis=2) - hg
    y = jnp.einsum('bsgc,gcd->bsgd', pooled.astype(h.dtype), w_pool).reshape(B, S, D)
    return y * scale


def _dilated_window_attention(q, k, v, window, dilation):
    B, S, H, Dh = q.shape
    span = window // dilation
    L = S // dilation
    nb = -(-L // span)
    Lp = nb * span

    def to_strided_blocks(t):
        t = t.reshape(B, L, dilation, H, Dh).transpose(0, 2, 3, 1, 4)
        t = jnp.pad(t, ((0, 0), (0, 0), (0, 0), (0, Lp - L), (0, 0)))
        return t.reshape(B, dilation, H, nb, span, Dh)

    def with_prev_block(t):
        prev = jnp.pad(t[:, :, :, :-1], ((0, 0), (0, 0), (0, 0), (1, 0), (0, 0), (0, 0)))
        return jnp.concatenate([prev, t], axis=4)

    qb = to_strided_blocks(q)
    kk = with_prev_block(to_strided_blocks(k))
    vv = with_prev_block(to_strided_blocks(v))
    s = jnp.einsum('brhnqe,brhnke->brhnqk', qb, kk,
                   preferred_element_type=jnp.float32) * (HEAD_DIM ** -0.5)
    blk = jnp.arange(nb)[:, None, None]
    qpos = blk * span + jnp.arange(span)[None, :, None]
    kpos = (blk - 1) * span + jnp.arange(2 * span)[None, None, :]
    allowed = (kpos <= qpos) & (kpos >= qpos - span) & (kpos >= 0)
    s = jnp.where(allowed, s, -jnp.inf)
    m = jnp.max(s, axis=-1, keepdims=True)
    p = jnp.exp(s - m)
    den = jnp.sum(p, axis=-1, keepdims=True)
    o = jnp.einsum('brhnqk,brhnke->brhnqe', p, vv.astype(jnp.float32)) / den
    lse = (m + jnp.log(den))[..., 0]
    o = o.reshape(B, dilation, H, Lp, Dh)[:, :, :, :L].transpose(0, 3, 1, 2, 4).reshape(B, S, H, Dh)
    lse = lse.reshape(B, dilation, H, Lp)[..., :L].transpose(0, 3, 1, 2).reshape(B, S, H)
    return o, lse


def _dilated_attention_mixer(h, cos, sin, w_qkv, w_o, q_gain, k_gain):
    B, S, D = h.shape
    qkv = (h @ w_qkv).reshape(B, S, N_DIL_GROUPS, 3, HEADS_PER_GROUP, HEAD_DIM)
    outs, lses = [], []
    for g, (window, dilation) in enumerate(DIL_PATTERNS):
        q = _partial_rope(_rmsnorm(qkv[:, :, g, 0], q_gain[g]), cos, sin)
        k = _partial_rope(_rmsnorm(qkv[:, :, g, 1], k_gain[g]), cos, sin)
        v = qkv[:, :, g, 2]
        o, lse = _dilated_window_attention(q, k, v, window, dilation)
        outs.append(o)
        lses.append(lse)
    alpha = jax.nn.softmax(jnp.stack(lses, axis=0), axis=0)
    o = jnp.einsum('gbsh,gbshe->bshe', alpha, jnp.stack(outs, axis=0))
    return o.reshape(B, S, GROUP_WIDTH).astype(h.dtype) @ w_o


def _swiglu(h, w_gu, w_down):
    g, u = jnp.split(h @ w_gu, 2, axis=-1)
    return (jax.nn.silu(g) * u) @ w_down


def _moe_swiglu(h, w_router, b_router, w_gu, w_down):
    B, S, D = h.shape
    t = h.reshape(B * S, D)
    logits = (t @ w_router).astype(jnp.float32) + b_router.astype(jnp.float32)
    top_val, top_idx = lax.top_k(logits, TOP_K)
    gates = jax.nn.softmax(top_val, axis=-1)
    combine = jnp.sum(jax.nn.one_hot(top_idx, N_EXPERTS, dtype=jnp.float32) * gates[..., None], axis=1)
    y = jnp.zeros((B * S, D), jnp.float32)
    for e in range(N_EXPERTS):
        y = y + combine[:, e:e + 1] * _swiglu(t, w_gu[e], w_down[e]).astype(jnp.float32)
    return y.reshape(B, S, D).astype(h.dtype)


def setup_inputs(seed: int = 0) -> dict:
    key = jax.random.key(seed)
    ks = jax.random.split(key, 20)
    f32 = jnp.float32
    nrm = lambda k, shape, fan_in, gain=1.0: jax.random.normal(k, shape, f32) * (gain * fan_in ** -0.5)
    x = jax.random.normal(ks[0], (BATCH, SEQ, D_MODEL), f32)
    c = jax.random.normal(ks[1], (BATCH, D_MODEL), f32)
    offset = jax.random.randint(ks[2], (BATCH, 1), 0, 4096, dtype=jnp.int32)
    positions = offset + jnp.arange(SEQ, dtype=jnp.int32)[None, :]
    return {
        "x": x,
        "c": c,
        "positions": positions,
        "norm1_g": 1.0 + 0.05 * jax.random.normal(ks[3], (DEPTH, D_MODEL), f32),
        "norm2_g": 1.0 + 0.05 * jax.random.normal(ks[4], (DEPTH, D_MODEL), f32),
        "ada_w": nrm(ks[5], (DEPTH, D_MODEL, 6 * D_MODEL), D_MODEL, 0.5),
        "ada_b": 0.02 * jax.random.normal(ks[6], (DEPTH, 6 * D_MODEL), f32),
        "pool_w": nrm(ks[7], (N_EVEN, N_POOL_GROUPS, POOL_GROUP_DIM, POOL_GROUP_DIM), POOL_GROUP_DIM),
        "pool_scale": 1.0 + 0.1 * jax.random.normal(ks[8], (N_EVEN, D_MODEL), f32),
        "attn_w_qkv": nrm(ks[9], (N_ODD, D_MODEL, QKV_WIDTH), D_MODEL),
        "attn_w_o": nrm(ks[10], (N_ODD, GROUP_WIDTH, D_MODEL), GROUP_WIDTH),
        "q_norm_g": 1.0 + 0.05 * jax.random.normal(ks[11], (N_ODD, N_DIL_GROUPS, HEAD_DIM), f32),
        "k_norm_g": 1.0 + 0.05 * jax.random.normal(ks[12], (N_ODD, N_DIL_GROUPS, HEAD_DIM), f32),
        "ffn_w_gu": nrm(ks[13], (N_EVEN, D_MODEL, 2 * D_FF), D_MODEL),
        "ffn_w_down": nrm(ks[14], (N_EVEN, D_FF, D_MODEL), D_FF),
        "router_w": nrm(ks[15], (N_ODD, D_MODEL, N_EXPERTS), D_MODEL),
        "router_b": 0.01 * jax.random.normal(ks[16], (N_ODD, N_EXPERTS), f32),
        "moe_w_gu": nrm(ks[17], (N_ODD, N_EXPERTS, D_MODEL, 2 * D_FF_EXPERT), D_MODEL),
        "moe_w_down": nrm(ks[18], (N_ODD, N_EXPERTS, D_FF_EXPERT, D_MODEL), D_FF_EXPERT),
    }


def reference(x, c, positions, norm1_g, norm2_g, ada_w, ada_b, pool_w, pool_scale,
              attn_w_qkv, attn_w_o, q_norm_g, k_norm_g, ffn_w_gu, ffn_w_down,
              router_w, router_b, moe_w_gu, moe_w_down):
    cos, sin = _rope_tables(positions)
    c_act = jax.nn.silu(c)
    for i in range(DEPTH):
        j = i // 2
        mod = (c_act @ ada_w[i] + ada_b[i])[:, None, :]
        sh1, sc1, g1, sh2, sc2, g2 = jnp.split(mod, 6, axis=-1)
        h = _rmsnorm(x, norm1_g[i]) * (1.0 + sc1) + sh1
        if i % N_MIXERS == 0:
            y = _pool_mixer(h, pool_w[j], pool_scale[j])
        else:
            y = _dilated_attention_mixer(h, cos, sin, attn_w_qkv[j], attn_w_o[j],
                                         q_norm_g[j], k_norm_g[j])
        x = x + g1 * y
        h = _rmsnorm(x, norm2_g[i]) * (1.0 + sc2) + sh2
        if i % 2 == 0:
            y = _swiglu(h, ffn_w_gu[j], ffn_w_down[j])
        else:
            y = _moe_swiglu(h, router_w[j], router_b[j], moe_w_gu[j], moe_w_down[j])
        x = x + g2 * y
    return x
```

```python
import contextlib
import numpy as np
import ml_dtypes
import concourse.bass as bass
import concourse.mybir as mybir
from concourse.bass_utils import run_bass_kernel_spmd

F32 = mybir.dt.float32
BF16 = mybir.dt.bfloat16
I32 = mybir.dt.int32
AF = mybir.ActivationFunctionType
ALU = mybir.AluOpType
AX = mybir.AxisListType

D = 1024
NCORES = 8
TOWN = 4096
THALO = 2048
SEQ = 8192
DFF = 2816
DFFE = 3584
NE = 8
EPS = 1e-6
DIL = (1, 4, 16)


class Res:
    __slots__ = ("name", "w", "rd", "dsem", "dcnt")

    def __init__(self, name):
        self.name = name
        self.w = None
        self.rd = {}
        self.dsem = None
        self.dcnt = 0


class _Eng:
    def __init__(self, key, sem):
        self.key = key
        self.sem = sem
        self.n = 0
        self.ops = []
        self.seen = {}


class Prog:
    def __init__(self, nc):
        self.nc = nc
        self.E = {}
        for k in ("pe", "act", "dve", "pool", "sp"):
            self.E[k] = _Eng(k, nc.alloc_semaphore(name="s_" + k))
        self.obj = {"pe": nc.tensor, "act": nc.scalar, "dve": nc.vector, "pool": nc.gpsimd, "sp": nc.sync}
        self.semname = {}
        self.dma_res = []

    def _deps(self, ek, reads, writes):
        need = {}

        def add(ev, kind):
            if ev is None:
                return
            s, v = ev
            own = s is self.E[ek].sem
            if own and (ek in ("pe", "sp") or kind == "war"):
                return
            key = id(s)
            self.semname[key] = s
            if need.get(key, 0) < v:
                need[key] = v

        for r in reads:
            add(r.w, "raw")
        for r in writes:
            add(r.w, "waw")
            for ev in r.rd.values():
                add(ev, "war")
        eng = self.E[ek]
        out = []
        for key, v in need.items():
            if eng.seen.get(key, 0) < v:
                eng.seen[key] = v
                out.append((self.semname[key], v))
        return out

    def op(self, ek, fn, reads=(), writes=()):
        eng = self.E[ek]
        e = self.obj[ek]
        waits = self._deps(ek, reads, writes)
        fns = fn if isinstance(fn, (list, tuple)) else [fn]
        for s, v in waits:
            e.wait_ge(s, v)
        for f in fns[:-1]:
            f(e)
        fns[-1](e).then_inc(eng.sem, 1)
        eng.n += 1
        ev = (eng.sem, eng.n)
        for r in reads:
            r.rd[ek] = ev
        for r in writes:
            r.w = ev
            r.rd = {}
        return ev

    def dma(self, qk, out, in_, sres, reads=(), writes=()):
        e = self.obj[qk]
        waits = self._deps(qk, reads, writes)
        if sres.dsem is None:
            sres.dsem = self.nc.alloc_semaphore(name="d_" + sres.name)
            self.dma_res.append(sres)
        sres.dcnt += 1
        sem = sres.dsem
        ev = (sem, 16 * sres.dcnt)
        for s, v in waits:
            e.wait_ge(s, v)
        e.dma_start(out=out, in_=in_).then_inc(sem, 16)
        for r in reads:
            r.rd[("d", id(sres))] = ev
        for r in writes:
            r.w = ev
            r.rd = {}
        return ev

    def barrier(self):
        evs = [(e.sem, e.n) for e in self.E.values() if e.n > 0]
        evs += [(r.dsem, 16 * r.dcnt) for r in self.dma_res]
        for ek, eng in self.E.items():
            for s, v in evs:
                if s is eng.sem:
                    continue
                if eng.seen.get(id(s), 0) < v:
                    eng.seen[id(s)] = v
                    self.obj[ek].wait_ge(s, v)


class Ctx:
    def __init__(self, nc, prog):
        self.nc = nc
        self.P = prog
        self.n = 0
        self.stacks = []

    def push(self):
        st = contextlib.ExitStack()
        st.__enter__()
        self.stacks.append(st)

    def pop(self):
        self.stacks.pop().__exit__(None, None, None)

    def sb(self, shape, dt, name=None):
        self.n += 1
        name = (name or "t") + "_%d" % self.n
        t = self.stacks[-1].enter_context(self.nc.sbuf_tensor(name, list(shape), dt))
        return t, Res(name)

    def ps(self, shape, dt, name=None):
        self.n += 1
        name = (name or "p") + "_%d" % self.n
        t = self.stacks[-1].enter_context(self.nc.psum_tensor(name, list(shape), dt))
        return t, Res(name)


def _dram_in(nc, name, shape, dt=F32):
    return nc.dram_tensor(name, list(shape), dt, kind="ExternalInput").ap()


class K:
    pass


def setup_common(k, io):
    P, C = k.P, k.C
    k.ident, k.ident_r = C.sb([128, 128], F32, "ident")
    P.dma("sp", k.ident[:], io["ident"], k.ident_r, writes=[k.ident_r])
    k.identb, k.identb_r = C.sb([128, 128], BF16, "identb")
    P.op("dve", lambda e: e.tensor_copy(out=k.identb[:], in_=k.ident[:]), reads=[k.ident_r], writes=[k.identb_r])
    k.ones, k.ones_r = C.sb([128, 128], F32, "ones")
    P.op("pool", lambda e: e.memset(k.ones[:], 1.0), writes=[k.ones_r])
    k.onesb, k.onesb_r = C.sb([128, 128], BF16, "onesb")
    P.op("pool", lambda e: e.memset(k.onesb[:], 1.0), writes=[k.onesb_r])
    k.hv, k.hv_r = C.sb([128, 1], F32, "hv")
    P.dma("sp", k.hv[:], io["hv"], k.hv_r, writes=[k.hv_r])
    k.tp = [C.ps([128, 8, 128], BF16, "tp") for _ in range(2)]
    k.pb = [C.ps([128, 512], F32, "pb") for _ in range(6)]
    k.cnt = {}


def rr(k, name, n):
    v = k.cnt.get(name, 0)
    k.cnt[name] = v + 1
    return v % n


def ada_phase(k, io, even):
    P, C = k.P, k.C
    bc = [C.sb([128, 1024], F32, "bc") for _ in range(6)]
    C.push()
    c_sb, c_r = C.sb([128, 8], F32)
    P.dma("sp", c_sb[:], io["c_col"], c_r, writes=[c_r])
    cact, cact_r = C.sb([128, 8], F32)
    P.op("act", lambda e: e.activation(out=cact[:], in_=c_sb[:], func=AF.Silu), reads=[c_r], writes=[cact_r])
    row, row_r = C.sb([1, 6144], F32)
    brow, brow_r = C.sb([1, 6144], F32)
    P.dma("sp", brow[:], io["ada_b"], brow_r, writes=[brow_r])
    grow, grow_r = C.sb([1, 3072], F32)
    P.dma("sp", grow[:, 0:1024], io["norm1_g"], grow_r, writes=[grow_r])
    P.dma("sp", grow[:, 1024:2048], io["norm2_g"], grow_r, writes=[grow_r])
    if even:
        P.dma("sp", grow[:, 2048:3072], io["pool_scale"], grow_r, writes=[grow_r])
    wbuf = [C.sb([128, 8, 512], F32) for _ in range(2)]
    for cb in range(12):
        wt, wr = wbuf[cb % 2]
        P.dma("sp", wt[:], io["ada_w"][:, cb * 512:(cb + 1) * 512].rearrange("(j p) n -> p j n", p=128), wr, writes=[wr])
        pt, pr = k.pb[cb % 2]
        P.op("pe", [lambda e, j=j: e.matmul(pt[0:1, :], lhsT=cact[:, j:j + 1], rhs=wt[:, j, :], start=(j == 0), stop=(j == 7))
                    for j in range(8)], reads=[cact_r, wr], writes=[pr])
        P.op("dve", lambda e: e.tensor_tensor(out=row[:, cb * 512:(cb + 1) * 512], in0=pt[0:1, :],
                                              in1=brow[:, cb * 512:(cb + 1) * 512], op=ALU.add),
             reads=[pr, brow_r], writes=[row_r])
    P.op("dve", lambda e: e.scalar_tensor_tensor(out=row[:, 1024:2048], in0=row[:, 1024:2048], scalar=1.0,
                                                 in1=grow[:, 0:1024], op0=ALU.add, op1=ALU.mult),
         reads=[row_r, grow_r], writes=[row_r])
    P.op("dve", lambda e: e.scalar_tensor_tensor(out=row[:, 4096:5120], in0=row[:, 4096:5120], scalar=1.0,
                                                 in1=grow[:, 1024:2048], op0=ALU.add, op1=ALU.mult),
         reads=[row_r, grow_r], writes=[row_r])
    if even:
        P.op("dve", lambda e: e.tensor_tensor(out=row[:, 2048:3072], in0=row[:, 2048:3072], in1=grow[:, 2048:3072],
                                              op=ALU.mult), reads=[row_r, grow_r], writes=[row_r])
    if io.get("mod_out") is not None:
        P.dma("sp", io["mod_out"], row[:], row_r, reads=[row_r])
    for idx in range(6):
        for half in range(2):
            pt, pr = k.pb[(idx * 2 + half) % 2]
            P.op("pe", lambda e: e.matmul(pt[:], lhsT=k.ones[0:1, :], rhs=row[0:1, idx * 1024 + half * 512: idx * 1024 + half * 512 + 512],
                                          start=True, stop=True), reads=[row_r, k.ones_r], writes=[pr])
            P.op("act", lambda e: e.copy(out=bc[idx][0][:, half * 512:(half + 1) * 512], in_=pt[:]), reads=[pr], writes=[bc[idx][1]])
    P.barrier()
    C.pop()
    return bc


def sumsq(k, x_ap, x_r, ss, ss_r, col, junk):
    jt, jr = junk
    k.P.op("act", lambda e: e.activation(out=jt[:], in_=x_ap, func=AF.Square, accum_out=ss[:, col:col + 1]),
           reads=[x_r], writes=[jr, ss_r])


def rstd_from_ss(k, ss, ss_r, n, inv_n):
    P = k.P
    P.op("dve", lambda e: e.tensor_scalar(out=ss[:, 0:n], in0=ss[:, 0:n], scalar1=inv_n, scalar2=EPS, op0=ALU.mult, op1=ALU.add),
         reads=[ss_r], writes=[ss_r])
    P.op("act", lambda e: e.activation(out=ss[:, 0:n], in_=ss[:, 0:n], func=AF.Sqrt), reads=[ss_r], writes=[ss_r])
    P.op("dve", lambda e: e.reciprocal(out=ss[:, 0:n], in_=ss[:, 0:n]), reads=[ss_r], writes=[ss_r])


def mod_norm(k, x_ap, x_r, rs_ap, rs_r, gain, shift, tmp, out_ap, out_r):
    P = k.P
    tt, tr = tmp
    P.op("dve", lambda e: e.scalar_tensor_tensor(out=tt[:], in0=x_ap, scalar=rs_ap, in1=gain[0][:], op0=ALU.mult, op1=ALU.mult),
         reads=[x_r, rs_r, gain[1]], writes=[tr])
    P.op("pool", lambda e: e.tensor_tensor(out=out_ap, in0=tt[:], in1=shift[0][:], op=ALU.add), reads=[tr, shift[1]], writes=[out_r])


def transpose_bf16(k, hb, hb_r, hT, hT_r, c0):
    P = k.P
    tp, tpr = k.tp[rr(k, "tp", 2)]
    P.op("pe", [lambda e, j=j: e.transpose(out=tp[:, j, :], in_=hb[:, j * 128:(j + 1) * 128], identity=k.identb[:]) for j in range(8)],
         reads=[hb_r, k.identb_r], writes=[tpr])
    P.op("act", lambda e: e.copy(out=hT[:, :, c0:c0 + 128], in_=tp[:]), reads=[tpr], writes=[hT_r])


def alloc_expert_bufs(k, nset=2):
    C = k.C
    k.wg = [C.sb([128, 8, 512], BF16, "wg") for _ in range(nset)]
    k.wu = [C.sb([128, 8, 512], BF16, "wu") for _ in range(nset)]
    k.wd = [C.sb([128, 4, 1024], BF16, "wd") for _ in range(nset)]
    k.aT = [C.sb([128, 4, 1024], BF16, "aT") for _ in range(2)]
    k.sg = [C.sb([128, 512], F32, "sg") for _ in range(2)]
    k.nset = nset


def expert_load(k, gu_src, d_src, f0, fp, ff, g2b):
    P = k.P
    s = rr(k, "wset", k.nset)
    nf = fp // 128
    wg, wgr = k.wg[s]
    wu, wur = k.wu[s]
    wd, wdr = k.wd[s]
    P.dma("pool", wg[:, :, 0:fp], gu_src[:, f0:f0 + fp].rearrange("(j p) n -> p j n", p=128), wgr, writes=[wgr])
    P.dma("pool", wu[:, :, 0:fp], gu_src[:, ff + f0:ff + f0 + fp].rearrange("(j p) n -> p j n", p=128), wur, writes=[wur])
    P.dma("pool", wd[:, 0:nf, :], d_src[f0:f0 + fp, :].rearrange("(j p) n -> p j n", p=128), wdr, writes=[wdr])
    P.op("pool", lambda e: e.tensor_tensor(out=wd[:, 0:nf, :], in0=wd[:, 0:nf, :],
                                           in1=g2b[0][:].unsqueeze(1).broadcast_to([128, nf, 1024]), op=ALU.mult),
         reads=[wdr, g2b[1]], writes=[wdr])
    return s


def expert_compute(k, s, fp, hT, hT_r, xt, xt_r, comb):
    P = k.P
    nf = fp // 128
    wg, wgr = k.wg[s]
    wu, wur = k.wu[s]
    wd, wdr = k.wd[s]
    aT, aTr = k.aT[rr(k, "aT", 2)]
    for fc in range(nf):
        for half in range(2):
            i = rr(k, "gu", 2)
            pg, pgr = k.pb[i]
            pu, pur = k.pb[2 + i]
            sg, sgr = k.sg[i]
            P.op("pe", [lambda e, j=j: e.matmul(pg[:], lhsT=wg[:, j, fc * 128:(fc + 1) * 128], rhs=hT[:, j, half * 512:(half + 1) * 512],
                                                start=(j == 0), stop=(j == 7)) for j in range(8)],
                 reads=[wgr, hT_r], writes=[pgr])
            P.op("pe", [lambda e, j=j: e.matmul(pu[:], lhsT=wu[:, j, fc * 128:(fc + 1) * 128], rhs=hT[:, j, half * 512:(half + 1) * 512],
                                                start=(j == 0), stop=(j == 7)) for j in range(8)],
                 reads=[wur, hT_r], writes=[pur])
            P.op("act", lambda e: e.activation(out=sg[:], in_=pg[:], func=AF.Silu), reads=[pgr], writes=[sgr])
            P.op("dve", lambda e: e.tensor_tensor(out=aT[:, fc, half * 512:(half + 1) * 512], in0=sg[:], in1=pu[:], op=ALU.mult),
                 reads=[sgr, pur], writes=[aTr])
    for sub in range(8):
        for dh in range(2):
            pd, pdr = k.pb[4 + rr(k, "pd", 2)]
            P.op("pe", [lambda e, fc=fc: e.matmul(pd[:], lhsT=aT[:, fc, sub * 128:(sub + 1) * 128], rhs=wd[:, fc, dh * 512:(dh + 1) * 512],
                                                  start=(fc == 0), stop=(fc == nf - 1)) for fc in range(nf)],
                 reads=[aTr, wdr], writes=[pdr])
            xs = xt[:, sub, dh * 512:(dh + 1) * 512]
            if comb is None:
                P.op("dve", lambda e: e.tensor_tensor(out=xs, in0=pd[:], in1=xs, op=ALU.add), reads=[pdr, xt_r], writes=[xt_r])
            else:
                ct, cr, ei = comb
                P.op("dve", lambda e: e.scalar_tensor_tensor(out=xs, in0=pd[:], scalar=ct[:, sub, ei:ei + 1], in1=xs,
                                                             op0=ALU.mult, op1=ALU.add), reads=[pdr, xt_r, cr], writes=[xt_r])


FFN_PIECES = [(0, 512), (512, 512), (1024, 512), (1536, 512), (2048, 512), (2560, 256)]
MOE_PIECES = [(i * 512, 512) for i in range(7)]


def even_layer(k, io, x_in, x_halo, x_out):
    P, C = k.P, k.C
    C.push()
    bc = ada_phase(k, io, True)
    sh1, gain1, gs1, sh2, gain2, g2b = bc
    poolA, poolA_r = C.sb([128, 12, 128], F32, "poolA")
    P.dma("sp", poolA[:], io["poolA"], poolA_r, writes=[poolA_r])
    wp, wp_r = C.sb([128, 4, 2, 256], BF16, "wpool")
    P.dma("pool", wp[:], io["pool_w"].rearrange("g (c p) n -> p g c n", p=128), wp_r, writes=[wp_r])
    xt, xt_r = C.sb([128, 8, 1024], F32, "xt")
    hf = [C.sb([128, 1024], F32, "hf") for _ in range(3)]
    tmp = [C.sb([128, 1024], F32, "tmp") for _ in range(2)]
    junk = C.sb([128, 1024], F32, "junk")
    pT = [C.sb([128, 8, 128], BF16, "pT") for _ in range(2)]
    hb = [C.sb([128, 1024], BF16, "hb") for _ in range(2)]
    hT, hT_r = C.sb([128, 8, 1024], BF16, "hT")
    ss, ss_r = C.sb([128, 8], F32, "ss")
    alloc_expert_bufs(k, 2)

    xh, xh_r = tmp[0]
    P.dma("sp", xh[:], x_halo[THALO - 128:THALO, :], xh_r, writes=[xh_r])
    sumsq(k, xh[:], xh_r, ss, ss_r, 0, junk)
    rstd_from_ss(k, ss, ss_r, 1, 1.0 / D)
    hprev = hf[rr(k, "hf", 3)]
    mod_norm(k, xh[:], xh_r, ss[:, 0:1], ss_r, gain1, sh1, tmp[1], hprev[0][:], hprev[1])
    P.op("dve", lambda e: e.tensor_scalar(out=hprev[0][:], in0=hprev[0][:], scalar1=k.hv[:, 0:1], scalar2=None, op0=ALU.mult),
         reads=[hprev[1], k.hv_r], writes=[hprev[1]])

    work = [(t, pc) for t in range(4) for pc in FFN_PIECES]
    slots = {}
    slots[0] = expert_load(k, io["ffn_w_gu"], io["ffn_w_down"], work[0][1][0], work[0][1][1], DFF, g2b)
    wi = 0
    for t in range(4):
        t0 = t * 1024
        P.dma("sp", xt[:], x_in[t0:t0 + 1024, :].rearrange("(s p) d -> p s d", p=128), xt_r, writes=[xt_r])
        for s in range(8):
            sumsq(k, xt[:, s, :], xt_r, ss, ss_r, s, junk)
        rstd_from_ss(k, ss, ss_r, 8, 1.0 / D)
        for s in range(8):
            hcur = hf[rr(k, "hf", 3)]
            mod_norm(k, xt[:, s, :], xt_r, ss[:, s:s + 1], ss_r, gain1, sh1, tmp[rr(k, "tmp", 2)], hcur[0][:], hcur[1])
            first = (t == 0 and s == 0)
            pp0, pp0r = k.pb[0]
            pp1, pp1r = k.pb[1]
            fns = []
            for c in range(8):
                g = c // 2
                pp = pp0 if c < 4 else pp1
                o = pp[:].rearrange("p (c t) -> p c t", c=4)[:, c % 4, :]
                fns.append(lambda e, c=c, g=g, o=o: e.matmul(o, lhsT=hprev[0][:, c * 128:(c + 1) * 128], rhs=poolA[:, 4 + g, :],
                                                            start=True, stop=False))
                fns.append(lambda e, c=c, g=g, o=o: e.matmul(o, lhsT=hcur[0][:, c * 128:(c + 1) * 128],
                                                            rhs=poolA[:, (8 if first else 0) + g, :], start=False, stop=True))
            P.op("pe", fns, reads=[hprev[1], hcur[1], poolA_r], writes=[pp0r, pp1r])
            pt, ptr = pT[rr(k, "pT", 2)]
            P.op("act", lambda e: e.copy(out=pt[:, 0:4, :], in_=pp0[:].rearrange("p (c t) -> p c t", c=4)), reads=[pp0r], writes=[ptr])
            P.op("act", lambda e: e.copy(out=pt[:, 4:8, :], in_=pp1[:].rearrange("p (c t) -> p c t", c=4)), reads=[pp1r], writes=[ptr])
            py0, py0r = k.pb[4]
            py1, py1r = k.pb[5]
            fns = []
            for g in range(4):
                py = py0 if g < 2 else py1
                for cc in range(2):
                    fns.append(lambda e, g=g, cc=cc, py=py: e.matmul(py[:, (g % 2) * 256:(g % 2) * 256 + 256], lhsT=pt[:, 2 * g + cc, :],
                                                                   rhs=wp[:, g, cc, :], start=(cc == 0), stop=(cc == 1)))
            P.op("pe", fns, reads=[ptr, wp_r], writes=[py0r, py1r])
            for hh, (py, pyr) in enumerate(((py0, py0r), (py1, py1r))):
                tq, tqr = tmp[rr(k, "tmp", 2)]
                P.op("dve", lambda e: e.tensor_tensor(out=tq[:, 0:512], in0=py[:], in1=gs1[0][:, hh * 512:(hh + 1) * 512], op=ALU.mult),
                     reads=[pyr, gs1[1]], writes=[tqr])
                P.op("pool", lambda e: e.tensor_tensor(out=xt[:, s, hh * 512:(hh + 1) * 512], in0=tq[:, 0:512],
                                                       in1=xt[:, s, hh * 512:(hh + 1) * 512], op=ALU.add), reads=[tqr, xt_r], writes=[xt_r])
            hprev = hcur
        for s in range(8):
            sumsq(k, xt[:, s, :], xt_r, ss, ss_r, s, junk)
        rstd_from_ss(k, ss, ss_r, 8, 1.0 / D)
        for s in range(8):
            h2 = hb[rr(k, "hb", 2)]
            mod_norm(k, xt[:, s, :], xt_r, ss[:, s:s + 1], ss_r, gain2, sh2, tmp[rr(k, "tmp", 2)], h2[0][:], h2[1])
            transpose_bf16(k, h2[0], h2[1], hT, hT_r, s * 128)
        for (f0, fp) in FFN_PIECES:
            if wi + 1 < len(work):
                nf0, nfp = work[wi + 1][1]
                slots[wi + 1] = expert_load(k, io["ffn_w_gu"], io["ffn_w_down"], nf0, nfp, DFF, g2b)
            expert_compute(k, slots[wi], fp, hT, hT_r, xt, xt_r, None)
            wi += 1
        P.dma("sp", x_out[t0:t0 + 1024, :].rearrange("(s p) d -> p s d", p=128), xt[:], xt_r, reads=[xt_r])
    P.barrier()
    C.pop()


def make_poolA(first_special):
    A = np.zeros((128, 12, 128), np.float32)
    tp = np.arange(128)[:, None]
    t = np.arange(128)[None, :]
    eye = (tp == t).astype(np.float32)
    for g, w in enumerate((2, 4, 8, 16)):
        cur = ((t - tp >= 0) & (t - tp < w)).astype(np.float32)
        prv = ((t - (tp - 128) >= 0) & (t - (tp - 128) < w)).astype(np.float32)
        A[:, g, :] = cur / w - eye
        A[:, 4 + g, :] = prv / w
        cnt = np.minimum(t + 1, w).astype(np.float32)
        A[:, 8 + g, :] = (cur / cnt - eye) if first_special else A[:, g, :]
    return A


_COMMON_IO = [("x_own", [TOWN, D]), ("x_halo", [THALO, D]), ("c_col", [128, 8]), ("hv", [128, 1]), ("ident", [128, 128]),
              ("ada_w", [D, 6 * D]), ("ada_b", [1, 6 * D]), ("norm1_g", [1, D]), ("norm2_g", [1, D])]
_EVEN_IO = [("poolA", [128, 12, 128]), ("pool_scale", [1, D]), ("pool_w", [4, 256, 256]),
            ("ffn_w_gu", [D, 2 * DFF]), ("ffn_w_down", [DFF, D])]


def new_k(nc):
    k = K()
    k.nc = nc
    k.P = Prog(nc)
    k.C = Ctx(nc, k.P)
    return k


def build_even():
    nc = bass.Bass("TRN2", target_bir_lowering=False)
    io = {n: _dram_in(nc, n, s) for n, s in _COMMON_IO + _EVEN_IO}
    x_out = nc.dram_tensor("x_out", [TOWN, D], F32, kind="ExternalOutput").ap()
    k = new_k(nc)
    k.C.push()
    setup_common(k, io)
    even_layer(k, io, io["x_own"], io["x_halo"], x_out)
    k.P.barrier()
    k.C.pop()
    return nc


def core_common_inputs(x_full, c, core):
    b, half = core // 2, core % 2
    s0 = half * TOWN
    x_own = np.ascontiguousarray(x_full[b, s0:s0 + TOWN])
    if half == 0:
        x_halo = np.zeros((THALO, D), np.float32)
    else:
        x_halo = np.ascontiguousarray(x_full[b, s0 - THALO:s0])
    return {
        "x_own": x_own, "x_halo": x_halo,
        "c_col": np.ascontiguousarray(c[b].reshape(8, 128).T),
        "hv": np.full((128, 1), float(half), np.float32),
        "ident": np.eye(128, dtype=np.float32),
    }


def run_even(nc, x_full, inp, i):
    j = i // 2
    maps = []
    for core in range(NCORES):
        m = core_common_inputs(x_full, inp["c"], core)
        m.update({
            "ada_w": inp["ada_w"][i], "ada_b": inp["ada_b"][i][None, :], "norm1_g": inp["norm1_g"][i][None, :],
            "norm2_g": inp["norm2_g"][i][None, :], "poolA": make_poolA(core % 2 == 0),
            "pool_scale": inp["pool_scale"][j][None, :], "pool_w": inp["pool_w"][j],
            "ffn_w_gu": inp["ffn_w_gu"][j], "ffn_w_down": inp["ffn_w_down"][j],
        })
        maps.append(m)
    res = run_bass_kernel_spmd(nc, maps, core_ids=list(range(NCORES)))
    out = np.empty_like(x_full)
    for core in range(NCORES):
        b, half = core // 2, core % 2
        out[b, half * TOWN:(half + 1) * TOWN] = res.results[core]["x_out"]
    return out


def grp(g):
    d = DIL[g]
    halo = 128 * d
    T = halo + TOWN
    return d, halo, T, T // 128, (T // d) // 128


def attn_phase_a(k, io, g, bc, scr):
    P, C = k.P, k.C
    sh1, gain1 = bc[0], bc[1]
    d, halo, T, nblk, nbr = grp(g)
    C.push()
    hT, hT_r = C.sb([128, 8, T], BF16, "hTg")
    C.push()
    xt, xt_r = C.sb([128, 4, 1024], F32, "xa")
    tmp = [C.sb([128, 1024], F32, "tmpa") for _ in range(2)]
    hb = [C.sb([128, 1024], BF16, "hba") for _ in range(2)]
    junk = C.sb([128, 1024], F32, "junka")
    ss, ss_r = C.sb([128, 8], F32, "ssa")
    for t in range((T + 511) // 512):
        u0 = t * 512
        ns_ = min(4, (T - u0) // 128)
        for s in range(ns_):
            u = u0 + s * 128
            if u < halo:
                src = io["x_halo"][THALO - halo + u: THALO - halo + u + 128, :]
            else:
                src = k.x_in[u - halo: u - halo + 128, :]
            P.dma("sp", xt[:, s, :], src, xt_r, writes=[xt_r])
        for s in range(ns_):
            sumsq(k, xt[:, s, :], xt_r, ss, ss_r, s, junk)
        rstd_from_ss(k, ss, ss_r, 4, 1.0 / D)
        for s in range(ns_):
            h = hb[rr(k, "hba", 2)]
            mod_norm(k, xt[:, s, :], xt_r, ss[:, s:s + 1], ss_r, gain1, sh1, tmp[rr(k, "tmpa", 2)], h[0][:], h[1])
            tp, tpr = k.tp[rr(k, "tp", 2)]
            P.op("pe", [lambda e, j=j: e.transpose(out=tp[:, j, :], in_=h[0][:, j * 128:(j + 1) * 128], identity=k.identb[:])
                        for j in range(8)], reads=[h[1], k.identb_r], writes=[tpr])
            us = u0 + s * 128
            if d == 1:
                P.op("act", lambda e: e.copy(out=hT[:, :, us:us + 128], in_=tp[:]), reads=[tpr], writes=[hT_r])
            else:
                o = hT[:].rearrange("p k (r j) -> p k j r", r=d)[:, :, us // d: us // d + 128 // d, :]
                i_ = tp[:].rearrange("p k (j r) -> p k j r", r=d)
                for j4 in range(0, 8, 4):
                    P.op("act", lambda e: e.copy(out=o[:, j4:j4 + 4], in_=i_[:, j4:j4 + 4]), reads=[tpr], writes=[hT_r])
    P.barrier()
    C.pop()
    posi, posi_r = C.sb([128, nblk], I32, "posi")
    P.dma("sp", posi[:], io["pos%d" % g], posi_r, writes=[posi_r])
    posf, posf_r = C.sb([128, nblk], F32, "posf")
    P.op("dve", lambda e: e.tensor_copy(out=posf[:], in_=posi[:]), reads=[posi_r], writes=[posf_r])
    invf, invf_r = C.sb([128, 16], F32, "invf")
    P.dma("sp", invf[:], io["invf"], invf_r, writes=[invf_r])
    pi_t, pi_r = C.sb([128, 1], F32, "pi")
    P.op("pool", lambda e: e.memset(pi_t[:], float(np.pi)), writes=[pi_r])
    cosT, cos_r = C.sb([128, nblk, 16], F32, "cosT")
    sinT, sin_r = C.sb([128, nblk, 16], F32, "sinT")
    ang, ang_r = C.sb([128, nblk, 16], F32, "ang")
    P.op("dve", lambda e: e.tensor_tensor(out=ang[:], in0=posf[:].unsqueeze(2).broadcast_to([128, nblk, 16]),
                                          in1=invf[:].unsqueeze(1).broadcast_to([128, nblk, 16]), op=ALU.mult),
         reads=[posf_r, invf_r], writes=[ang_r])
    TWO_PI = float(2.0 * np.pi)
    ki, ki_r = C.sb([128, nblk, 16], I32, "ki")
    kf, kf_r = C.sb([128, nblk, 16], F32, "kf")

    def sin_of(dst, dst_r, shift):
        P.op("dve", lambda e: e.tensor_scalar(out=dst[:], in0=ang[:], scalar1=shift, scalar2=None, op0=ALU.add), reads=[ang_r], writes=[dst_r])
        P.op("dve", lambda e: e.tensor_scalar(out=kf[:], in0=dst[:], scalar1=1.0 / TWO_PI, scalar2=None, op0=ALU.mult), reads=[dst_r], writes=[kf_r])
        P.op("dve", lambda e: e.tensor_copy(out=ki[:], in_=kf[:]), reads=[kf_r], writes=[ki_r])
        P.op("dve", lambda e: e.tensor_copy(out=kf[:], in_=ki[:]), reads=[ki_r], writes=[kf_r])
        P.op("dve", lambda e: e.scalar_tensor_tensor(out=dst[:], in0=kf[:], scalar=-TWO_PI, in1=dst[:], op0=ALU.mult, op1=ALU.add),
             reads=[kf_r, dst_r], writes=[dst_r])
        P.op("dve", lambda e: e.tensor_scalar(out=kf[:], in0=dst[:], scalar1=float(np.pi), scalar2=-TWO_PI, op0=ALU.is_gt, op1=ALU.mult),
             reads=[dst_r], writes=[kf_r])
        P.op("dve", lambda e: e.tensor_tensor(out=dst[:], in0=dst[:], in1=kf[:], op=ALU.add), reads=[dst_r, kf_r], writes=[dst_r])
        P.op("dve", lambda e: e.tensor_scalar(out=kf[:], in0=dst[:], scalar1=float(-np.pi), scalar2=TWO_PI, op0=ALU.is_lt, op1=ALU.mult),
             reads=[dst_r], writes=[kf_r])
        P.op("dve", lambda e: e.tensor_tensor(out=dst[:], in0=dst[:], in1=kf[:], op=ALU.add), reads=[dst_r, kf_r], writes=[dst_r])
        P.op("dve", lambda e: e.tensor_scalar(out=dst[:], in0=dst[:], scalar1=3.1415925, scalar2=-3.1415925, op0=ALU.min, op1=ALU.max),
             reads=[dst_r], writes=[dst_r])
        P.op("act", lambda e: e.activation(out=dst[:], in_=dst[:], func=AF.Sin), reads=[dst_r], writes=[dst_r])

    sin_of(sinT, sin_r, 0.0)
    sin_of(cosT, cos_r, float(np.pi / 2))
    grow, grow_r = C.sb([1, 256], F32, "qkg")
    P.dma("sp", grow[:, 0:128], io["q_norm_g"][g:g + 1, :], grow_r, writes=[grow_r])
    P.dma("sp", grow[:, 128:256], io["k_norm_g"][g:g + 1, :], grow_r, writes=[grow_r])
    gb, gb_r = C.sb([128, 2, 128], F32, "qkgb")
    pt, pr = k.pb[0]
    P.op("pe", lambda e: e.matmul(pt[:, 0:256], lhsT=k.ones[0:1, :], rhs=grow[0:1, :], start=True, stop=True), reads=[grow_r, k.ones_r], writes=[pr])
    P.op("act", lambda e: e.copy(out=gb[:].rearrange("p a b -> p (a b)"), in_=pt[:, 0:256]), reads=[pr], writes=[gb_r])
    NB = 8
    wq = [C.sb([128, 8, 512], BF16, "wqkv") for _ in range(2)]
    vst = [C.sb([128, NB, 512], BF16, "vst") for _ in range(2)]
    qst = [C.sb([128, 4, NB * 128], BF16, "qst") for _ in range(2)]
    sq = [C.sb([128, 512], F32, "sq") for _ in range(2)]
    qn = [C.sb([128, 512], F32, "qn") for _ in range(2)]
    qb = [C.sb([128, 512], BF16, "qb") for _ in range(2)]
    rt = [C.sb([128, 4, 4, 16], F32, "rt") for _ in range(2)]
    s4 = [C.sb([128, 4], F32, "s4") for _ in range(3)]
    for typ in range(3):
        for hh in range(2):
            w, w_r = wq[rr(k, "wqkv", 2)]
            c0 = g * 3072 + typ * 1024 + hh * 512
            P.dma("pool", w[:], io["attn_w_qkv"][:, c0:c0 + 512].rearrange("(j p) n -> p j n", p=128), w_r, writes=[w_r])
            blocks = [bi for bi in range(nblk) if (typ != 0 or bi % nbr != 0)]
            for c_ in range(0, len(blocks), NB):
                chunk = blocks[c_:c_ + NB]
                if typ == 2:
                    st, st_r = vst[rr(k, "vst", 2)]
                else:
                    st, st_r = qst[rr(k, "qst", 2)]
                for si, bi in enumerate(chunk):
                    ps, ps_r = k.pb[2 + rr(k, "pqkv", 4)]
                    P.op("pe", [lambda e, j=j: e.matmul(ps[:], lhsT=hT[:, j, bi * 128:(bi + 1) * 128], rhs=w[:, j, :], start=(j == 0), stop=(j == 7))
                                for j in range(8)], reads=[hT_r, w_r], writes=[ps_r])
                    is_halo = (bi % nbr == 0)
                    if typ == 2:
                        if is_halo:
                            P.op("act", lambda e: e.activation(out=st[:, si, :], in_=ps[:], func=AF.Copy, scale=k.hv[:, 0:1]),
                                 reads=[ps_r, k.hv_r], writes=[st_r])
                        else:
                            P.op("act", lambda e: e.copy(out=st[:, si, :], in_=ps[:]), reads=[ps_r], writes=[st_r])
                        continue
                    sqt, sq_r = sq[rr(k, "sq", 2)]
                    s4t, s4_r = s4[rr(k, "s4", 3)]
                    qnt, qn_r = qn[rr(k, "qn", 2)]
                    qbt, qb_r = qb[rr(k, "qb", 2)]
                    rtt, rt_r = rt[rr(k, "rt", 2)]
                    P.op("act", lambda e: e.activation(out=sqt[:], in_=ps[:], func=AF.Square), reads=[ps_r], writes=[sq_r])
                    P.op("dve", lambda e: e.tensor_reduce(out=s4t[:], in_=sqt[:].rearrange("p (h e) -> p h e", h=4), axis=AX.X, op=ALU.add),
                         reads=[sq_r], writes=[s4_r])
                    rstd_from_ss(k, s4t, s4_r, 4, 1.0 / 128)
                    P.op("dve", lambda e: e.tensor_tensor(out=qnt[:].rearrange("p (h e) -> p h e", h=4), in0=ps[:].rearrange("p (h e) -> p h e", h=4),
                                                          in1=s4t[:].unsqueeze(2).broadcast_to([128, 4, 128]), op=ALU.mult),
                         reads=[ps_r, s4_r], writes=[qn_r])
                    q3 = qnt[:].rearrange("p (h e) -> p h e", h=4)
                    P.op("pool", lambda e: e.tensor_tensor(out=q3, in0=q3, in1=gb[:, typ, :].unsqueeze(1).broadcast_to([128, 4, 128]), op=ALU.mult),
                         reads=[qn_r, gb_r], writes=[qn_r])
                    cb_ = cosT[:, bi, :].unsqueeze(1).broadcast_to([128, 4, 16])
                    sb_ = sinT[:, bi, :].unsqueeze(1).broadcast_to([128, 4, 16])
                    x1, x2 = q3[:, :, 0:16], q3[:, :, 16:32]
                    P.op("pool", [lambda e: e.tensor_tensor(out=rtt[:, 0], in0=x1, in1=cb_, op=ALU.mult),
                                  lambda e: e.tensor_tensor(out=rtt[:, 1], in0=x2, in1=sb_, op=ALU.mult),
                                  lambda e: e.tensor_tensor(out=rtt[:, 2], in0=x2, in1=cb_, op=ALU.mult),
                                  lambda e: e.tensor_tensor(out=rtt[:, 3], in0=x1, in1=sb_, op=ALU.mult)],
                         reads=[qn_r, cos_r, sin_r], writes=[rt_r])
                    b3 = qbt[:].rearrange("p (h e) -> p h e", h=4)
                    P.op("dve", [lambda e: e.tensor_tensor(out=b3[:, :, 0:16], in0=rtt[:, 0], in1=rtt[:, 1], op=ALU.subtract),
                                 lambda e: e.tensor_tensor(out=b3[:, :, 16:32], in0=rtt[:, 2], in1=rtt[:, 3], op=ALU.add)],
                         reads=[rt_r], writes=[qb_r])
                    P.op("act", lambda e: e.copy(out=b3[:, :, 32:128], in_=q3[:, :, 32:128]), reads=[qn_r], writes=[qb_r])
                    tp, tpr = k.tp[rr(k, "tp", 2)]
                    P.op("pe", [lambda e, h_=h_: e.transpose(out=tp[:, h_, :], in_=qbt[:, h_ * 128:(h_ + 1) * 128], identity=k.identb[:])
                                for h_ in range(4)], reads=[qb_r, k.identb_r], writes=[tpr])
                    P.op("act", lambda e: e.copy(out=st[:, :, si * 128:(si + 1) * 128], in_=tp[:, 0:4, :]), reads=[tpr], writes=[st_r])
                runs = []
                for si, bi in enumerate(chunk):
                    if runs and runs[-1][1] + runs[-1][2] == bi:
                        runs[-1][2] += 1
                    else:
                        runs.append([si, bi, 1])
                for si0, bi0, n in runs:
                    for hd in range(4):
                        if typ == 2:
                            dst = scr["v"][g][hh * 4 + hd][bi0 * 128:(bi0 + n) * 128, :].rearrange("(b p) e -> p b e", p=128)
                            P.dma("sp", dst, st[:, si0:si0 + n, hd * 128:(hd + 1) * 128], st_r, reads=[st_r])
                        else:
                            dst = scr["q" if typ == 0 else "k"][g][hh * 4 + hd][:, bi0 * 128:(bi0 + n) * 128]
                            P.dma("sp", dst, st[:, hd, si0 * 128:(si0 + n) * 128], st_r, reads=[st_r])
    P.barrier()
    C.pop()


def attn_phase_b(k, io, scr):
    P, C = k.P, k.C
    C.push()
    mask_f, mask_fr = C.sb([128, 256], F32, "maskf")
    P.dma("sp", mask_f[:], io["mask2"], mask_fr, writes=[mask_fr])
    mask, mask_r = C.sb([128, 256], BF16, "mask")
    P.op("dve", lambda e: e.tensor_copy(out=mask[:], in_=mask_f[:]), reads=[mask_fr], writes=[mask_r])
    gq, gq_r = C.sb([1, 768], F32, "gqk")
    P.dma("sp", gq[:, 0:384], io["q_norm_g"].rearrange("(o g) e -> o (g e)", o=1), gq_r, writes=[gq_r])
    P.dma("sp", gq[:, 384:768], io["k_norm_g"].rearrange("(o g) e -> o (g e)", o=1), gq_r, writes=[gq_r])
    mx, mx_r = C.sb([1, 4], F32, "mx")
    P.op("act", lambda e: e.activation(out=gq[:], in_=gq[:], func=AF.Abs), reads=[gq_r], writes=[gq_r])
    P.op("dve", lambda e: e.tensor_reduce(out=mx[:, 0:2], in_=gq[:].rearrange("o (a b) -> o a b", a=2), axis=AX.X, op=ALU.max),
         reads=[gq_r], writes=[mx_r])
    P.op("dve", lambda e: e.scalar_tensor_tensor(out=mx[:, 2:3], in0=mx[:, 0:1], scalar=-float(np.sqrt(128.0)), in1=mx[:, 1:2],
                                                 op0=ALU.mult, op1=ALU.mult), reads=[mx_r], writes=[mx_r])
    negc, negc_r = C.sb([128, 1], F32, "negc")
    pt, pr = k.pb[0]
    P.op("pe", lambda e: e.matmul(pt[:, 0:1], lhsT=k.ones[0:1, :], rhs=mx[0:1, 2:3], start=True, stop=True), reads=[mx_r, k.ones_r], writes=[pr])
    P.op("act", lambda e: e.copy(out=negc[:], in_=pt[:, 0:1]), reads=[pr], writes=[negc_r])
    TMAX = grp(2)[2]
    kT = [C.sb([128, TMAX], BF16, "kT") for _ in range(2)]
    qT = [C.sb([128, TMAX], BF16, "qT") for _ in range(2)]
    vv = [C.sb([128, TMAX // 128, 128], BF16, "vv") for _ in range(2)]
    acc, acc_r = C.sb([128, 2, TOWN], F32, "acc")
    Et = [C.sb([128, 256], BF16, "Et") for _ in range(3)]
    Pt = [C.sb([128, 256], BF16, "Pt") for _ in range(3)]
    oT, oT_r = C.sb([128, TOWN], BF16, "oT")
    scale = float(1.0 / np.sqrt(128.0))
    for hd in range(8):
        for g in range(3):
            d, halo, T, nblk, nbr = grp(g)
            i = rr(k, "kqv", 2)
            kt, kt_r = kT[i]
            qt, qt_r = qT[i]
            vt, vt_r = vv[i]
            P.dma("sp", kt[:, 0:T], scr["k"][g][hd], kt_r, writes=[kt_r])
            P.dma("sp", qt[:, 0:T], scr["q"][g][hd], qt_r, writes=[qt_r])
            P.dma("sp", vt[:, 0:nblk, :], scr["v"][g][hd].rearrange("(b p) e -> p b e", p=128), vt_r, writes=[vt_r])
            for r in range(d):
                for jb in range(1, nbr):
                    bq = r * nbr + jb
                    sp_, sp_r = k.pb[rr(k, "ps", 2)]
                    P.op("pe", [lambda e: e.matmul(sp_[:, 0:128], lhsT=kt[:, (bq - 1) * 128:bq * 128], rhs=qt[:, bq * 128:(bq + 1) * 128], start=True, stop=True),
                                lambda e: e.matmul(sp_[:, 128:256], lhsT=kt[:, bq * 128:(bq + 1) * 128], rhs=qt[:, bq * 128:(bq + 1) * 128], start=True, stop=True)],
                         reads=[kt_r, qt_r], writes=[sp_r])
                    ei = rr(k, "Et", 3)
                    et, et_r = Et[ei]
                    pt_, pt_r = Pt[ei]
                    P.op("act", lambda e: e.activation(out=et[:], in_=sp_[:, 0:256], func=AF.Exp, bias=negc[:, 0:1], scale=scale),
                         reads=[sp_r, negc_r], writes=[et_r])
                    P.op("pool", lambda e: e.tensor_tensor(out=pt_[:], in0=et[:], in1=mask[:], op=ALU.mult), reads=[et_r, mask_r], writes=[pt_r])
                    if jb == 1:
                        P.op("dve", lambda e: e.tensor_scalar(out=pt_[:, 0:128], in0=pt_[:, 0:128], scalar1=k.hv[:, 0:1], scalar2=None, op0=ALU.mult),
                             reads=[pt_r, k.hv_r], writes=[pt_r])
                    up, up_r = k.pb[2 + rr(k, "pu", 2)]
                    P.op("pe", [lambda e: e.matmul(up[:, 0:128], lhsT=vt[:, bq - 1, :], rhs=pt_[:, 0:128], start=True, stop=False),
                                lambda e: e.matmul(up[:, 0:128], lhsT=vt[:, bq, :], rhs=pt_[:, 128:256], start=False, stop=True),
                                lambda e: e.matmul(up[:, 128:256], lhsT=k.onesb[:], rhs=pt_[:, 0:128], start=True, stop=False),
                                lambda e: e.matmul(up[:, 128:256], lhsT=k.onesb[:], rhs=pt_[:, 128:256], start=False, stop=True)],
                         reads=[vt_r, pt_r, k.onesb_r], writes=[up_r])
                    t0 = d * 128 * (jb - 1) + r
                    dst = acc[:, :, t0: t0 + d * 127 + 1: d]
                    src = up[:, 0:256].rearrange("p (a b) -> p a b", a=2)
                    if g == 0:
                        P.op("dve", lambda e: e.tensor_copy(out=dst, in_=src), reads=[up_r], writes=[acc_r])
                    else:
                        P.op("dve", lambda e: e.tensor_tensor(out=dst, in0=src, in1=dst, op=ALU.add), reads=[up_r, acc_r], writes=[acc_r])
        P.op("dve", lambda e: e.reciprocal(out=acc[:, 1, :], in_=acc[:, 1, :]), reads=[acc_r], writes=[acc_r])
        P.op("pool", lambda e: e.tensor_tensor(out=oT[:], in0=acc[:, 0, :], in1=acc[:, 1, :], op=ALU.mult), reads=[acc_r], writes=[oT_r])
        P.dma("sp", scr["o"][hd], oT[:], oT_r, reads=[oT_r])
    P.barrier()
    C.pop()


def attn_phase_c(k, io, bc, scr, x1_dst):
    P, C = k.P, k.C
    g1b = bc[2]
    C.push()
    wo, wo_r = C.sb([128, 8, 1024], BF16, "wo")
    P.dma("pool", wo[:], io["attn_w_o"].rearrange("(h p) n -> p h n", p=128), wo_r, writes=[wo_r])
    ot = [C.sb([128, 8, 512], BF16, "ot") for _ in range(2)]
    xs = [C.sb([128, 4, 1024], F32, "xc") for _ in range(2)]
    tmp = [C.sb([128, 512], F32, "tmpc") for _ in range(2)]
    for t in range(TOWN // 512):
        t0 = t * 512
        o_, o_r = ot[t % 2]
        x_, x_r = xs[t % 2]
        for hd in range(8):
            P.dma("sp", o_[:, hd, :], scr["o"][hd][:, t0:t0 + 512], o_r, writes=[o_r])
        P.dma("sp", x_[:], k.x_in[t0:t0 + 512, :].rearrange("(s p) d -> p s d", p=128), x_r, writes=[x_r])
        for s in range(4):
            for half in range(2):
                pp, pp_r = k.pb[rr(k, "pc", 4)]
                P.op("pe", [lambda e, hd=hd: e.matmul(pp[:], lhsT=o_[:, hd, s * 128:(s + 1) * 128], rhs=wo[:, hd, half * 512:(half + 1) * 512],
                                                      start=(hd == 0), stop=(hd == 7)) for hd in range(8)], reads=[o_r, wo_r], writes=[pp_r])
                tq, tq_r = tmp[rr(k, "tmpc", 2)]
                P.op("dve", lambda e: e.tensor_tensor(out=tq[:], in0=pp[:], in1=g1b[0][:, half * 512:(half + 1) * 512], op=ALU.mult),
                     reads=[pp_r, g1b[1]], writes=[tq_r])
                P.op("pool", lambda e: e.tensor_tensor(out=x_[:, s, half * 512:(half + 1) * 512], in0=tq[:], in1=x_[:, s, half * 512:(half + 1) * 512],
                                                       op=ALU.add), reads=[tq_r, x_r], writes=[x_r])
        P.dma("sp", x1_dst[t0:t0 + 512, :].rearrange("(s p) d -> p s d", p=128), x_[:], x_r, reads=[x_r])
    P.barrier()
    C.pop()


def moe_phase(k, io, bc, x1_src, x_out):
    P, C = k.P, k.C
    sh2, gain2, g2b = bc[3], bc[4], bc[5]
    C.push()
    xt, xt_r = C.sb([128, 8, 1024], F32, "xm")
    hf = [C.sb([128, 1024], F32, "hfm") for _ in range(2)]
    tmp = [C.sb([128, 1024], F32, "tmpm") for _ in range(2)]
    junk = C.sb([128, 1024], F32, "junkm")
    hT, hT_r = C.sb([128, 8, 1024], BF16, "hTm")
    h32 = [C.sb([128, 8, 128], F32, "h32") for _ in range(2)]
    ss, ss_r = C.sb([128, 8], F32, "ssm")
    rw, rw_r = C.sb([128, 8, 8], F32, "rw")
    P.dma("sp", rw[:], io["router_w"].rearrange("(j p) e -> p j e", p=128), rw_r, writes=[rw_r])
    rbrow, rbrow_r = C.sb([1, 8], F32, "rbrow")
    P.dma("sp", rbrow[:], io["router_b"], rbrow_r, writes=[rbrow_r])
    rbb, rbb_r = C.sb([128, 8], F32, "rbb")
    pt, pr = k.pb[4]
    P.op("pe", lambda e: e.matmul(pt[:, 0:8], lhsT=k.ones[0:1, :], rhs=rbrow[0:1, :], start=True, stop=True), reads=[rbrow_r, k.ones_r], writes=[pr])
    P.op("act", lambda e: e.copy(out=rbb[:], in_=pt[:, 0:8]), reads=[pr], writes=[rbb_r])
    comb, comb_r = C.sb([128, 8, 8], F32, "comb")
    rt_ = [C.sb([128, 40], F32, "rtm") for _ in range(2)]
    alloc_expert_bufs(k, 2)
    import os
    NT_ = int(os.environ.get("MOE_TILES", "4"))
    NE_ = int(os.environ.get("MOE_EXP", str(NE)))
    work = [(t, e_, pc) for t in range(NT_) for e_ in range(NE_) for pc in MOE_PIECES]
    slots = {}
    slots[0] = expert_load(k, io["moe_w_gu0"], io["moe_w_down0"], 0, 512, DFFE, g2b)
    wi = 0
    for t in range(NT_):
        t0 = t * 1024
        P.dma("sp", xt[:], x1_src[t0:t0 + 1024, :].rearrange("(s p) d -> p s d", p=128), xt_r, writes=[xt_r])
        for s in range(8):
            sumsq(k, xt[:, s, :], xt_r, ss, ss_r, s, junk)
        rstd_from_ss(k, ss, ss_r, 8, 1.0 / D)
        for s in range(8):
            h = hf[rr(k, "hfm", 2)]
            mod_norm(k, xt[:, s, :], xt_r, ss[:, s:s + 1], ss_r, gain2, sh2, tmp[rr(k, "tmpm", 2)], h[0][:], h[1])
            p0, p0r = k.pb[0]
            p1, p1r = k.pb[1]
            fns = []
            for j in range(8):
                pp = p0 if j < 4 else p1
                fns.append(lambda e, j=j, pp=pp: e.matmul(pp[:, (j % 4) * 128:(j % 4) * 128 + 128], lhsT=h[0][:, j * 128:(j + 1) * 128], rhs=k.ident[:],
                                                          start=True, stop=True))
            P.op("pe", fns, reads=[h[1], k.ident_r], writes=[p0r, p1r])
            h3, h3r = h32[rr(k, "h32", 2)]
            for half, (pp, ppr) in enumerate(((p0, p0r), (p1, p1r))):
                src = pp[:].rearrange("p (j t) -> p j t", j=4)
                P.op("act", lambda e: e.copy(out=hT[:, half * 4:(half + 1) * 4, s * 128:(s + 1) * 128], in_=src), reads=[ppr], writes=[hT_r])
                P.op("dve", lambda e: e.tensor_copy(out=h3[:, half * 4:(half + 1) * 4, :], in_=src), reads=[ppr], writes=[h3r])
            pl, plr = k.pb[4 + rr(k, "pl", 2)]
            P.op("pe", [lambda e, j=j: e.matmul(pl[:, 0:8], lhsT=h3[:, j, :], rhs=rw[:, j, :], start=(j == 0), stop=(j == 7)) for j in range(8)],
                 reads=[h3r, rw_r], writes=[plr])
            r_, r_r = rt_[rr(k, "rtm", 2)]
            lg, eq1, l2, eq2, mm = r_[:, 0:8], r_[:, 8:16], r_[:, 16:24], r_[:, 24:32], r_[:, 32:40]
            P.op("dve", [lambda e: e.tensor_tensor(out=lg, in0=pl[:, 0:8], in1=rbb[:], op=ALU.add),
                         lambda e: e.tensor_reduce(out=mm[:, 0:1], in_=lg, axis=AX.X, op=ALU.max)], reads=[plr, rbb_r], writes=[r_r])
            P.op("dve", [lambda e: e.tensor_scalar(out=eq1, in0=lg, scalar1=mm[:, 0:1], scalar2=None, op0=ALU.is_equal),
                         ], reads=[r_r], writes=[r_r])
            P.op("dve", lambda e: e.scalar_tensor_tensor(out=l2, in0=eq1, scalar=-1.0e30, in1=lg, op0=ALU.mult, op1=ALU.add), reads=[r_r], writes=[r_r])
            P.op("dve", lambda e: e.tensor_reduce(out=mm[:, 1:2], in_=l2, axis=AX.X, op=ALU.max), reads=[r_r], writes=[r_r])
            P.op("dve", lambda e: e.tensor_scalar(out=eq2, in0=l2, scalar1=mm[:, 1:2], scalar2=None, op0=ALU.is_equal), reads=[r_r], writes=[r_r])
            P.op("dve", lambda e: e.tensor_tensor(out=mm[:, 2:3], in0=mm[:, 1:2], in1=mm[:, 0:1], op=ALU.subtract), reads=[r_r], writes=[r_r])
            P.op("act", lambda e: e.activation(out=mm[:, 3:4], in_=mm[:, 2:3], func=AF.Sigmoid), reads=[r_r], writes=[r_r])
            P.op("act", lambda e: e.activation(out=mm[:, 4:5], in_=mm[:, 2:3], func=AF.Sigmoid, scale=-1.0), reads=[r_r], writes=[r_r])
            P.op("dve", lambda e: e.tensor_scalar(out=eq1, in0=eq1, scalar1=mm[:, 4:5], scalar2=None, op0=ALU.mult), reads=[r_r], writes=[r_r])
            P.op("dve", lambda e: e.scalar_tensor_tensor(out=comb[:, s, :], in0=eq2, scalar=mm[:, 3:4], in1=eq1, op0=ALU.mult, op1=ALU.add),
                 reads=[r_r], writes=[comb_r])
        for e_ in range(NE_):
            for (f0, fp) in MOE_PIECES:
                if wi + 1 < len(work):
                    _, ne, (nf0, nfp) = work[wi + 1]
                    slots[wi + 1] = expert_load(k, io["moe_w_gu%d" % ne], io["moe_w_down%d" % ne], nf0, nfp, DFFE, g2b)
                expert_compute(k, slots[wi], fp, hT, hT_r, xt, xt_r, (comb, comb_r, e_))
                wi += 1
        P.dma("sp", x_out[t0:t0 + 1024, :].rearrange("(s p) d -> p s d", p=128), xt[:], xt_r, reads=[xt_r])
    P.barrier()
    C.pop()


def alloc_scratch(nc):
    scr = {"q": [], "k": [], "v": [], "o": []}
    for g in range(3):
        T = grp(g)[2]
        scr["q"].append([nc.dram_tensor("qT_%d_%d" % (g, h), [128, T], BF16, kind="Internal").ap() for h in range(8)])
        scr["k"].append([nc.dram_tensor("kT_%d_%d" % (g, h), [128, T], BF16, kind="Internal").ap() for h in range(8)])
        scr["v"].append([nc.dram_tensor("v_%d_%d" % (g, h), [T, 128], BF16, kind="Internal").ap() for h in range(8)])
    scr["o"] = [nc.dram_tensor("oT_%d" % h, [128, TOWN], BF16, kind="Internal").ap() for h in range(8)]
    scr["x1"] = nc.dram_tensor("x1_s", [TOWN, D], F32, kind="Internal").ap()
    return scr


def odd_layer(k, io, x_in, x_out, scr, phases="abcd"):
    P, C = k.P, k.C
    C.push()
    k.x_in = x_in
    bc = ada_phase(k, io, False)
    if "a" in phases:
        for g in range(3):
            attn_phase_a(k, io, g, bc, scr)
    if "b" in phases:
        attn_phase_b(k, io, scr)
    if "c" in phases:
        attn_phase_c(k, io, bc, scr, scr["x1"] if "d" in phases else x_out)
    if "d" in phases:
        moe_phase(k, io, bc, scr["x1"] if "c" in phases else x_in, x_out)
    P.barrier()
    C.pop()


_ODD_IO = [("attn_w_qkv", [D, 9216]), ("attn_w_o", [D, D]), ("q_norm_g", [3, 128]), ("k_norm_g", [3, 128]),
           ("router_w", [D, NE]), ("router_b", [1, NE]), ("mask2", [128, 256]), ("invf", [128, 16])]
_ODD_IO += [("moe_w_gu%d" % e_, [D, 2 * DFFE]) for e_ in range(NE)] + [("moe_w_down%d" % e_, [DFFE, D]) for e_ in range(NE)]


def build_odd(phases="abcd"):
    nc = bass.Bass("TRN2", target_bir_lowering=False)
    io = {n: _dram_in(nc, n, s) for n, s in _COMMON_IO + _ODD_IO}
    for g in range(3):
        io["pos%d" % g] = _dram_in(nc, "pos%d" % g, [128, grp(g)[3]], I32)
    x_out = nc.dram_tensor("x_out", [TOWN, D], F32, kind="ExternalOutput").ap()
    scr = alloc_scratch(nc)
    k = new_k(nc)
    k.C.push()
    setup_common(k, io)
    odd_layer(k, io, io["x_own"], x_out, scr, phases)
    k.P.barrier()
    k.C.pop()
    return nc


def pos_layout(positions, core, g):
    b, half = core // 2, core % 2
    d, halo, T, nblk, nbr = grp(g)
    s0 = half * TOWN
    i = np.arange(128)[:, None]
    bi = np.arange(nblk)[None, :]
    r, jb = bi // nbr, bi % nbr
    tok = s0 - halo + d * (128 * jb + i) + r
    out = np.where(tok >= 0, positions[b][np.clip(tok, 0, SEQ - 1)], 0)
    return np.ascontiguousarray(out.astype(np.int32))


def attn_consts():
    p = np.arange(128)[:, None]
    f = np.arange(128)[None, :]
    mask2 = np.concatenate([(p >= f), (p <= f)], axis=1).astype(np.float32)
    half = 16
    inv_freq = (500000.0 ** (-np.arange(half, dtype=np.float32) * 2.0 / 32)).astype(np.float32)
    return {"mask2": mask2, "invf": np.ascontiguousarray(np.broadcast_to(inv_freq[None, :], (128, 16)))}


def run_odd(nc, x_full, inp, i):
    j = i // 2
    ac = attn_consts()
    maps = []
    for core in range(NCORES):
        m = core_common_inputs(x_full, inp["c"], core)
        m.update(ac)
        m.update({
            "ada_w": inp["ada_w"][i], "ada_b": inp["ada_b"][i][None, :], "norm1_g": inp["norm1_g"][i][None, :],
            "norm2_g": inp["norm2_g"][i][None, :],
            "attn_w_qkv": inp["attn_w_qkv"][j], "attn_w_o": inp["attn_w_o"][j], "q_norm_g": inp["q_norm_g"][j],
            "k_norm_g": inp["k_norm_g"][j], "router_w": inp["router_w"][j], "router_b": inp["router_b"][j][None, :],
        })
        for e_ in range(NE):
            m["moe_w_gu%d" % e_] = inp["moe_w_gu"][j][e_]
            m["moe_w_down%d" % e_] = inp["moe_w_down"][j][e_]
        for g in range(3):
            m["pos%d" % g] = pos_layout(inp["positions"], core, g)
        maps.append(m)
    res = run_bass_kernel_spmd(nc, maps, core_ids=list(range(NCORES)))
    out = np.empty_like(x_full)
    for core in range(NCORES):
        b, half = core // 2, core % 2
        out[b, half * TOWN:(half + 1) * TOWN] = res.results[core]["x_out"]
    return out


_PROGS = {}


def kernel(**inp):
    inp = {k_: np.asarray(v) for k_, v in inp.items()}
    x = np.ascontiguousarray(inp["x"], dtype=np.float32)
    if "even" not in _PROGS:
        _PROGS["even"] = build_even()
        _PROGS["odd"] = build_odd()
    for i in range(4):
        if i % 2 == 0:
            x = run_even(_PROGS["even"], x, inp, i)
        else:
            x = run_odd(_PROGS["odd"], x, inp, i)
    return x


_MOEX_IO = [("x1", [TOWN, D]), ("xacc", [TOWN, D]), ("mod_in", [1, 6 * D]), ("esel", [128, NE]), ("ident", [128, 128]),
            ("hv", [128, 1]), ("router_w", [D, NE]), ("router_b", [1, NE]), ("w_gu", [D, 2 * DFFE]), ("w_down", [DFFE, D])]


def moe_expert_phase(k, io, x_out):
    P, C = k.P, k.C
    import os
    MODE = os.environ.get("MOEX_MODE", "full")
    C.push()
    row, row_r = C.sb([1, 3 * D], F32, "mrow")
    P.dma("sp", row[:], io["mod_in"][:, 3 * D:6 * D], row_r, writes=[row_r])
    bc = [C.sb([128, 1024], F32, "bcx") for _ in range(3)]
    for idx in range(3):
        for half in range(2):
            pt, pr = k.pb[(idx * 2 + half) % 2]
            P.op("pe", lambda e: e.matmul(pt[:], lhsT=k.ones[0:1, :], rhs=row[0:1, idx * 1024 + half * 512: idx * 1024 + half * 512 + 512],
                                          start=True, stop=True), reads=[row_r, k.ones_r], writes=[pr])
            P.op("act", lambda e: e.copy(out=bc[idx][0][:, half * 512:(half + 1) * 512], in_=pt[:]), reads=[pr], writes=[bc[idx][1]])
    sh2, gain2, g2b = bc
    xt, xt_r = C.sb([128, 8, 1024], F32, "xm")
    x1t, x1t_r = C.sb([128, 8, 1024], F32, "x1m")
    hf = [C.sb([128, 1024], F32, "hfm") for _ in range(2)]
    tmp = [C.sb([128, 1024], F32, "tmpm") for _ in range(2)]
    junk = C.sb([128, 1024], F32, "junkm")
    hT, hT_r = C.sb([128, 8, 1024], BF16, "hTm")
    hbh = [C.sb([128, 1024], BF16, "hbh") for _ in range(2)]
    hbl = [C.sb([128, 1024], BF16, "hbl") for _ in range(2)]
    hTlo = [C.sb([128, 8, 128], BF16, "hTlo") for _ in range(2)]
    ss, ss_r = C.sb([128, 8], F32, "ssm")
    rw, rw_r = C.sb([128, 8, 8], F32, "rw")
    P.dma("sp", rw[:], io["router_w"].rearrange("(j p) e -> p j e", p=128), rw_r, writes=[rw_r])
    rwh, rwh_r = C.sb([128, 8, 8], BF16, "rwh")
    rwl, rwl_r = C.sb([128, 8, 8], BF16, "rwl")
    P.op("dve", lambda e: e.tensor_copy(out=rwh[:], in_=rw[:]), reads=[rw_r], writes=[rwh_r])
    P.op("dve", lambda e: e.tensor_tensor(out=rwl[:], in0=rw[:], in1=rwh[:], op=ALU.subtract), reads=[rw_r, rwh_r], writes=[rwl_r])
    esel, esel_r = C.sb([128, 8], F32, "esel")
    P.dma("sp", esel[:], io["esel"], esel_r, writes=[esel_r])
    rbrow, rbrow_r = C.sb([1, 8], F32, "rbrow")
    P.dma("sp", rbrow[:], io["router_b"], rbrow_r, writes=[rbrow_r])
    rbb, rbb_r = C.sb([128, 8], F32, "rbb")
    pt, pr = k.pb[4]
    P.op("pe", lambda e: e.matmul(pt[:, 0:8], lhsT=k.ones[0:1, :], rhs=rbrow[0:1, :], start=True, stop=True), reads=[rbrow_r, k.ones_r], writes=[pr])
    P.op("act", lambda e: e.copy(out=rbb[:], in_=pt[:, 0:8]), reads=[pr], writes=[rbb_r])
    comb, comb_r = C.sb([128, 8, 8], F32, "comb")
    rt_ = [C.sb([128, 48], F32, "rtm") for _ in range(2)]
    alloc_expert_bufs(k, 2)
    work = [(t, pc) for t in range(4) for pc in MOE_PIECES]
    slots = {}
    slots[0] = expert_load(k, io["w_gu"], io["w_down"], 0, 512, DFFE, g2b)
    wi = 0
    for t in range(4):
        t0 = t * 1024
        P.dma("sp", x1t[:], io["x1"][t0:t0 + 1024, :].rearrange("(s p) d -> p s d", p=128), x1t_r, writes=[x1t_r])
        P.dma("sp", xt[:], io["xacc"][t0:t0 + 1024, :].rearrange("(s p) d -> p s d", p=128), xt_r, writes=[xt_r])
        for s in range(8):
            if MODE == "pro":
                break
            sumsq(k, x1t[:, s, :], x1t_r, ss, ss_r, s, junk)
        if MODE != "pro":
            rstd_from_ss(k, ss, ss_r, 8, 1.0 / D)
        for s in range(8):
            if MODE == "pro":
                break
            h = hf[rr(k, "hfm", 2)]
            mod_norm(k, x1t[:, s, :], x1t_r, ss[:, s:s + 1], ss_r, gain2, sh2, tmp[rr(k, "tmpm", 2)], h[0][:], h[1])
            if MODE == "norm":
                continue
            hh, hh_r = hbh[rr(k, "hbh", 2)]
            hl, hl_r = hbl[rr(k, "hbl", 2)]
            P.op("act", lambda e: e.copy(out=hh[:], in_=h[0][:]), reads=[h[1]], writes=[hh_r])
            P.op("dve", lambda e: e.tensor_tensor(out=hl[:], in0=h[0][:], in1=hh[:], op=ALU.subtract), reads=[h[1], hh_r], writes=[hl_r])
            transpose_bf16(k, hh, hh_r, hT, hT_r, s * 128)
            tp, tpr = k.tp[rr(k, "tp", 2)]
            P.op("pe", [lambda e, j=j: e.transpose(out=tp[:, j, :], in_=hl[:, j * 128:(j + 1) * 128], identity=k.identb[:]) for j in range(8)],
                 reads=[hl_r, k.identb_r], writes=[tpr])
            hTl, hTl_r = hTlo[rr(k, "hTlo", 2)]
            P.op("dve", lambda e: e.tensor_copy(out=hTl[:], in_=tp[:]), reads=[tpr], writes=[hTl_r])
            if MODE in ("norouter", "noexp2"):
                P.op("pool", lambda e: e.memset(comb[:, s, :], 0.25), writes=[comb_r])
                continue
            pl, plr = k.pb[4 + rr(k, "pl", 2)]
            fns = [lambda e, j=j: e.matmul(pl[:, 0:8], lhsT=hT[:, j, s * 128:(s + 1) * 128], rhs=rwh[:, j, :], start=(j == 0), stop=False) for j in range(8)]
            fns += [lambda e, j=j: e.matmul(pl[:, 0:8], lhsT=hT[:, j, s * 128:(s + 1) * 128], rhs=rwl[:, j, :], start=False, stop=False) for j in range(8)]
            fns += [lambda e, j=j: e.matmul(pl[:, 0:8], lhsT=hTl[:, j, :], rhs=rwh[:, j, :], start=False, stop=(j == 7)) for j in range(8)]
            P.op("pe", fns, reads=[hT_r, hTl_r, rwh_r, rwl_r], writes=[plr])
            r_, r_r = rt_[rr(k, "rtm", 2)]
            lg, eq1, l2, eq2, mm, cs = r_[:, 0:8], r_[:, 8:16], r_[:, 16:24], r_[:, 24:32], r_[:, 32:40], r_[:, 40:48]
            P.op("dve", lambda e: e.tensor_tensor(out=lg, in0=pl[:, 0:8], in1=rbb[:], op=ALU.add), reads=[plr, rbb_r], writes=[r_r])
            P.op("dve", lambda e: e.tensor_reduce(out=mm[:, 0:1], in_=lg, axis=AX.X, op=ALU.max), reads=[r_r], writes=[r_r])
            P.op("dve", lambda e: e.tensor_scalar(out=eq1, in0=lg, scalar1=mm[:, 0:1], scalar2=None, op0=ALU.is_equal), reads=[r_r], writes=[r_r])
            P.op("dve", lambda e: e.scalar_tensor_tensor(out=l2, in0=eq1, scalar=-1.0e30, in1=lg, op0=ALU.mult, op1=ALU.add), reads=[r_r], writes=[r_r])
            P.op("dve", lambda e: e.tensor_reduce(out=mm[:, 1:2], in_=l2, axis=AX.X, op=ALU.max), reads=[r_r], writes=[r_r])
            P.op("dve", lambda e: e.tensor_scalar(out=eq2, in0=l2, scalar1=mm[:, 1:2], scalar2=None, op0=ALU.is_equal), reads=[r_r], writes=[r_r])
            P.op("dve", lambda e: e.tensor_tensor(out=mm[:, 2:3], in0=mm[:, 1:2], in1=mm[:, 0:1], op=ALU.subtract), reads=[r_r], writes=[r_r])
            P.op("act", lambda e: e.activation(out=mm[:, 3:4], in_=mm[:, 2:3], func=AF.Sigmoid), reads=[r_r], writes=[r_r])
            P.op("act", lambda e: e.activation(out=mm[:, 4:5], in_=mm[:, 2:3], func=AF.Sigmoid, scale=-1.0), reads=[r_r], writes=[r_r])
            P.op("dve", lambda e: e.tensor_scalar(out=eq1, in0=eq1, scalar1=mm[:, 4:5], scalar2=None, op0=ALU.mult), reads=[r_r], writes=[r_r])
            P.op("dve", lambda e: e.scalar_tensor_tensor(out=cs, in0=eq2, scalar=mm[:, 3:4], in1=eq1, op0=ALU.mult, op1=ALU.add), reads=[r_r], writes=[r_r])
            P.op("dve", lambda e: e.tensor_tensor(out=cs, in0=cs, in1=esel[:], op=ALU.mult), reads=[r_r, esel_r], writes=[r_r])
            P.op("dve", lambda e: e.tensor_reduce(out=comb[:, s, 0:1], in_=cs, axis=AX.X, op=ALU.add), reads=[r_r], writes=[comb_r])
        for (f0, fp) in MOE_PIECES:
            if MODE in ("noexp", "noexp2", "pro", "norm"):
                break
            if wi + 1 < len(work):
                nf0, nfp = work[wi + 1][1]
                slots[wi + 1] = expert_load(k, io["w_gu"], io["w_down"], nf0, nfp, DFFE, g2b)
            expert_compute(k, slots[wi], fp, hT, hT_r, xt, xt_r, (comb, comb_r, 0))
            wi += 1
        P.dma("sp", x_out[t0:t0 + 1024, :].rearrange("(s p) d -> p s d", p=128), xt[:], xt_r, reads=[xt_r])
    P.barrier()
    C.pop()


def build_moex():
    nc = bass.Bass("TRN2", target_bir_lowering=False)
    io = {n: _dram_in(nc, n, s) for n, s in _MOEX_IO}
    x_out = nc.dram_tensor("x_out", [TOWN, D], F32, kind="ExternalOutput").ap()
    k = new_k(nc)
    k.C.push()
    setup_common(k, io)
    moe_expert_phase(k, io, x_out)
    k.P.barrier()
    k.C.pop()
    return nc


def build_attn():
    nc = bass.Bass("TRN2", target_bir_lowering=False)
    io = {n: _dram_in(nc, n, s) for n, s in _COMMON_IO + _ODD_IO[:6] + [("mask2", [128, 256]), ("invf", [128, 16])]}
    for g in range(3):
        io["pos%d" % g] = _dram_in(nc, "pos%d" % g, [128, grp(g)[3]], I32)
    x_out = nc.dram_tensor("x_out", [TOWN, D], F32, kind="ExternalOutput").ap()
    io["mod_out"] = nc.dram_tensor("mod_out", [1, 6 * D], F32, kind="ExternalOutput").ap()
    scr = alloc_scratch(nc)
    k = new_k(nc)
    k.C.push()
    setup_common(k, io)
    odd_layer(k, io, io["x_own"], x_out, scr, "abc")
    k.P.barrier()
    k.C.pop()
    return nc


def _gather(res, name="x_out"):
    out = np.empty((4, SEQ, D), np.float32)
    for core in range(NCORES):
        b, half = core // 2, core % 2
        out[b, half * TOWN:(half + 1) * TOWN] = res.results[core][name]
    return out


def run_attn(nc, x_full, inp, i):
    j = i // 2
    ac = attn_consts()
    maps = []
    for core in range(NCORES):
        m = core_common_inputs(x_full, inp["c"], core)
        m.update(ac)
        m.update({
            "ada_w": inp["ada_w"][i], "ada_b": inp["ada_b"][i][None, :], "norm1_g": inp["norm1_g"][i][None, :],
            "norm2_g": inp["norm2_g"][i][None, :],
            "attn_w_qkv": inp["attn_w_qkv"][j], "attn_w_o": inp["attn_w_o"][j], "q_norm_g": inp["q_norm_g"][j],
            "k_norm_g": inp["k_norm_g"][j], "router_w": inp["router_w"][j], "router_b": inp["router_b"][j][None, :],
        })
        for g in range(3):
            m["pos%d" % g] = pos_layout(inp["positions"], core, g)
        maps.append(m)
    res = run_bass_kernel_spmd(nc, maps, core_ids=list(range(NCORES)))
    return _gather(res), [res.results[c]["mod_out"] for c in range(NCORES)]


def run_moex(nc, x1_full, xacc_full, mods, inp, i, e_):
    j = i // 2
    esel = np.zeros((128, NE), np.float32)
    esel[:, e_] = 1.0
    maps = []
    for core in range(NCORES):
        b, half = core // 2, core % 2
        maps.append({
            "x1": np.ascontiguousarray(x1_full[b, half * TOWN:(half + 1) * TOWN]),
            "xacc": np.ascontiguousarray(xacc_full[b, half * TOWN:(half + 1) * TOWN]),
            "mod_in": mods[core], "esel": esel, "ident": np.eye(128, dtype=np.float32),
            "hv": np.full((128, 1), float(half), np.float32),
            "router_w": inp["router_w"][j], "router_b": inp["router_b"][j][None, :],
            "w_gu": inp["moe_w_gu"][j][e_], "w_down": inp["moe_w_down"][j][e_],
        })
    res = run_bass_kernel_spmd(nc, maps, core_ids=list(range(NCORES)))
    return _gather(res)


def kernel(**inp):
    inp = {k_: np.asarray(v) for k_, v in inp.items()}
    x = np.ascontiguousarray(inp["x"], dtype=np.float32)
    if "even" not in _PROGS:
        _PROGS["even"] = build_even()
        _PROGS["attn"] = build_attn()
        _PROGS["moex"] = build_moex()
    for i in range(4):
        if i % 2 == 0:
            x = run_even(_PROGS["even"], x, inp, i)
        else:
            x1, mods = run_attn(_PROGS["attn"], x, inp, i)
            acc = x1
            for e_ in range(NE):
                acc = run_moex(_PROGS["moex"], x1, acc, mods, inp, i, e_)
            x = acc
    return x
```

```python
import contextlib
import numpy as np
import ml_dtypes
import concourse.bass as bass
import concourse.mybir as mybir
from concourse.bass_utils import run_bass_kernel_spmd

F32 = mybir.dt.float32
BF16 = mybir.dt.bfloat16
I32 = mybir.dt.int32
AF = mybir.ActivationFunctionType
ALU = mybir.AluOpType
AX = mybir.AxisListType

D = 1024
NCORES = 8
TOWN = 4096
THALO = 2048
SEQ = 8192
DFF = 2816
DFFE = 3584
NE = 8
EPS = 1e-6
DIL = (1, 4, 16)


class Res:
    __slots__ = ("name", "w", "rd", "dsem", "dcnt")

    def __init__(self, name):
        self.name = name
        self.w = None
        self.rd = {}
        self.dsem = None
        self.dcnt = 0


class _Eng:
    def __init__(self, key, sem):
        self.key = key
        self.sem = sem
        self.n = 0
        self.ops = []
        self.seen = {}


class Prog:
    def __init__(self, nc):
        self.nc = nc
        self.E = {}
        for k in ("pe", "act", "dve", "pool", "sp"):
            self.E[k] = _Eng(k, nc.alloc_semaphore(name="s_" + k))
        self.obj = {"pe": nc.tensor, "act": nc.scalar, "dve": nc.vector, "pool": nc.gpsimd, "sp": nc.sync}
        self.semname = {}
        self.dma_res = []

    def _deps(self, ek, reads, writes):
        need = {}

        def add(ev, kind):
            if ev is None:
                return
            s, v = ev
            own = s is self.E[ek].sem
            if own and (ek in ("pe", "sp") or kind == "war"):
                return
            key = id(s)
            self.semname[key] = s
            if need.get(key, 0) < v:
                need[key] = v

        for r in reads:
            add(r.w, "raw")
        for r in writes:
            add(r.w, "waw")
            for ev in r.rd.values():
                add(ev, "war")
        eng = self.E[ek]
        out = []
        for key, v in need.items():
            if eng.seen.get(key, 0) < v:
                eng.seen[key] = v
                out.append((self.semname[key], v))
        return out

    def op(self, ek, fn, reads=(), writes=()):
        eng = self.E[ek]
        e = self.obj[ek]
        waits = self._deps(ek, reads, writes)
        fns = fn if isinstance(fn, (list, tuple)) else [fn]
        for s, v in waits:
            e.wait_ge(s, v)
        for f in fns[:-1]:
            f(e)
        fns[-1](e).then_inc(eng.sem, 1)
        eng.n += 1
        ev = (eng.sem, eng.n)
        for r in reads:
            r.rd[ek] = ev
        for r in writes:
            r.w = ev
            r.rd = {}
        return ev

    def dma(self, qk, out, in_, sres, reads=(), writes=()):
        e = self.obj[qk]
        waits = self._deps(qk, reads, writes)
        if sres.dsem is None:
            sres.dsem = self.nc.alloc_semaphore(name="d_" + sres.name)
            self.dma_res.append(sres)
        sres.dcnt += 1
        sem = sres.dsem
        ev = (sem, 16 * sres.dcnt)
        for s, v in waits:
            e.wait_ge(s, v)
        e.dma_start(out=out, in_=in_).then_inc(sem, 16)
        for r in reads:
            r.rd[("d", id(sres))] = ev
        for r in writes:
            r.w = ev
            r.rd = {}
        return ev

    def barrier(self):
        evs = [(e.sem, e.n) for e in self.E.values() if e.n > 0]
        evs += [(r.dsem, 16 * r.dcnt) for r in self.dma_res]
        for ek, eng in self.E.items():
            for s, v in evs:
                if s is eng.sem:
                    continue
                if eng.seen.get(id(s), 0) < v:
                    eng.seen[id(s)] = v
                    self.obj[ek].wait_ge(s, v)


class Ctx:
    def __init__(self, nc, prog):
        self.nc = nc
        self.P = prog
        self.n = 0
        self.stacks = []

    def push(self):
        st = contextlib.ExitStack()
        st.__enter__()
        self.stacks.append(st)

    def pop(self):
        self.stacks.pop().__exit__(None, None, None)

    def sb(self, shape, dt, name=None):
        self.n += 1
        name = (name or "t") + "_%d" % self.n
        t = self.stacks[-1].enter_context(self.nc.sbuf_tensor(name, list(shape), dt))
        return t, Res(name)

    def ps(self, shape, dt, name=None):
        self.n += 1
        name = (name or "p") + "_%d" % self.n
        t = self.stacks[-1].enter_context(self.nc.psum_tensor(name, list(shape), dt))
        return t, Res(name)


def _dram_in(nc, name, shape, dt=F32):
    return nc.dram_tensor(name, list(shape), dt, kind="ExternalInput").ap()


class K:
    pass


def setup_common(k, io):
    P, C = k.P, k.C
    k.ident, k.ident_r = C.sb([128, 128], F32, "ident")
    P.dma("sp", k.ident[:], io["ident"], k.ident_r, writes=[k.ident_r])
    k.identb, k.identb_r = C.sb([128, 128], BF16, "identb")
    P.op("dve", lambda e: e.tensor_copy(out=k.identb[:], in_=k.ident[:]), reads=[k.ident_r], writes=[k.identb_r])
    k.ones, k.ones_r = C.sb([128, 128], F32, "ones")
    P.op("pool", lambda e: e.memset(k.ones[:], 1.0), writes=[k.ones_r])
    k.onesb, k.onesb_r = C.sb([128, 128], BF16, "onesb")
    P.op("pool", lambda e: e.memset(k.onesb[:], 1.0), writes=[k.onesb_r])
    k.hv, k.hv_r = C.sb([128, 1], F32, "hv")
    P.dma("sp", k.hv[:], io["hv"], k.hv_r, writes=[k.hv_r])
    k.tp = [C.ps([128, 8, 128], BF16, "tp") for _ in range(2)]
    k.pb = [C.ps([128, 512], F32, "pb") for _ in range(6)]
    k.cnt = {}


def rr(k, name, n):
    v = k.cnt.get(name, 0)
    k.cnt[name] = v + 1
    return v % n


def ada_phase(k, io, even):
    P, C = k.P, k.C
    bc = [C.sb([128, 1024], F32, "bc") for _ in range(6)]
    C.push()
    c_sb, c_r = C.sb([128, 8], F32)
    P.dma("sp", c_sb[:], io["c_col"], c_r, writes=[c_r])
    cact, cact_r = C.sb([128, 8], F32)
    P.op("act", lambda e: e.activation(out=cact[:], in_=c_sb[:], func=AF.Silu), reads=[c_r], writes=[cact_r])
    row, row_r = C.sb([1, 6144], F32)
    brow, brow_r = C.sb([1, 6144], F32)
    P.dma("sp", brow[:], io["ada_b"], brow_r, writes=[brow_r])
    grow, grow_r = C.sb([1, 3072], F32)
    P.dma("sp", grow[:, 0:1024], io["norm1_g"], grow_r, writes=[grow_r])
    P.dma("sp", grow[:, 1024:2048], io["norm2_g"], grow_r, writes=[grow_r])
    if even:
        P.dma("sp", grow[:, 2048:3072], io["pool_scale"], grow_r, writes=[grow_r])
    wbuf = [C.sb([128, 8, 512], F32) for _ in range(2)]
    for cb in range(12):
        wt, wr = wbuf[cb % 2]
        P.dma("sp", wt[:], io["ada_w"][:, cb * 512:(cb + 1) * 512].rearrange("(j p) n -> p j n", p=128), wr, writes=[wr])
        pt, pr = k.pb[cb % 2]
        P.op("pe", [lambda e, j=j: e.matmul(pt[0:1, :], lhsT=cact[:, j:j + 1], rhs=wt[:, j, :], start=(j == 0), stop=(j == 7))
                    for j in range(8)], reads=[cact_r, wr], writes=[pr])
        P.op("dve", lambda e: e.tensor_tensor(out=row[:, cb * 512:(cb + 1) * 512], in0=pt[0:1, :],
                                              in1=brow[:, cb * 512:(cb + 1) * 512], op=ALU.add),
             reads=[pr, brow_r], writes=[row_r])
    P.op("dve", lambda e: e.scalar_tensor_tensor(out=row[:, 1024:2048], in0=row[:, 1024:2048], scalar=1.0,
                                                 in1=grow[:, 0:1024], op0=ALU.add, op1=ALU.mult),
         reads=[row_r, grow_r], writes=[row_r])
    P.op("dve", lambda e: e.scalar_tensor_tensor(out=row[:, 4096:5120], in0=row[:, 4096:5120], scalar=1.0,
                                                 in1=grow[:, 1024:2048], op0=ALU.add, op1=ALU.mult),
         reads=[row_r, grow_r], writes=[row_r])
    if even:
        P.op("dve", lambda e: e.tensor_tensor(out=row[:, 2048:3072], in0=row[:, 2048:3072], in1=grow[:, 2048:3072],
                                              op=ALU.mult), reads=[row_r, grow_r], writes=[row_r])
    if io.get("mod_out") is not None:
        P.dma("sp", io["mod_out"], row[:], row_r, reads=[row_r])
    for idx in range(6):
        for half in range(2):
            pt, pr = k.pb[(idx * 2 + half) % 2]
            P.op("pe", lambda e: e.matmul(pt[:], lhsT=k.ones[0:1, :], rhs=row[0:1, idx * 1024 + half * 512: idx * 1024 + half * 512 + 512],
                                          start=True, stop=True), reads=[row_r, k.ones_r], writes=[pr])
            P.op("act", lambda e: e.copy(out=bc[idx][0][:, half * 512:(half + 1) * 512], in_=pt[:]), reads=[pr], writes=[bc[idx][1]])
    P.barrier()
    C.pop()
    return bc


def sumsq(k, x_ap, x_r, ss, ss_r, col, junk):
    jt, jr = junk
    k.P.op("act", lambda e: e.activation(out=jt[:], in_=x_ap, func=AF.Square, accum_out=ss[:, col:col + 1]),
           reads=[x_r], writes=[jr, ss_r])


def rstd_from_ss(k, ss, ss_r, n, inv_n):
    P = k.P
    P.op("dve", lambda e: e.tensor_scalar(out=ss[:, 0:n], in0=ss[:, 0:n], scalar1=inv_n, scalar2=EPS, op0=ALU.mult, op1=ALU.add),
         reads=[ss_r], writes=[ss_r])
    P.op("act", lambda e: e.activation(out=ss[:, 0:n], in_=ss[:, 0:n], func=AF.Sqrt), reads=[ss_r], writes=[ss_r])
    P.op("dve", lambda e: e.reciprocal(out=ss[:, 0:n], in_=ss[:, 0:n]), reads=[ss_r], writes=[ss_r])


def mod_norm(k, x_ap, x_r, rs_ap, rs_r, gain, shift, tmp, out_ap, out_r):
    P = k.P
    tt, tr = tmp
    P.op("dve", lambda e: e.scalar_tensor_tensor(out=tt[:], in0=x_ap, scalar=rs_ap, in1=gain[0][:], op0=ALU.mult, op1=ALU.mult),
         reads=[x_r, rs_r, gain[1]], writes=[tr])
    P.op("pool", lambda e: e.tensor_tensor(out=out_ap, in0=tt[:], in1=shift[0][:], op=ALU.add), reads=[tr, shift[1]], writes=[out_r])


def transpose_bf16(k, hb, hb_r, hT, hT_r, c0):
    P = k.P
    tp, tpr = k.tp[rr(k, "tp", 2)]
    P.op("pe", [lambda e, j=j: e.transpose(out=tp[:, j, :], in_=hb[:, j * 128:(j + 1) * 128], identity=k.identb[:]) for j in range(8)],
         reads=[hb_r, k.identb_r], writes=[tpr])
    P.op("act", lambda e: e.copy(out=hT[:, :, c0:c0 + 128], in_=tp[:]), reads=[tpr], writes=[hT_r])


def alloc_expert_bufs(k, nset=2):
    C = k.C
    k.wg = [C.sb([128, 8, 512], BF16, "wg") for _ in range(nset)]
    k.wu = [C.sb([128, 8, 512], BF16, "wu") for _ in range(nset)]
    k.wd = [C.sb([128, 4, 1024], BF16, "wd") for _ in range(nset)]
    k.aT = [C.sb([128, 4, 1024], BF16, "aT") for _ in range(2)]
    k.sg = [C.sb([128, 512], F32, "sg") for _ in range(2)]
    k.nset = nset


def expert_load(k, gu_src, d_src, f0, fp, ff, g2b):
    P = k.P
    s = rr(k, "wset", k.nset)
    nf = fp // 128
    wg, wgr = k.wg[s]
    wu, wur = k.wu[s]
    wd, wdr = k.wd[s]
    P.dma("pool", wg[:, :, 0:fp], gu_src[:, f0:f0 + fp].rearrange("(j p) n -> p j n", p=128), wgr, writes=[wgr])
    P.dma("pool", wu[:, :, 0:fp], gu_src[:, ff + f0:ff + f0 + fp].rearrange("(j p) n -> p j n", p=128), wur, writes=[wur])
    P.dma("pool", wd[:, 0:nf, :], d_src[f0:f0 + fp, :].rearrange("(j p) n -> p j n", p=128), wdr, writes=[wdr])
    P.op("pool", lambda e: e.tensor_tensor(out=wd[:, 0:nf, :], in0=wd[:, 0:nf, :],
                                           in1=g2b[0][:].unsqueeze(1).broadcast_to([128, nf, 1024]), op=ALU.mult),
         reads=[wdr, g2b[1]], writes=[wdr])
    return s


def expert_compute(k, s, fp, hT, hT_r, xt, xt_r, comb):
    P = k.P
    nf = fp // 128
    wg, wgr = k.wg[s]
    wu, wur = k.wu[s]
    wd, wdr = k.wd[s]
    aT, aTr = k.aT[rr(k, "aT", 2)]
    for fc in range(nf):
        for half in range(2):
            i = rr(k, "gu", 2)
            pg, pgr = k.pb[i]
            pu, pur = k.pb[2 + i]
            sg, sgr = k.sg[i]
            P.op("pe", [lambda e, j=j: e.matmul(pg[:], lhsT=wg[:, j, fc * 128:(fc + 1) * 128], rhs=hT[:, j, half * 512:(half + 1) * 512],
                                                start=(j == 0), stop=(j == 7)) for j in range(8)],
                 reads=[wgr, hT_r], writes=[pgr])
            P.op("pe", [lambda e, j=j: e.matmul(pu[:], lhsT=wu[:, j, fc * 128:(fc + 1) * 128], rhs=hT[:, j, half * 512:(half + 1) * 512],
                                                start=(j == 0), stop=(j == 7)) for j in range(8)],
                 reads=[wur, hT_r], writes=[pur])
            P.op("act", lambda e: e.activation(out=sg[:], in_=pg[:], func=AF.Silu), reads=[pgr], writes=[sgr])
            P.op("dve", lambda e: e.tensor_tensor(out=aT[:, fc, half * 512:(half + 1) * 512], in0=sg[:], in1=pu[:], op=ALU.mult),
                 reads=[sgr, pur], writes=[aTr])
    for sub in range(8):
        for dh in range(2):
            pd, pdr = k.pb[4 + rr(k, "pd", 2)]
            P.op("pe", [lambda e, fc=fc: e.matmul(pd[:], lhsT=aT[:, fc, sub * 128:(sub + 1) * 128], rhs=wd[:, fc, dh * 512:(dh + 1) * 512],
                                                  start=(fc == 0), stop=(fc == nf - 1)) for fc in range(nf)],
                 reads=[aTr, wdr], writes=[pdr])
            xs = xt[:, sub, dh * 512:(dh + 1) * 512]
            if comb is None:
                P.op("dve", lambda e: e.tensor_tensor(out=xs, in0=pd[:], in1=xs, op=ALU.add), reads=[pdr, xt_r], writes=[xt_r])
            else:
                ct, cr, ei = comb
                P.op("dve", lambda e: e.scalar_tensor_tensor(out=xs, in0=pd[:], scalar=ct[:, sub, ei:ei + 1], in1=xs,
                                                             op0=ALU.mult, op1=ALU.add), reads=[pdr, xt_r, cr], writes=[xt_r])


FFN_PIECES = [(0, 512), (512, 512), (1024, 512), (1536, 512), (2048, 512), (2560, 256)]
MOE_PIECES = [(i * 512, 512) for i in range(7)]


def even_layer(k, io, x_in, x_halo, x_out):
    P, C = k.P, k.C
    C.push()
    bc = ada_phase(k, io, True)
    sh1, gain1, gs1, sh2, gain2, g2b = bc
    poolA, poolA_r = C.sb([128, 12, 128], F32, "poolA")
    P.dma("sp", poolA[:], io["poolA"], poolA_r, writes=[poolA_r])
    wp, wp_r = C.sb([128, 4, 2, 256], BF16, "wpool")
    P.dma("pool", wp[:], io["pool_w"].rearrange("g (c p) n -> p g c n", p=128), wp_r, writes=[wp_r])
    xt, xt_r = C.sb([128, 8, 1024], F32, "xt")
    hf = [C.sb([128, 1024], F32, "hf") for _ in range(3)]
    tmp = [C.sb([128, 1024], F32, "tmp") for _ in range(2)]
    junk = C.sb([128, 1024], F32, "junk")
    pT = [C.sb([128, 8, 128], BF16, "pT") for _ in range(2)]
    hb = [C.sb([128, 1024], BF16, "hb") for _ in range(2)]
    hT, hT_r = C.sb([128, 8, 1024], BF16, "hT")
    ss, ss_r = C.sb([128, 8], F32, "ss")
    alloc_expert_bufs(k, 2)

    xh, xh_r = tmp[0]
    P.dma("sp", xh[:], x_halo[THALO - 128:THALO, :], xh_r, writes=[xh_r])
    sumsq(k, xh[:], xh_r, ss, ss_r, 0, junk)
    rstd_from_ss(k, ss, ss_r, 1, 1.0 / D)
    hprev = hf[rr(k, "hf", 3)]
    mod_norm(k, xh[:], xh_r, ss[:, 0:1], ss_r, gain1, sh1, tmp[1], hprev[0][:], hprev[1])
    P.op("dve", lambda e: e.tensor_scalar(out=hprev[0][:], in0=hprev[0][:], scalar1=k.hv[:, 0:1], scalar2=None, op0=ALU.mult),
         reads=[hprev[1], k.hv_r], writes=[hprev[1]])

    work = [(t, pc) for t in range(4) for pc in FFN_PIECES]
    slots = {}
    slots[0] = expert_load(k, io["ffn_w_gu"], io["ffn_w_down"], work[0][1][0], work[0][1][1], DFF, g2b)
    wi = 0
    for t in range(4):
        t0 = t * 1024
        P.dma("sp", xt[:], x_in[t0:t0 + 1024, :].rearrange("(s p) d -> p s d", p=128), xt_r, writes=[xt_r])
        for s in range(8):
            sumsq(k, xt[:, s, :], xt_r, ss, ss_r, s, junk)
        rstd_from_ss(k, ss, ss_r, 8, 1.0 / D)
        for s in range(8):
            hcur = hf[rr(k, "hf", 3)]
            mod_norm(k, xt[:, s, :], xt_r, ss[:, s:s + 1], ss_r, gain1, sh1, tmp[rr(k, "tmp", 2)], hcur[0][:], hcur[1])
            first = (t == 0 and s == 0)
            pp0, pp0r = k.pb[0]
            pp1, pp1r = k.pb[1]
            fns = []
            for c in range(8):
                g = c // 2
                pp = pp0 if c < 4 else pp1
                o = pp[:].rearrange("p (c t) -> p c t", c=4)[:, c % 4, :]
                fns.append(lambda e, c=c, g=g, o=o: e.matmul(o, lhsT=hprev[0][:, c * 128:(c + 1) * 128], rhs=poolA[:, 4 + g, :],
                                                            start=True, stop=False))
                fns.append(lambda e, c=c, g=g, o=o: e.matmul(o, lhsT=hcur[0][:, c * 128:(c + 1) * 128],
                                                            rhs=poolA[:, (8 if first else 0) + g, :], start=False, stop=True))
            P.op("pe", fns, reads=[hprev[1], hcur[1], poolA_r], writes=[pp0r, pp1r])
            pt, ptr = pT[rr(k, "pT", 2)]
            P.op("act", lambda e: e.copy(out=pt[:, 0:4, :], in_=pp0[:].rearrange("p (c t) -> p c t", c=4)), reads=[pp0r], writes=[ptr])
            P.op("act", lambda e: e.copy(out=pt[:, 4:8, :], in_=pp1[:].rearrange("p (c t) -> p c t", c=4)), reads=[pp1r], writes=[ptr])
            py0, py0r = k.pb[4]
            py1, py1r = k.pb[5]
            fns = []
            for g in range(4):
                py = py0 if g < 2 else py1
                for cc in range(2):
                    fns.append(lambda e, g=g, cc=cc, py=py: e.matmul(py[:, (g % 2) * 256:(g % 2) * 256 + 256], lhsT=pt[:, 2 * g + cc, :],
                                                                   rhs=wp[:, g, cc, :], start=(cc == 0), stop=(cc == 1)))
            P.op("pe", fns, reads=[ptr, wp_r], writes=[py0r, py1r])
            for hh, (py, pyr) in enumerate(((py0, py0r), (py1, py1r))):
                tq, tqr = tmp[rr(k, "tmp", 2)]
                P.op("dve", lambda e: e.tensor_tensor(out=tq[:, 0:512], in0=py[:], in1=gs1[0][:, hh * 512:(hh + 1) * 512], op=ALU.mult),
                     reads=[pyr, gs1[1]], writes=[tqr])
                P.op("pool", lambda e: e.tensor_tensor(out=xt[:, s, hh * 512:(hh + 1) * 512], in0=tq[:, 0:512],
                                                       in1=xt[:, s, hh * 512:(hh + 1) * 512], op=ALU.add), reads=[tqr, xt_r], writes=[xt_r])
            hprev = hcur
        for s in range(8):
            sumsq(k, xt[:, s, :], xt_r, ss, ss_r, s, junk)
        rstd_from_ss(k, ss, ss_r, 8, 1.0 / D)
        for s in range(8):
            h2 = hb[rr(k, "hb", 2)]
            mod_norm(k, xt[:, s, :], xt_r, ss[:, s:s + 1], ss_r, gain2, sh2, tmp[rr(k, "tmp", 2)], h2[0][:], h2[1])
            transpose_bf16(k, h2[0], h2[1], hT, hT_r, s * 128)
        for (f0, fp) in FFN_PIECES:
            if wi + 1 < len(work):
                nf0, nfp = work[wi + 1][1]
                slots[wi + 1] = expert_load(k, io["ffn_w_gu"], io["ffn_w_down"], nf0, nfp, DFF, g2b)
            expert_compute(k, slots[wi], fp, hT, hT_r, xt, xt_r, None)
            wi += 1
        P.dma("sp", x_out[t0:t0 + 1024, :].rearrange("(s p) d -> p s d", p=128), xt[:], xt_r, reads=[xt_r])
    P.barrier()
    C.pop()


def make_poolA(first_special):
    A = np.zeros((128, 12, 128), np.float32)
    tp = np.arange(128)[:, None]
    t = np.arange(128)[None, :]
    eye = (tp == t).astype(np.float32)
    for g, w in enumerate((2, 4, 8, 16)):
        cur = ((t - tp >= 0) & (t - tp < w)).astype(np.float32)
        prv = ((t - (tp - 128) >= 0) & (t - (tp - 128) < w)).astype(np.float32)
        A[:, g, :] = cur / w - eye
        A[:, 4 + g, :] = prv / w
        cnt = np.minimum(t + 1, w).astype(np.float32)
        A[:, 8 + g, :] = (cur / cnt - eye) if first_special else A[:, g, :]
    return A


_COMMON_IO = [("x_own", [TOWN, D]), ("x_halo", [THALO, D]), ("c_col", [128, 8]), ("hv", [128, 1]), ("ident", [128, 128]),
              ("ada_w", [D, 6 * D]), ("ada_b", [1, 6 * D]), ("norm1_g", [1, D]), ("norm2_g", [1, D])]
_EVEN_IO = [("poolA", [128, 12, 128]), ("pool_scale", [1, D]), ("pool_w", [4, 256, 256]),
            ("ffn_w_gu", [D, 2 * DFF]), ("ffn_w_down", [DFF, D])]


def new_k(nc):
    k = K()
    k.nc = nc
    k.P = Prog(nc)
    k.C = Ctx(nc, k.P)
    return k


def build_even():
    nc = bass.Bass("TRN2", target_bir_lowering=False)
    io = {n: _dram_in(nc, n, s) for n, s in _COMMON_IO + _EVEN_IO}
    x_out = nc.dram_tensor("x_out", [TOWN, D], F32, kind="ExternalOutput").ap()
    k = new_k(nc)
    k.C.push()
    setup_common(k, io)
    even_layer(k, io, io["x_own"], io["x_halo"], x_out)
    k.P.barrier()
    k.C.pop()
    return nc


def core_common_inputs(x_full, c, core):
    b, half = core // 2, core % 2
    s0 = half * TOWN
    x_own = np.ascontiguousarray(x_full[b, s0:s0 + TOWN])
    if half == 0:
        x_halo = np.zeros((THALO, D), np.float32)
    else:
        x_halo = np.ascontiguousarray(x_full[b, s0 - THALO:s0])
    return {
        "x_own": x_own, "x_halo": x_halo,
        "c_col": np.ascontiguousarray(c[b].reshape(8, 128).T),
        "hv": np.full((128, 1), float(half), np.float32),
        "ident": np.eye(128, dtype=np.float32),
    }


def run_even(nc, x_full, inp, i):
    j = i // 2
    maps = []
    for core in range(NCORES):
        m = core_common_inputs(x_full, inp["c"], core)
        m.update({
            "ada_w": inp["ada_w"][i], "ada_b": inp["ada_b"][i][None, :], "norm1_g": inp["norm1_g"][i][None, :],
            "norm2_g": inp["norm2_g"][i][None, :], "poolA": make_poolA(core % 2 == 0),
            "pool_scale": inp["pool_scale"][j][None, :], "pool_w": inp["pool_w"][j],
            "ffn_w_gu": inp["ffn_w_gu"][j], "ffn_w_down": inp["ffn_w_down"][j],
        })
        maps.append(m)
    res = run_bass_kernel_spmd(nc, maps, core_ids=list(range(NCORES)))
    out = np.empty_like(x_full)
    for core in range(NCORES):
        b, half = core // 2, core % 2
        out[b, half * TOWN:(half + 1) * TOWN] = res.results[core]["x_out"]
    return out


def grp(g):
    d = DIL[g]
    halo = 128 * d
    T = halo + TOWN
    return d, halo, T, T // 128, (T // d) // 128


def attn_phase_a(k, io, g, bc, scr):
    P, C = k.P, k.C
    sh1, gain1 = bc[0], bc[1]
    d, halo, T, nblk, nbr = grp(g)
    C.push()
    hT, hT_r = C.sb([128, 8, T], BF16, "hTg")
    C.push()
    xt, xt_r = C.sb([128, 4, 1024], F32, "xa")
    tmp = [C.sb([128, 1024], F32, "tmpa") for _ in range(2)]
    hb = [C.sb([128, 1024], BF16, "hba") for _ in range(2)]
    junk = C.sb([128, 1024], F32, "junka")
    ss, ss_r = C.sb([128, 8], F32, "ssa")
    for t in range((T + 511) // 512):
        u0 = t * 512
        ns_ = min(4, (T - u0) // 128)
        for s in range(ns_):
            u = u0 + s * 128
            if u < halo:
                src = io["x_halo"][THALO - halo + u: THALO - halo + u + 128, :]
            else:
                src = k.x_in[u - halo: u - halo + 128, :]
            P.dma("sp", xt[:, s, :], src, xt_r, writes=[xt_r])
        for s in range(ns_):
            sumsq(k, xt[:, s, :], xt_r, ss, ss_r, s, junk)
        rstd_from_ss(k, ss, ss_r, 4, 1.0 / D)
        for s in range(ns_):
            h = hb[rr(k, "hba", 2)]
            mod_norm(k, xt[:, s, :], xt_r, ss[:, s:s + 1], ss_r, gain1, sh1, tmp[rr(k, "tmpa", 2)], h[0][:], h[1])
            tp, tpr = k.tp[rr(k, "tp", 2)]
            P.op("pe", [lambda e, j=j: e.transpose(out=tp[:, j, :], in_=h[0][:, j * 128:(j + 1) * 128], identity=k.identb[:])
                        for j in range(8)], reads=[h[1], k.identb_r], writes=[tpr])
            us = u0 + s * 128
            if d == 1:
                P.op("act", lambda e: e.copy(out=hT[:, :, us:us + 128], in_=tp[:]), reads=[tpr], writes=[hT_r])
            else:
                o = hT[:].rearrange("p k (r j) -> p k j r", r=d)[:, :, us // d: us // d + 128 // d, :]
                i_ = tp[:].rearrange("p k (j r) -> p k j r", r=d)
                for j4 in range(0, 8, 4):
                    P.op("act", lambda e: e.copy(out=o[:, j4:j4 + 4], in_=i_[:, j4:j4 + 4]), reads=[tpr], writes=[hT_r])
    P.barrier()
    C.pop()
    posi, posi_r = C.sb([128, nblk], I32, "posi")
    P.dma("sp", posi[:], io["pos%d" % g], posi_r, writes=[posi_r])
    posf, posf_r = C.sb([128, nblk], F32, "posf")
    P.op("dve", lambda e: e.tensor_copy(out=posf[:], in_=posi[:]), reads=[posi_r], writes=[posf_r])
    invf, invf_r = C.sb([128, 16], F32, "invf")
    P.dma("sp", invf[:], io["invf"], invf_r, writes=[invf_r])
    pi_t, pi_r = C.sb([128, 1], F32, "pi")
    P.op("pool", lambda e: e.memset(pi_t[:], float(np.pi)), writes=[pi_r])
    cosT, cos_r = C.sb([128, nblk, 16], F32, "cosT")
    sinT, sin_r = C.sb([128, nblk, 16], F32, "sinT")
    C.push()
    ang, ang_r = C.sb([128, nblk, 16], F32, "ang")
    P.op("dve", lambda e: e.tensor_tensor(out=ang[:], in0=posf[:].unsqueeze(2).broadcast_to([128, nblk, 16]),
                                          in1=invf[:].unsqueeze(1).broadcast_to([128, nblk, 16]), op=ALU.mult),
         reads=[posf_r, invf_r], writes=[ang_r])
    TWO_PI = float(2.0 * np.pi)
    ki, ki_r = C.sb([128, nblk, 16], I32, "ki")
    kf, kf_r = C.sb([128, nblk, 16], F32, "kf")

    def sin_of(dst, dst_r, shift):
        P.op("dve", lambda e: e.tensor_scalar(out=dst[:], in0=ang[:], scalar1=shift, scalar2=None, op0=ALU.add), reads=[ang_r], writes=[dst_r])
        P.op("dve", lambda e: e.tensor_scalar(out=kf[:], in0=dst[:], scalar1=1.0 / TWO_PI, scalar2=None, op0=ALU.mult), reads=[dst_r], writes=[kf_r])
        P.op("dve", lambda e: e.tensor_copy(out=ki[:], in_=kf[:]), reads=[kf_r], writes=[ki_r])
        P.op("dve", lambda e: e.tensor_copy(out=kf[:], in_=ki[:]), reads=[ki_r], writes=[kf_r])
        P.op("dve", lambda e: e.scalar_tensor_tensor(out=dst[:], in0=kf[:], scalar=-TWO_PI, in1=dst[:], op0=ALU.mult, op1=ALU.add),
             reads=[kf_r, dst_r], writes=[dst_r])
        P.op("dve", lambda e: e.tensor_scalar(out=kf[:], in0=dst[:], scalar1=float(np.pi), scalar2=-TWO_PI, op0=ALU.is_gt, op1=ALU.mult),
             reads=[dst_r], writes=[kf_r])
        P.op("dve", lambda e: e.tensor_tensor(out=dst[:], in0=dst[:], in1=kf[:], op=ALU.add), reads=[dst_r, kf_r], writes=[dst_r])
        P.op("dve", lambda e: e.tensor_scalar(out=kf[:], in0=dst[:], scalar1=float(-np.pi), scalar2=TWO_PI, op0=ALU.is_lt, op1=ALU.mult),
             reads=[dst_r], writes=[kf_r])
        P.op("dve", lambda e: e.tensor_tensor(out=dst[:], in0=dst[:], in1=kf[:], op=ALU.add), reads=[dst_r, kf_r], writes=[dst_r])
        P.op("dve", lambda e: e.tensor_scalar(out=dst[:], in0=dst[:], scalar1=3.1415925, scalar2=-3.1415925, op0=ALU.min, op1=ALU.max),
             reads=[dst_r], writes=[dst_r])
        P.op("act", lambda e: e.activation(out=dst[:], in_=dst[:], func=AF.Sin), reads=[dst_r], writes=[dst_r])

    sin_of(sinT, sin_r, 0.0)
    sin_of(cosT, cos_r, float(np.pi / 2))
    P.barrier()
    C.pop()
    grow, grow_r = C.sb([1, 256], F32, "qkg")
    P.dma("sp", grow[:, 0:128], io["q_norm_g"][g:g + 1, :], grow_r, writes=[grow_r])
    P.dma("sp", grow[:, 128:256], io["k_norm_g"][g:g + 1, :], grow_r, writes=[grow_r])
    gb, gb_r = C.sb([128, 2, 128], F32, "qkgb")
    pt, pr = k.pb[0]
    P.op("pe", lambda e: e.matmul(pt[:, 0:256], lhsT=k.ones[0:1, :], rhs=grow[0:1, :], start=True, stop=True), reads=[grow_r, k.ones_r], writes=[pr])
    P.op("act", lambda e: e.copy(out=gb[:].rearrange("p a b -> p (a b)"), in_=pt[:, 0:256]), reads=[pr], writes=[gb_r])
    NB = 8
    wq = [C.sb([128, 8, 512], BF16, "wqkv") for _ in range(2)]
    vst = [C.sb([128, NB, 512], BF16, "vst") for _ in range(2)]
    qst = [C.sb([128, 4, NB * 128], BF16, "qst") for _ in range(2)]
    sq = [C.sb([128, 512], F32, "sq") for _ in range(4)]
    qn = [C.sb([128, 512], F32, "qn") for _ in range(4)]
    qb = [C.sb([128, 512], BF16, "qb") for _ in range(4)]
    rt = [C.sb([128, 4, 4, 16], F32, "rt") for _ in range(4)]
    s4 = [C.sb([128, 4], F32, "s4") for _ in range(4)]
    def make_block(typ, hh, w, w_r, st, st_r, si, bi):
        ps, ps_r = k.pb[rr(k, "pqkv", 6)]
        is_halo = (bi % nbr == 0)
        stages = []
        stages.append(lambda: P.op("pe", [lambda e, j=j: e.matmul(ps[:], lhsT=hT[:, j, bi * 128:(bi + 1) * 128], rhs=w[:, j, :],
                                                                 start=(j == 0), stop=(j == 7)) for j in range(8)],
                                   reads=[hT_r, w_r], writes=[ps_r]))
        if typ == 2:
            if is_halo:
                stages.append(lambda: P.op("act", lambda e: e.activation(out=st[:, si, :], in_=ps[:], func=AF.Copy, scale=k.hv[:, 0:1]),
                                           reads=[ps_r, k.hv_r], writes=[st_r]))
            else:
                stages.append(lambda: P.op("act", lambda e: e.copy(out=st[:, si, :], in_=ps[:]), reads=[ps_r], writes=[st_r]))
            return stages
        sqt, sq_r = sq[rr(k, "sq", 4)]
        s4t, s4_r = s4[rr(k, "s4", 4)]
        qnt, qn_r = qn[rr(k, "qn", 4)]
        qbt, qb_r = qb[rr(k, "qb", 4)]
        rtt, rt_r = rt[rr(k, "rt", 4)]
        tp, tpr = k.tp[rr(k, "tp", 2)]
        q3 = qnt[:].rearrange("p (h e) -> p h e", h=4)
        b3 = qbt[:].rearrange("p (h e) -> p h e", h=4)
        cb_ = cosT[:, bi, :].unsqueeze(1).broadcast_to([128, 4, 16])
        sb_ = sinT[:, bi, :].unsqueeze(1).broadcast_to([128, 4, 16])
        x1, x2 = q3[:, :, 0:16], q3[:, :, 16:32]
        stages.append(lambda: P.op("act", lambda e: e.activation(out=sqt[:], in_=ps[:], func=AF.Square), reads=[ps_r], writes=[sq_r]))

        def s2():
            P.op("dve", lambda e: e.tensor_reduce(out=s4t[:], in_=sqt[:].rearrange("p (h e) -> p h e", h=4), axis=AX.X, op=ALU.add),
                 reads=[sq_r], writes=[s4_r])
            P.op("dve", lambda e: e.tensor_scalar(out=s4t[:], in0=s4t[:], scalar1=1.0 / 128, scalar2=EPS, op0=ALU.mult, op1=ALU.add),
                 reads=[s4_r], writes=[s4_r])
        stages.append(s2)
        stages.append(lambda: P.op("act", lambda e: e.activation(out=s4t[:], in_=s4t[:], func=AF.Sqrt), reads=[s4_r], writes=[s4_r]))

        def s4_():
            P.op("dve", lambda e: e.reciprocal(out=s4t[:], in_=s4t[:]), reads=[s4_r], writes=[s4_r])
            P.op("dve", lambda e: e.tensor_tensor(out=q3, in0=ps[:].rearrange("p (h e) -> p h e", h=4),
                                                  in1=s4t[:].unsqueeze(2).broadcast_to([128, 4, 128]), op=ALU.mult),
                 reads=[ps_r, s4_r], writes=[qn_r])
        stages.append(s4_)

        def s5():
            P.op("pool", lambda e: e.tensor_tensor(out=q3, in0=q3, in1=gb[:, typ, :].unsqueeze(1).broadcast_to([128, 4, 128]), op=ALU.mult),
                 reads=[qn_r, gb_r], writes=[qn_r])
            P.op("pool", [lambda e: e.tensor_tensor(out=rtt[:, 0], in0=x1, in1=cb_, op=ALU.mult),
                          lambda e: e.tensor_tensor(out=rtt[:, 1], in0=x2, in1=sb_, op=ALU.mult),
                          lambda e: e.tensor_tensor(out=rtt[:, 2], in0=x2, in1=cb_, op=ALU.mult),
                          lambda e: e.tensor_tensor(out=rtt[:, 3], in0=x1, in1=sb_, op=ALU.mult)],
                 reads=[qn_r, cos_r, sin_r], writes=[rt_r])
        stages.append(s5)

        def s6():
            P.op("dve", [lambda e: e.tensor_tensor(out=b3[:, :, 0:16], in0=rtt[:, 0], in1=rtt[:, 1], op=ALU.subtract),
                         lambda e: e.tensor_tensor(out=b3[:, :, 16:32], in0=rtt[:, 2], in1=rtt[:, 3], op=ALU.add)],
                 reads=[rt_r], writes=[qb_r])
            P.op("act", lambda e: e.copy(out=b3[:, :, 32:128], in_=q3[:, :, 32:128]), reads=[qn_r], writes=[qb_r])
        stages.append(s6)
        stages.append(lambda: P.op("pe", [lambda e, h_=h_: e.transpose(out=tp[:, h_, :], in_=qbt[:, h_ * 128:(h_ + 1) * 128], identity=k.identb[:])
                                          for h_ in range(4)], reads=[qb_r, k.identb_r], writes=[tpr]))
        stages.append(lambda: P.op("act", lambda e: e.copy(out=st[:, :, si * 128:(si + 1) * 128], in_=tp[:, 0:4, :]), reads=[tpr], writes=[st_r]))
        return stages

    def flush(typ, hh, st, st_r, chunk):
        runs = []
        for si, bi in enumerate(chunk):
            if runs and runs[-1][1] + runs[-1][2] == bi:
                runs[-1][2] += 1
            else:
                runs.append([si, bi, 1])
        for si0, bi0, n in runs:
            for hd in range(4):
                if typ == 2:
                    dst = scr["v"][g][hh * 4 + hd][bi0 * 128:(bi0 + n) * 128, :].rearrange("(b p) e -> p b e", p=128)
                    P.dma("sp", dst, st[:, si0:si0 + n, hd * 128:(hd + 1) * 128], st_r, reads=[st_r])
                else:
                    dst = scr["q" if typ == 0 else "k"][g][hh * 4 + hd][:, bi0 * 128:(bi0 + n) * 128]
                    P.dma("sp", dst, st[:, hd, si0 * 128:(si0 + n) * 128], st_r, reads=[st_r])

    items = []
    cbs = []
    for typ in range(3):
        for hh in range(2):
            w, w_r = wq[rr(k, "wqkv", 2)]
            c0 = g * 3072 + typ * 1024 + hh * 512
            blocks = [bi for bi in range(nblk) if (typ != 0 or bi % nbr != 0)]
            first = True
            for c_ in range(0, len(blocks), NB):
                chunk = blocks[c_:c_ + NB]
                if typ == 2:
                    st, st_r = vst[rr(k, "vst", 2)]
                else:
                    st, st_r = qst[rr(k, "qst", 2)]
                for si, bi in enumerate(chunk):
                    pre = None
                    if first:
                        pre = len(cbs)
                        cbs.append((w, w_r, c0))
                        first = False
                    fl = (typ, hh, st, st_r, chunk) if si == len(chunk) - 1 else None
                    items.append((pre, (typ, hh, w, w_r, st, st_r, si, bi), fl))
    live = []
    nxt = 0
    DEPTH = 3
    while nxt < len(items) or live:
        if nxt < len(items):
            pre, args, fl = items[nxt]
            nxt += 1
            if pre is not None:
                for n_ in ([0, 1] if pre == 0 else [pre + 1]):
                    if n_ < len(cbs):
                        w_, w_r_, c0_ = cbs[n_]
                        P.dma("pool", w_[:], io["attn_w_qkv"][:, c0_:c0_ + 512].rearrange("(j p) n -> p j n", p=128), w_r_, writes=[w_r_])
            live.append([make_block(*args), 0, fl])
        for ent in list(live):
            ent[0][ent[1]]()
            ent[1] += 1
            if ent[1] == len(ent[0]):
                if ent[2] is not None:
                    flush(*ent[2])
                live.remove(ent)
    P.barrier()
    C.pop()


def attn_phase_b(k, io, scr):
    P, C = k.P, k.C
    C.push()
    mask_f, mask_fr = C.sb([128, 256], F32, "maskf")
    P.dma("sp", mask_f[:], io["mask2"], mask_fr, writes=[mask_fr])
    mask, mask_r = C.sb([128, 256], BF16, "mask")
    P.op("dve", lambda e: e.tensor_copy(out=mask[:], in_=mask_f[:]), reads=[mask_fr], writes=[mask_r])
    gq, gq_r = C.sb([1, 768], F32, "gqk")
    P.dma("sp", gq[:, 0:384], io["q_norm_g"].rearrange("(o g) e -> o (g e)", o=1), gq_r, writes=[gq_r])
    P.dma("sp", gq[:, 384:768], io["k_norm_g"].rearrange("(o g) e -> o (g e)", o=1), gq_r, writes=[gq_r])
    mx, mx_r = C.sb([1, 4], F32, "mx")
    P.op("act", lambda e: e.activation(out=gq[:], in_=gq[:], func=AF.Abs), reads=[gq_r], writes=[gq_r])
    P.op("dve", lambda e: e.tensor_reduce(out=mx[:, 0:2], in_=gq[:].rearrange("o (a b) -> o a b", a=2), axis=AX.X, op=ALU.max),
         reads=[gq_r], writes=[mx_r])
    P.op("dve", lambda e: e.scalar_tensor_tensor(out=mx[:, 2:3], in0=mx[:, 0:1], scalar=-float(np.sqrt(128.0)), in1=mx[:, 1:2],
                                                 op0=ALU.mult, op1=ALU.mult), reads=[mx_r], writes=[mx_r])
    negc, negc_r = C.sb([128, 1], F32, "negc")
    pt, pr = k.pb[0]
    P.op("pe", lambda e: e.matmul(pt[:, 0:1], lhsT=k.ones[0:1, :], rhs=mx[0:1, 2:3], start=True, stop=True), reads=[mx_r, k.ones_r], writes=[pr])
    P.op("act", lambda e: e.copy(out=negc[:], in_=pt[:, 0:1]), reads=[pr], writes=[negc_r])
    TMAX = grp(2)[2]
    kT = [C.sb([128, TMAX], BF16, "kT") for _ in range(2)]
    qT = [C.sb([128, TMAX], BF16, "qT") for _ in range(2)]
    vv = [C.sb([128, TMAX // 128, 128], BF16, "vv") for _ in range(2)]
    acc, acc_r = C.sb([128, 2, TOWN], F32, "acc")
    Et = [C.sb([128, 256], BF16, "Et") for _ in range(3)]
    Pt = [C.sb([128, 256], BF16, "Pt") for _ in range(3)]
    oT, oT_r = C.sb([128, TOWN], BF16, "oT")
    scale = float(1.0 / np.sqrt(128.0))
    for hd in range(8):
        for g in range(3):
            d, halo, T, nblk, nbr = grp(g)
            i = rr(k, "kqv", 2)
            kt, kt_r = kT[i]
            qt, qt_r = qT[i]
            vt, vt_r = vv[i]
            P.dma("sp", kt[:, 0:T], scr["k"][g][hd], kt_r, writes=[kt_r])
            P.dma("sp", qt[:, 0:T], scr["q"][g][hd], qt_r, writes=[qt_r])
            P.dma("sp", vt[:, 0:nblk, :], scr["v"][g][hd].rearrange("(b p) e -> p b e", p=128), vt_r, writes=[vt_r])
            for r in range(d):
                for jb in range(1, nbr):
                    bq = r * nbr + jb
                    sp_, sp_r = k.pb[rr(k, "ps", 2)]
                    P.op("pe", [lambda e: e.matmul(sp_[:, 0:128], lhsT=kt[:, (bq - 1) * 128:bq * 128], rhs=qt[:, bq * 128:(bq + 1) * 128], start=True, stop=True),
                                lambda e: e.matmul(sp_[:, 128:256], lhsT=kt[:, bq * 128:(bq + 1) * 128], rhs=qt[:, bq * 128:(bq + 1) * 128], start=True, stop=True)],
                         reads=[kt_r, qt_r], writes=[sp_r])
                    ei = rr(k, "Et", 3)
                    et, et_r = Et[ei]
                    pt_, pt_r = Pt[ei]
                    P.op("act", lambda e: e.activation(out=et[:], in_=sp_[:, 0:256], func=AF.Exp, bias=negc[:, 0:1], scale=scale),
                         reads=[sp_r, negc_r], writes=[et_r])
                    P.op("pool", lambda e: e.tensor_tensor(out=pt_[:], in0=et[:], in1=mask[:], op=ALU.mult), reads=[et_r, mask_r], writes=[pt_r])
                    if jb == 1:
                        P.op("dve", lambda e: e.tensor_scalar(out=pt_[:, 0:128], in0=pt_[:, 0:128], scalar1=k.hv[:, 0:1], scalar2=None, op0=ALU.mult),
                             reads=[pt_r, k.hv_r], writes=[pt_r])
                    up, up_r = k.pb[2 + rr(k, "pu", 2)]
                    P.op("pe", [lambda e: e.matmul(up[:, 0:128], lhsT=vt[:, bq - 1, :], rhs=pt_[:, 0:128], start=True, stop=False),
                                lambda e: e.matmul(up[:, 0:128], lhsT=vt[:, bq, :], rhs=pt_[:, 128:256], start=False, stop=True),
                                lambda e: e.matmul(up[:, 128:256], lhsT=k.onesb[:], rhs=pt_[:, 0:128], start=True, stop=False),
                                lambda e: e.matmul(up[:, 128:256], lhsT=k.onesb[:], rhs=pt_[:, 128:256], start=False, stop=True)],
                         reads=[vt_r, pt_r, k.onesb_r], writes=[up_r])
                    t0 = d * 128 * (jb - 1) + r
                    dst = acc[:, :, t0: t0 + d * 127 + 1: d]
                    src = up[:, 0:256].rearrange("p (a b) -> p a b", a=2)
                    if g == 0:
                        P.op("dve", lambda e: e.tensor_copy(out=dst, in_=src), reads=[up_r], writes=[acc_r])
                    else:
                        P.op("dve", lambda e: e.tensor_tensor(out=dst, in0=src, in1=dst, op=ALU.add), reads=[up_r, acc_r], writes=[acc_r])
        P.op("dve", lambda e: e.reciprocal(out=acc[:, 1, :], in_=acc[:, 1, :]), reads=[acc_r], writes=[acc_r])
        P.op("pool", lambda e: e.tensor_tensor(out=oT[:], in0=acc[:, 0, :], in1=acc[:, 1, :], op=ALU.mult), reads=[acc_r], writes=[oT_r])
        P.dma("sp", scr["o"][hd], oT[:], oT_r, reads=[oT_r])
    P.barrier()
    C.pop()


def attn_phase_c(k, io, bc, scr, x1_dst):
    P, C = k.P, k.C
    g1b = bc[2]
    C.push()
    wo, wo_r = C.sb([128, 8, 1024], BF16, "wo")
    P.dma("pool", wo[:], io["attn_w_o"].rearrange("(h p) n -> p h n", p=128), wo_r, writes=[wo_r])
    ot = [C.sb([128, 8, 512], BF16, "ot") for _ in range(2)]
    xs = [C.sb([128, 4, 1024], F32, "xc") for _ in range(2)]
    tmp = [C.sb([128, 512], F32, "tmpc") for _ in range(2)]
    for t in range(TOWN // 512):
        t0 = t * 512
        o_, o_r = ot[t % 2]
        x_, x_r = xs[t % 2]
        for hd in range(8):
            P.dma("sp", o_[:, hd, :], scr["o"][hd][:, t0:t0 + 512], o_r, writes=[o_r])
        P.dma("sp", x_[:], k.x_in[t0:t0 + 512, :].rearrange("(s p) d -> p s d", p=128), x_r, writes=[x_r])
        for s in range(4):
            for half in range(2):
                pp, pp_r = k.pb[rr(k, "pc", 4)]
                P.op("pe", [lambda e, hd=hd: e.matmul(pp[:], lhsT=o_[:, hd, s * 128:(s + 1) * 128], rhs=wo[:, hd, half * 512:(half + 1) * 512],
                                                      start=(hd == 0), stop=(hd == 7)) for hd in range(8)], reads=[o_r, wo_r], writes=[pp_r])
                tq, tq_r = tmp[rr(k, "tmpc", 2)]
                P.op("dve", lambda e: e.tensor_tensor(out=tq[:], in0=pp[:], in1=g1b[0][:, half * 512:(half + 1) * 512], op=ALU.mult),
                     reads=[pp_r, g1b[1]], writes=[tq_r])
                P.op("pool", lambda e: e.tensor_tensor(out=x_[:, s, half * 512:(half + 1) * 512], in0=tq[:], in1=x_[:, s, half * 512:(half + 1) * 512],
                                                       op=ALU.add), reads=[tq_r, x_r], writes=[x_r])
        P.dma("sp", x1_dst[t0:t0 + 512, :].rearrange("(s p) d -> p s d", p=128), x_[:], x_r, reads=[x_r])
    P.barrier()
    C.pop()


def moe_phase(k, io, bc, x1_src, x_out):
    P, C = k.P, k.C
    sh2, gain2, g2b = bc[3], bc[4], bc[5]
    C.push()
    xt, xt_r = C.sb([128, 8, 1024], F32, "xm")
    hf = [C.sb([128, 1024], F32, "hfm") for _ in range(2)]
    tmp = [C.sb([128, 1024], F32, "tmpm") for _ in range(2)]
    junk = C.sb([128, 1024], F32, "junkm")
    hT, hT_r = C.sb([128, 8, 1024], BF16, "hTm")
    h32 = [C.sb([128, 8, 128], F32, "h32") for _ in range(2)]
    ss, ss_r = C.sb([128, 8], F32, "ssm")
    rw, rw_r = C.sb([128, 8, 8], F32, "rw")
    P.dma("sp", rw[:], io["router_w"].rearrange("(j p) e -> p j e", p=128), rw_r, writes=[rw_r])
    rbrow, rbrow_r = C.sb([1, 8], F32, "rbrow")
    P.dma("sp", rbrow[:], io["router_b"], rbrow_r, writes=[rbrow_r])
    rbb, rbb_r = C.sb([128, 8], F32, "rbb")
    pt, pr = k.pb[4]
    P.op("pe", lambda e: e.matmul(pt[:, 0:8], lhsT=k.ones[0:1, :], rhs=rbrow[0:1, :], start=True, stop=True), reads=[rbrow_r, k.ones_r], writes=[pr])
    P.op("act", lambda e: e.copy(out=rbb[:], in_=pt[:, 0:8]), reads=[pr], writes=[rbb_r])
    comb, comb_r = C.sb([128, 8, 8], F32, "comb")
    rt_ = [C.sb([128, 40], F32, "rtm") for _ in range(2)]
    alloc_expert_bufs(k, 2)
    import os
    NT_ = int(os.environ.get("MOE_TILES", "4"))
    NE_ = int(os.environ.get("MOE_EXP", str(NE)))
    work = [(t, e_, pc) for t in range(NT_) for e_ in range(NE_) for pc in MOE_PIECES]
    slots = {}
    slots[0] = expert_load(k, io["moe_w_gu0"], io["moe_w_down0"], 0, 512, DFFE, g2b)
    wi = 0
    for t in range(NT_):
        t0 = t * 1024
        P.dma("sp", xt[:], x1_src[t0:t0 + 1024, :].rearrange("(s p) d -> p s d", p=128), xt_r, writes=[xt_r])
        for s in range(8):
            sumsq(k, xt[:, s, :], xt_r, ss, ss_r, s, junk)
        rstd_from_ss(k, ss, ss_r, 8, 1.0 / D)
        for s in range(8):
            h = hf[rr(k, "hfm", 2)]
            mod_norm(k, xt[:, s, :], xt_r, ss[:, s:s + 1], ss_r, gain2, sh2, tmp[rr(k, "tmpm", 2)], h[0][:], h[1])
            p0, p0r = k.pb[0]
            p1, p1r = k.pb[1]
            fns = []
            for j in range(8):
                pp = p0 if j < 4 else p1
                fns.append(lambda e, j=j, pp=pp: e.matmul(pp[:, (j % 4) * 128:(j % 4) * 128 + 128], lhsT=h[0][:, j * 128:(j + 1) * 128], rhs=k.ident[:],
                                                          start=True, stop=True))
            P.op("pe", fns, reads=[h[1], k.ident_r], writes=[p0r, p1r])
            h3, h3r = h32[rr(k, "h32", 2)]
            for half, (pp, ppr) in enumerate(((p0, p0r), (p1, p1r))):
                src = pp[:].rearrange("p (j t) -> p j t", j=4)
                P.op("act", lambda e: e.copy(out=hT[:, half * 4:(half + 1) * 4, s * 128:(s + 1) * 128], in_=src), reads=[ppr], writes=[hT_r])
                P.op("dve", lambda e: e.tensor_copy(out=h3[:, half * 4:(half + 1) * 4, :], in_=src), reads=[ppr], writes=[h3r])
            pl, plr = k.pb[4 + rr(k, "pl", 2)]
            P.op("pe", [lambda e, j=j: e.matmul(pl[:, 0:8], lhsT=h3[:, j, :], rhs=rw[:, j, :], start=(j == 0), stop=(j == 7)) for j in range(8)],
                 reads=[h3r, rw_r], writes=[plr])
            r_, r_r = rt_[rr(k, "rtm", 2)]
            lg, eq1, l2, eq2, mm = r_[:, 0:8], r_[:, 8:16], r_[:, 16:24], r_[:, 24:32], r_[:, 32:40]
            P.op("dve", [lambda e: e.tensor_tensor(out=lg, in0=pl[:, 0:8], in1=rbb[:], op=ALU.add),
                         lambda e: e.tensor_reduce(out=mm[:, 0:1], in_=lg, axis=AX.X, op=ALU.max)], reads=[plr, rbb_r], writes=[r_r])
            P.op("dve", [lambda e: e.tensor_scalar(out=eq1, in0=lg, scalar1=mm[:, 0:1], scalar2=None, op0=ALU.is_equal),
                         ], reads=[r_r], writes=[r_r])
            P.op("dve", lambda e: e.scalar_tensor_tensor(out=l2, in0=eq1, scalar=-1.0e30, in1=lg, op0=ALU.mult, op1=ALU.add), reads=[r_r], writes=[r_r])
            P.op("dve", lambda e: e.tensor_reduce(out=mm[:, 1:2], in_=l2, axis=AX.X, op=ALU.max), reads=[r_r], writes=[r_r])
            P.op("dve", lambda e: e.tensor_scalar(out=eq2, in0=l2, scalar1=mm[:, 1:2], scalar2=None, op0=ALU.is_equal), reads=[r_r], writes=[r_r])
            P.op("dve", lambda e: e.tensor_tensor(out=mm[:, 2:3], in0=mm[:, 1:2], in1=mm[:, 0:1], op=ALU.subtract), reads=[r_r], writes=[r_r])
            P.op("act", lambda e: e.activation(out=mm[:, 3:4], in_=mm[:, 2:3], func=AF.Sigmoid), reads=[r_r], writes=[r_r])
            P.op("act", lambda e: e.activation(out=mm[:, 4:5], in_=mm[:, 2:3], func=AF.Sigmoid, scale=-1.0), reads=[r_r], writes=[r_r])
            P.op("dve", lambda e: e.tensor_scalar(out=eq1, in0=eq1, scalar1=mm[:, 4:5], scalar2=None, op0=ALU.mult), reads=[r_r], writes=[r_r])
            P.op("dve", lambda e: e.scalar_tensor_tensor(out=comb[:, s, :], in0=eq2, scalar=mm[:, 3:4], in1=eq1, op0=ALU.mult, op1=ALU.add),
                 reads=[r_r], writes=[comb_r])
        for e_ in range(NE_):
            for (f0, fp) in MOE_PIECES:
                if wi + 1 < len(work):
                    _, ne, (nf0, nfp) = work[wi + 1]
                    slots[wi + 1] = expert_load(k, io["moe_w_gu%d" % ne], io["moe_w_down%d" % ne], nf0, nfp, DFFE, g2b)
                expert_compute(k, slots[wi], fp, hT, hT_r, xt, xt_r, (comb, comb_r, e_))
                wi += 1
        P.dma("sp", x_out[t0:t0 + 1024, :].rearrange("(s p) d -> p s d", p=128), xt[:], xt_r, reads=[xt_r])
    P.barrier()
    C.pop()


def alloc_scratch(nc):
    scr = {"q": [], "k": [], "v": [], "o": []}
    for g in range(3):
        T = grp(g)[2]
        scr["q"].append([nc.dram_tensor("qT_%d_%d" % (g, h), [128, T], BF16, kind="Internal").ap() for h in range(8)])
        scr["k"].append([nc.dram_tensor("kT_%d_%d" % (g, h), [128, T], BF16, kind="Internal").ap() for h in range(8)])
        scr["v"].append([nc.dram_tensor("v_%d_%d" % (g, h), [T, 128], BF16, kind="Internal").ap() for h in range(8)])
    scr["o"] = [nc.dram_tensor("oT_%d" % h, [128, TOWN], BF16, kind="Internal").ap() for h in range(8)]
    scr["x1"] = nc.dram_tensor("x1_s", [TOWN, D], F32, kind="Internal").ap()
    return scr


def odd_layer(k, io, x_in, x_out, scr, phases="abcd"):
    P, C = k.P, k.C
    C.push()
    k.x_in = x_in
    bc = ada_phase(k, io, False)
    if "a" in phases:
        for g in range(3):
            attn_phase_a(k, io, g, bc, scr)
    if "b" in phases:
        attn_phase_b(k, io, scr)
    if "c" in phases:
        attn_phase_c(k, io, bc, scr, scr["x1"] if "d" in phases else x_out)
    if "d" in phases:
        moe_phase(k, io, bc, scr["x1"] if "c" in phases else x_in, x_out)
    P.barrier()
    C.pop()


_ODD_IO = [("attn_w_qkv", [D, 9216]), ("attn_w_o", [D, D]), ("q_norm_g", [3, 128]), ("k_norm_g", [3, 128]),
           ("router_w", [D, NE]), ("router_b", [1, NE]), ("mask2", [128, 256]), ("invf", [128, 16])]
_ODD_IO += [("moe_w_gu%d" % e_, [D, 2 * DFFE]) for e_ in range(NE)] + [("moe_w_down%d" % e_, [DFFE, D]) for e_ in range(NE)]


def build_odd(phases="abcd"):
    nc = bass.Bass("TRN2", target_bir_lowering=False)
    io = {n: _dram_in(nc, n, s) for n, s in _COMMON_IO + _ODD_IO}
    for g in range(3):
        io["pos%d" % g] = _dram_in(nc, "pos%d" % g, [128, grp(g)[3]], I32)
    x_out = nc.dram_tensor("x_out", [TOWN, D], F32, kind="ExternalOutput").ap()
    scr = alloc_scratch(nc)
    k = new_k(nc)
    k.C.push()
    setup_common(k, io)
    odd_layer(k, io, io["x_own"], x_out, scr, phases)
    k.P.barrier()
    k.C.pop()
    return nc


def pos_layout(positions, core, g):
    b, half = core // 2, core % 2
    d, halo, T, nblk, nbr = grp(g)
    s0 = half * TOWN
    i = np.arange(128)[:, None]
    bi = np.arange(nblk)[None, :]
    r, jb = bi // nbr, bi % nbr
    tok = s0 - halo + d * (128 * jb + i) + r
    out = np.where(tok >= 0, positions[b][np.clip(tok, 0, SEQ - 1)], 0)
    return np.ascontiguousarray(out.astype(np.int32))


def attn_consts():
    p = np.arange(128)[:, None]
    f = np.arange(128)[None, :]
    mask2 = np.concatenate([(p >= f), (p <= f)], axis=1).astype(np.float32)
    half = 16
    inv_freq = (500000.0 ** (-np.arange(half, dtype=np.float32) * 2.0 / 32)).astype(np.float32)
    return {"mask2": mask2, "invf": np.ascontiguousarray(np.broadcast_to(inv_freq[None, :], (128, 16)))}


def run_odd(nc, x_full, inp, i):
    j = i // 2
    ac = attn_consts()
    maps = []
    for core in range(NCORES):
        m = core_common_inputs(x_full, inp["c"], core)
        m.update(ac)
        m.update({
            "ada_w": inp["ada_w"][i], "ada_b": inp["ada_b"][i][None, :], "norm1_g": inp["norm1_g"][i][None, :],
            "norm2_g": inp["norm2_g"][i][None, :],
            "attn_w_qkv": inp["attn_w_qkv"][j], "attn_w_o": inp["attn_w_o"][j], "q_norm_g": inp["q_norm_g"][j],
            "k_norm_g": inp["k_norm_g"][j], "router_w": inp["router_w"][j], "router_b": inp["router_b"][j][None, :],
        })
        for e_ in range(NE):
            m["moe_w_gu%d" % e_] = inp["moe_w_gu"][j][e_]
            m["moe_w_down%d" % e_] = inp["moe_w_down"][j][e_]
        for g in range(3):
            m["pos%d" % g] = pos_layout(inp["positions"], core, g)
        maps.append(m)
    res = run_bass_kernel_spmd(nc, maps, core_ids=list(range(NCORES)))
    out = np.empty_like(x_full)
    for core in range(NCORES):
        b, half = core // 2, core % 2
        out[b, half * TOWN:(half + 1) * TOWN] = res.results[core]["x_out"]
    return out


_PROGS = {}


def kernel(**inp):
    inp = {k_: np.asarray(v) for k_, v in inp.items()}
    x = np.ascontiguousarray(inp["x"], dtype=np.float32)
    if "even" not in _PROGS:
        _PROGS["even"] = build_even()
        _PROGS["odd"] = build_odd()
    for i in range(4):
        if i % 2 == 0:
            x = run_even(_PROGS["even"], x, inp, i)
        else:
            x = run_odd(_PROGS["odd"], x, inp, i)
    return x


_MOEX_IO = [("x1", [TOWN, D]), ("xacc", [TOWN, D]), ("mod_in", [1, 6 * D]), ("esel", [128, NE]), ("ident", [128, 128]),
            ("hv", [128, 1]), ("router_w", [D, NE]), ("router_b", [1, NE]), ("w_gu", [D, 2 * DFFE]), ("w_down", [DFFE, D])]


def moe_expert_phase(k, io, x_out):
    P, C = k.P, k.C
    import os
    MODE = os.environ.get("MOEX_MODE", "full")
    C.push()
    row, row_r = C.sb([1, 3 * D], F32, "mrow")
    P.dma("sp", row[:], io["mod_in"][:, 3 * D:6 * D], row_r, writes=[row_r])
    bc = [C.sb([128, 1024], F32, "bcx") for _ in range(3)]
    for idx in range(3):
        for half in range(2):
            pt, pr = k.pb[(idx * 2 + half) % 2]
            P.op("pe", lambda e: e.matmul(pt[:], lhsT=k.ones[0:1, :], rhs=row[0:1, idx * 1024 + half * 512: idx * 1024 + half * 512 + 512],
                                          start=True, stop=True), reads=[row_r, k.ones_r], writes=[pr])
            P.op("act", lambda e: e.copy(out=bc[idx][0][:, half * 512:(half + 1) * 512], in_=pt[:]), reads=[pr], writes=[bc[idx][1]])
    sh2, gain2, g2b = bc
    xt, xt_r = C.sb([128, 8, 1024], F32, "xm")
    x1t, x1t_r = C.sb([128, 8, 1024], F32, "x1m")
    hf = [C.sb([128, 1024], F32, "hfm") for _ in range(2)]
    tmp = [C.sb([128, 1024], F32, "tmpm") for _ in range(2)]
    junk = C.sb([128, 1024], F32, "junkm")
    hT, hT_r = C.sb([128, 8, 1024], BF16, "hTm")
    hbh = [C.sb([128, 1024], BF16, "hbh") for _ in range(2)]
    hbl = [C.sb([128, 1024], BF16, "hbl") for _ in range(2)]
    hTlo = [C.sb([128, 8, 128], BF16, "hTlo") for _ in range(2)]
    ss, ss_r = C.sb([128, 8], F32, "ssm")
    rw, rw_r = C.sb([128, 8, 8], F32, "rw")
    P.dma("sp", rw[:], io["router_w"].rearrange("(j p) e -> p j e", p=128), rw_r, writes=[rw_r])
    rwh, rwh_r = C.sb([128, 8, 8], BF16, "rwh")
    rwl, rwl_r = C.sb([128, 8, 8], BF16, "rwl")
    P.op("dve", lambda e: e.tensor_copy(out=rwh[:], in_=rw[:]), reads=[rw_r], writes=[rwh_r])
    P.op("dve", lambda e: e.tensor_tensor(out=rwl[:], in0=rw[:], in1=rwh[:], op=ALU.subtract), reads=[rw_r, rwh_r], writes=[rwl_r])
    esel, esel_r = C.sb([128, 8], F32, "esel")
    P.dma("sp", esel[:], io["esel"], esel_r, writes=[esel_r])
    rbrow, rbrow_r = C.sb([1, 8], F32, "rbrow")
    P.dma("sp", rbrow[:], io["router_b"], rbrow_r, writes=[rbrow_r])
    rbb, rbb_r = C.sb([128, 8], F32, "rbb")
    pt, pr = k.pb[4]
    P.op("pe", lambda e: e.matmul(pt[:, 0:8], lhsT=k.ones[0:1, :], rhs=rbrow[0:1, :], start=True, stop=True), reads=[rbrow_r, k.ones_r], writes=[pr])
    P.op("act", lambda e: e.copy(out=rbb[:], in_=pt[:, 0:8]), reads=[pr], writes=[rbb_r])
    comb, comb_r = C.sb([128, 8, 8], F32, "comb")
    rt_ = [C.sb([128, 48], F32, "rtm") for _ in range(2)]
    alloc_expert_bufs(k, 2)
    work = [(t, pc) for t in range(4) for pc in MOE_PIECES]
    slots = {}
    slots[0] = expert_load(k, io["w_gu"], io["w_down"], 0, 512, DFFE, g2b)
    wi = 0
    for t in range(4):
        t0 = t * 1024
        if t == 0:
            P.dma("sp", x1t[:], io["x1"][t0:t0 + 1024, :].rearrange("(s p) d -> p s d", p=128), x1t_r, writes=[x1t_r])
        P.dma("sp", xt[:], io["xacc"][t0:t0 + 1024, :].rearrange("(s p) d -> p s d", p=128), xt_r, writes=[xt_r])
        for s in range(8):
            if MODE == "pro":
                break
            sumsq(k, x1t[:, s, :], x1t_r, ss, ss_r, s, junk)
        if MODE != "pro":
            rstd_from_ss(k, ss, ss_r, 8, 1.0 / D)
        for s in range(8):
            if MODE == "pro":
                break
            h = hf[rr(k, "hfm", 2)]
            mod_norm(k, x1t[:, s, :], x1t_r, ss[:, s:s + 1], ss_r, gain2, sh2, tmp[rr(k, "tmpm", 2)], h[0][:], h[1])
            if MODE == "norm":
                continue
            hh, hh_r = hbh[rr(k, "hbh", 2)]
            hl, hl_r = hbl[rr(k, "hbl", 2)]
            P.op("act", lambda e: e.copy(out=hh[:], in_=h[0][:]), reads=[h[1]], writes=[hh_r])
            P.op("dve", lambda e: e.tensor_tensor(out=hl[:], in0=h[0][:], in1=hh[:], op=ALU.subtract), reads=[h[1], hh_r], writes=[hl_r])
            transpose_bf16(k, hh, hh_r, hT, hT_r, s * 128)
            tp, tpr = k.tp[rr(k, "tp", 2)]
            P.op("pe", [lambda e, j=j: e.transpose(out=tp[:, j, :], in_=hl[:, j * 128:(j + 1) * 128], identity=k.identb[:]) for j in range(8)],
                 reads=[hl_r, k.identb_r], writes=[tpr])
            hTl, hTl_r = hTlo[rr(k, "hTlo", 2)]
            P.op("dve", lambda e: e.tensor_copy(out=hTl[:], in_=tp[:]), reads=[tpr], writes=[hTl_r])
            if MODE in ("norouter", "noexp2"):
                P.op("pool", lambda e: e.memset(comb[:, s, :], 0.25), writes=[comb_r])
                continue
            pl, plr = k.pb[4 + rr(k, "pl", 2)]
            fns = [lambda e, j=j: e.matmul(pl[:, 0:8], lhsT=hT[:, j, s * 128:(s + 1) * 128], rhs=rwh[:, j, :], start=(j == 0), stop=False) for j in range(8)]
            fns += [lambda e, j=j: e.matmul(pl[:, 0:8], lhsT=hT[:, j, s * 128:(s + 1) * 128], rhs=rwl[:, j, :], start=False, stop=False) for j in range(8)]
            fns += [lambda e, j=j: e.matmul(pl[:, 0:8], lhsT=hTl[:, j, :], rhs=rwh[:, j, :], start=False, stop=(j == 7)) for j in range(8)]
            P.op("pe", fns, reads=[hT_r, hTl_r, rwh_r, rwl_r], writes=[plr])
            r_, r_r = rt_[rr(k, "rtm", 2)]
            lg, eq1, l2, eq2, mm, cs = r_[:, 0:8], r_[:, 8:16], r_[:, 16:24], r_[:, 24:32], r_[:, 32:40], r_[:, 40:48]
            P.op("dve", lambda e: e.tensor_tensor(out=lg, in0=pl[:, 0:8], in1=rbb[:], op=ALU.add), reads=[plr, rbb_r], writes=[r_r])
            P.op("dve", lambda e: e.tensor_reduce(out=mm[:, 0:1], in_=lg, axis=AX.X, op=ALU.max), reads=[r_r], writes=[r_r])
            P.op("dve", lambda e: e.tensor_scalar(out=eq1, in0=lg, scalar1=mm[:, 0:1], scalar2=None, op0=ALU.is_equal), reads=[r_r], writes=[r_r])
            P.op("dve", lambda e: e.scalar_tensor_tensor(out=l2, in0=eq1, scalar=-1.0e30, in1=lg, op0=ALU.mult, op1=ALU.add), reads=[r_r], writes=[r_r])
            P.op("dve", lambda e: e.tensor_reduce(out=mm[:, 1:2], in_=l2, axis=AX.X, op=ALU.max), reads=[r_r], writes=[r_r])
            P.op("dve", lambda e: e.tensor_scalar(out=eq2, in0=l2, scalar1=mm[:, 1:2], scalar2=None, op0=ALU.is_equal), reads=[r_r], writes=[r_r])
            P.op("dve", lambda e: e.tensor_tensor(out=mm[:, 2:3], in0=mm[:, 1:2], in1=mm[:, 0:1], op=ALU.subtract), reads=[r_r], writes=[r_r])
            P.op("act", lambda e: e.activation(out=mm[:, 3:4], in_=mm[:, 2:3], func=AF.Sigmoid), reads=[r_r], writes=[r_r])
            P.op("act", lambda e: e.activation(out=mm[:, 4:5], in_=mm[:, 2:3], func=AF.Sigmoid, scale=-1.0), reads=[r_r], writes=[r_r])
            P.op("dve", lambda e: e.tensor_scalar(out=eq1, in0=eq1, scalar1=mm[:, 4:5], scalar2=None, op0=ALU.mult), reads=[r_r], writes=[r_r])
            P.op("dve", lambda e: e.scalar_tensor_tensor(out=cs, in0=eq2, scalar=mm[:, 3:4], in1=eq1, op0=ALU.mult, op1=ALU.add), reads=[r_r], writes=[r_r])
            P.op("dve", lambda e: e.tensor_tensor(out=cs, in0=cs, in1=esel[:], op=ALU.mult), reads=[r_r, esel_r], writes=[r_r])
            P.op("dve", lambda e: e.tensor_reduce(out=comb[:, s, 0:1], in_=cs, axis=AX.X, op=ALU.add), reads=[r_r], writes=[comb_r])
        if t + 1 < 4:
            P.dma("sp", x1t[:], io["x1"][t0 + 1024:t0 + 2048, :].rearrange("(s p) d -> p s d", p=128), x1t_r, writes=[x1t_r])
        for (f0, fp) in MOE_PIECES:
            if MODE in ("noexp", "noexp2", "pro", "norm"):
                break
            if wi + 1 < len(work):
                nf0, nfp = work[wi + 1][1]
                slots[wi + 1] = expert_load(k, io["w_gu"], io["w_down"], nf0, nfp, DFFE, g2b)
            expert_compute(k, slots[wi], fp, hT, hT_r, xt, xt_r, (comb, comb_r, 0))
            wi += 1
        P.dma("sp", x_out[t0:t0 + 1024, :].rearrange("(s p) d -> p s d", p=128), xt[:], xt_r, reads=[xt_r])
    P.barrier()
    C.pop()


def build_moex():
    nc = bass.Bass("TRN2", target_bir_lowering=False)
    io = {n: _dram_in(nc, n, s) for n, s in _MOEX_IO}
    x_out = nc.dram_tensor("x_out", [TOWN, D], F32, kind="ExternalOutput").ap()
    k = new_k(nc)
    k.C.push()
    setup_common(k, io)
    moe_expert_phase(k, io, x_out)
    k.P.barrier()
    k.C.pop()
    return nc


def build_attn(phases="abc"):
    nc = bass.Bass("TRN2", target_bir_lowering=False)
    io = {n: _dram_in(nc, n, s) for n, s in _COMMON_IO + _ODD_IO[:6] + [("mask2", [128, 256]), ("invf", [128, 16])]}
    for g in range(3):
        io["pos%d" % g] = _dram_in(nc, "pos%d" % g, [128, grp(g)[3]], I32)
    x_out = nc.dram_tensor("x_out", [TOWN, D], F32, kind="ExternalOutput").ap()
    io["mod_out"] = nc.dram_tensor("mod_out", [1, 6 * D], F32, kind="ExternalOutput").ap()
    scr = alloc_scratch(nc)
    k = new_k(nc)
    k.C.push()
    setup_common(k, io)
    odd_layer(k, io, io["x_own"], x_out, scr, phases)
    k.P.barrier()
    k.C.pop()
    return nc


def _gather(res, name="x_out"):
    out = np.empty((4, SEQ, D), np.float32)
    for core in range(NCORES):
        b, half = core // 2, core % 2
        out[b, half * TOWN:(half + 1) * TOWN] = res.results[core][name]
    return out


def run_attn(nc, x_full, inp, i):
    j = i // 2
    ac = attn_consts()
    maps = []
    for core in range(NCORES):
        m = core_common_inputs(x_full, inp["c"], core)
        m.update(ac)
        m.update({
            "ada_w": inp["ada_w"][i], "ada_b": inp["ada_b"][i][None, :], "norm1_g": inp["norm1_g"][i][None, :],
            "norm2_g": inp["norm2_g"][i][None, :],
            "attn_w_qkv": inp["attn_w_qkv"][j], "attn_w_o": inp["attn_w_o"][j], "q_norm_g": inp["q_norm_g"][j],
            "k_norm_g": inp["k_norm_g"][j], "router_w": inp["router_w"][j], "router_b": inp["router_b"][j][None, :],
        })
        for g in range(3):
            m["pos%d" % g] = pos_layout(inp["positions"], core, g)
        maps.append(m)
    res = run_bass_kernel_spmd(nc, maps, core_ids=list(range(NCORES)))
    return _gather(res), [res.results[c]["mod_out"] for c in range(NCORES)]


def run_moex(nc, x1_full, xacc_full, mods, inp, i, e_):
    j = i // 2
    esel = np.zeros((128, NE), np.float32)
    esel[:, e_] = 1.0
    maps = []
    for core in range(NCORES):
        b, half = core // 2, core % 2
        maps.append({
            "x1": np.ascontiguousarray(x1_full[b, half * TOWN:(half + 1) * TOWN]),
            "xacc": np.ascontiguousarray(xacc_full[b, half * TOWN:(half + 1) * TOWN]),
            "mod_in": mods[core], "esel": esel, "ident": np.eye(128, dtype=np.float32),
            "hv": np.full((128, 1), float(half), np.float32),
            "router_w": inp["router_w"][j], "router_b": inp["router_b"][j][None, :],
            "w_gu": inp["moe_w_gu"][j][e_], "w_down": inp["moe_w_down"][j][e_],
        })
    res = run_bass_kernel_spmd(nc, maps, core_ids=list(range(NCORES)))
    return _gather(res)


def kernel(**inp):
    inp = {k_: np.asarray(v) for k_, v in inp.items()}
    x = np.ascontiguousarray(inp["x"], dtype=np.float32)
    if "even" not in _PROGS:
        _PROGS["even"] = build_even()
        _PROGS["attn"] = build_attn()
        _PROGS["moex"] = build_moex()
    for i in range(4):
        if i % 2 == 0:
            x = run_even(_PROGS["even"], x, inp, i)
        else:
            x1, mods = run_attn(_PROGS["attn"], x, inp, i)
            acc = x1
            for e_ in range(NE):
                acc = run_moex(_PROGS["moex"], x1, acc, mods, inp, i, e_)
            x = acc
    return x
```

```python
import contextlib
import numpy as np
import ml_dtypes
import concourse.bass as bass
import concourse.mybir as mybir
from concourse.bass_utils import run_bass_kernel_spmd

F32 = mybir.dt.float32
BF16 = mybir.dt.bfloat16
I32 = mybir.dt.int32
AF = mybir.ActivationFunctionType
ALU = mybir.AluOpType
AX = mybir.AxisListType

D = 1024
NCORES = 8
TOWN = 4096
THALO = 2048
SEQ = 8192
DFF = 2816
DFFE = 3584
NE = 8
EPS = 1e-6
DIL = (1, 4, 16)


class Res:
    __slots__ = ("name", "w", "rd", "dsem", "dcnt")

    def __init__(self, name):
        self.name = name
        self.w = None
        self.rd = {}
        self.dsem = None
        self.dcnt = 0


class _Eng:
    def __init__(self, key, sem):
        self.key = key
        self.sem = sem
        self.n = 0
        self.ops = []
        self.seen = {}


class Prog:
    def __init__(self, nc):
        self.nc = nc
        self.E = {}
        for k in ("pe", "act", "dve", "pool", "sp"):
            self.E[k] = _Eng(k, nc.alloc_semaphore(name="s_" + k))
        self.obj = {"pe": nc.tensor, "act": nc.scalar, "dve": nc.vector, "pool": nc.gpsimd, "sp": nc.sync}
        self.semname = {}
        self.dma_res = []

    def _deps(self, ek, reads, writes):
        need = {}

        def add(ev, kind):
            if ev is None:
                return
            s, v = ev
            own = s is self.E[ek].sem
            if own and (ek in ("pe", "sp") or kind == "war"):
                return
            key = id(s)
            self.semname[key] = s
            if need.get(key, 0) < v:
                need[key] = v

        for r in reads:
            add(r.w, "raw")
        for r in writes:
            add(r.w, "waw")
            for ev in r.rd.values():
                add(ev, "war")
        eng = self.E[ek]
        out = []
        for key, v in need.items():
            if eng.seen.get(key, 0) < v:
                eng.seen[key] = v
                out.append((self.semname[key], v))
        return out

    def op(self, ek, fn, reads=(), writes=()):
        eng = self.E[ek]
        e = self.obj[ek]
        waits = self._deps(ek, reads, writes)
        fns = fn if isinstance(fn, (list, tuple)) else [fn]
        for s, v in waits:
            e.wait_ge(s, v)
        for f in fns[:-1]:
            f(e)
        fns[-1](e).then_inc(eng.sem, 1)
        eng.n += 1
        ev = (eng.sem, eng.n)
        for r in reads:
            r.rd[ek] = ev
        for r in writes:
            r.w = ev
            r.rd = {}
        return ev

    def dma(self, qk, out, in_, sres, reads=(), writes=()):
        e = self.obj[qk]
        waits = self._deps(qk, reads, writes)
        if sres.dsem is None:
            sres.dsem = self.nc.alloc_semaphore(name="d_" + sres.name)
            self.dma_res.append(sres)
        sres.dcnt += 1
        sem = sres.dsem
        ev = (sem, 16 * sres.dcnt)
        for s, v in waits:
            e.wait_ge(s, v)
        e.dma_start(out=out, in_=in_).then_inc(sem, 16)
        for r in reads:
            r.rd[("d", id(sres))] = ev
        for r in writes:
            r.w = ev
            r.rd = {}
        return ev

    def barrier(self):
        evs = [(e.sem, e.n) for e in self.E.values() if e.n > 0]
        evs += [(r.dsem, 16 * r.dcnt) for r in self.dma_res]
        for ek, eng in self.E.items():
            for s, v in evs:
                if s is eng.sem:
                    continue
                if eng.seen.get(id(s), 0) < v:
                    eng.seen[id(s)] = v
                    self.obj[ek].wait_ge(s, v)


class Ctx:
    def __init__(self, nc, prog):
        self.nc = nc
        self.P = prog
        self.n = 0
        self.stacks = []

    def push(self):
        st = contextlib.ExitStack()
        st.__enter__()
        self.stacks.append(st)

    def pop(self):
        self.stacks.pop().__exit__(None, None, None)

    def sb(self, shape, dt, name=None):
        self.n += 1
        name = (name or "t") + "_%d" % self.n
        t = self.stacks[-1].enter_context(self.nc.sbuf_tensor(name, list(shape), dt))
        return t, Res(name)

    def ps(self, shape, dt, name=None):
        self.n += 1
        name = (name or "p") + "_%d" % self.n
        t = self.stacks[-1].enter_context(self.nc.psum_tensor(name, list(shape), dt))
        return t, Res(name)


def _dram_in(nc, name, shape, dt=F32):
    return nc.dram_tensor(name, list(shape), dt, kind="ExternalInput").ap()


class K:
    pass


def setup_common(k, io):
    P, C = k.P, k.C
    k.ident, k.ident_r = C.sb([128, 128], F32, "ident")
    P.dma("sp", k.ident[:], io["ident"], k.ident_r, writes=[k.ident_r])
    k.identb, k.identb_r = C.sb([128, 128], BF16, "identb")
    P.op("dve", lambda e: e.tensor_copy(out=k.identb[:], in_=k.ident[:]), reads=[k.ident_r], writes=[k.identb_r])
    k.ones, k.ones_r = C.sb([128, 128], F32, "ones")
    P.op("pool", lambda e: e.memset(k.ones[:], 1.0), writes=[k.ones_r])
    k.onesb, k.onesb_r = C.sb([128, 128], BF16, "onesb")
    P.op("pool", lambda e: e.memset(k.onesb[:], 1.0), writes=[k.onesb_r])
    k.hv, k.hv_r = C.sb([128, 1], F32, "hv")
    P.dma("sp", k.hv[:], io["hv"], k.hv_r, writes=[k.hv_r])
    k.tp = [C.ps([128, 8, 128], BF16, "tp") for _ in range(2)]
    k.pb = [C.ps([128, 512], F32, "pb") for _ in range(6)]
    k.cnt = {}


def rr(k, name, n):
    v = k.cnt.get(name, 0)
    k.cnt[name] = v + 1
    return v % n


def ada_phase(k, io, even):
    P, C = k.P, k.C
    bc = [C.sb([128, 1024], F32, "bc") for _ in range(6)]
    C.push()
    c_sb, c_r = C.sb([128, 8], F32)
    P.dma("sp", c_sb[:], io["c_col"], c_r, writes=[c_r])
    cact, cact_r = C.sb([128, 8], F32)
    P.op("act", lambda e: e.activation(out=cact[:], in_=c_sb[:], func=AF.Silu), reads=[c_r], writes=[cact_r])
    row, row_r = C.sb([1, 6144], F32)
    brow, brow_r = C.sb([1, 6144], F32)
    P.dma("sp", brow[:], io["ada_b"], brow_r, writes=[brow_r])
    grow, grow_r = C.sb([1, 3072], F32)
    P.dma("sp", grow[:, 0:1024], io["norm1_g"], grow_r, writes=[grow_r])
    P.dma("sp", grow[:, 1024:2048], io["norm2_g"], grow_r, writes=[grow_r])
    if even:
        P.dma("sp", grow[:, 2048:3072], io["pool_scale"], grow_r, writes=[grow_r])
    wbuf = [C.sb([128, 8, 512], F32) for _ in range(2)]
    for cb in range(12):
        wt, wr = wbuf[cb % 2]
        P.dma("sp", wt[:], io["ada_w"][:, cb * 512:(cb + 1) * 512].rearrange("(j p) n -> p j n", p=128), wr, writes=[wr])
        pt, pr = k.pb[cb % 2]
        P.op("pe", [lambda e, j=j: e.matmul(pt[0:1, :], lhsT=cact[:, j:j + 1], rhs=wt[:, j, :], start=(j == 0), stop=(j == 7))
                    for j in range(8)], reads=[cact_r, wr], writes=[pr])
        P.op("dve", lambda e: e.tensor_tensor(out=row[:, cb * 512:(cb + 1) * 512], in0=pt[0:1, :],
                                              in1=brow[:, cb * 512:(cb + 1) * 512], op=ALU.add),
             reads=[pr, brow_r], writes=[row_r])
    P.op("dve", lambda e: e.scalar_tensor_tensor(out=row[:, 1024:2048], in0=row[:, 1024:2048], scalar=1.0,
                                                 in1=grow[:, 0:1024], op0=ALU.add, op1=ALU.mult),
         reads=[row_r, grow_r], writes=[row_r])
    P.op("dve", lambda e: e.scalar_tensor_tensor(out=row[:, 4096:5120], in0=row[:, 4096:5120], scalar=1.0,
                                                 in1=grow[:, 1024:2048], op0=ALU.add, op1=ALU.mult),
         reads=[row_r, grow_r], writes=[row_r])
    if even:
        P.op("dve", lambda e: e.tensor_tensor(out=row[:, 2048:3072], in0=row[:, 2048:3072], in1=grow[:, 2048:3072],
                                              op=ALU.mult), reads=[row_r, grow_r], writes=[row_r])
    if io.get("mod_out") is not None:
        P.dma("sp", io["mod_out"], row[:], row_r, reads=[row_r])
    for idx in range(6):
        for half in range(2):
            pt, pr = k.pb[(idx * 2 + half) % 2]
            P.op("pe", lambda e: e.matmul(pt[:], lhsT=k.ones[0:1, :], rhs=row[0:1, idx * 1024 + half * 512: idx * 1024 + half * 512 + 512],
                                          start=True, stop=True), reads=[row_r, k.ones_r], writes=[pr])
            P.op("act", lambda e: e.copy(out=bc[idx][0][:, half * 512:(half + 1) * 512], in_=pt[:]), reads=[pr], writes=[bc[idx][1]])
    P.barrier()
    C.pop()
    return bc


def sumsq(k, x_ap, x_r, ss, ss_r, col, junk):
    jt, jr = junk
    k.P.op("act", lambda e: e.activation(out=jt[:], in_=x_ap, func=AF.Square, accum_out=ss[:, col:col + 1]),
           reads=[x_r], writes=[jr, ss_r])


def rstd_from_ss(k, ss, ss_r, n, inv_n):
    P = k.P
    P.op("dve", lambda e: e.tensor_scalar(out=ss[:, 0:n], in0=ss[:, 0:n], scalar1=inv_n, scalar2=EPS, op0=ALU.mult, op1=ALU.add),
         reads=[ss_r], writes=[ss_r])
    P.op("act", lambda e: e.activation(out=ss[:, 0:n], in_=ss[:, 0:n], func=AF.Sqrt), reads=[ss_r], writes=[ss_r])
    P.op("dve", lambda e: e.reciprocal(out=ss[:, 0:n], in_=ss[:, 0:n]), reads=[ss_r], writes=[ss_r])


def mod_norm(k, x_ap, x_r, rs_ap, rs_r, gain, shift, tmp, out_ap, out_r):
    P = k.P
    tt, tr = tmp
    P.op("dve", lambda e: e.scalar_tensor_tensor(out=tt[:], in0=x_ap, scalar=rs_ap, in1=gain[0][:], op0=ALU.mult, op1=ALU.mult),
         reads=[x_r, rs_r, gain[1]], writes=[tr])
    P.op("pool", lambda e: e.tensor_tensor(out=out_ap, in0=tt[:], in1=shift[0][:], op=ALU.add), reads=[tr, shift[1]], writes=[out_r])


def transpose_bf16(k, hb, hb_r, hT, hT_r, c0):
    P = k.P
    tp, tpr = k.tp[rr(k, "tp", 2)]
    P.op("pe", [lambda e, j=j: e.transpose(out=tp[:, j, :], in_=hb[:, j * 128:(j + 1) * 128], identity=k.identb[:]) for j in range(8)],
         reads=[hb_r, k.identb_r], writes=[tpr])
    P.op("act", lambda e: e.copy(out=hT[:, :, c0:c0 + 128], in_=tp[:]), reads=[tpr], writes=[hT_r])


def alloc_expert_bufs(k, nset=2):
    C = k.C
    k.wg = [C.sb([128, 8, 512], BF16, "wg") for _ in range(nset)]
    k.wu = [C.sb([128, 8, 512], BF16, "wu") for _ in range(nset)]
    k.wd = [C.sb([128, 4, 1024], BF16, "wd") for _ in range(nset)]
    k.aT = [C.sb([128, 4, 1024], BF16, "aT") for _ in range(2)]
    k.sg = [C.sb([128, 512], F32, "sg") for _ in range(2)]
    k.nset = nset


def expert_load(k, gu_src, d_src, f0, fp, ff, g2b):
    P = k.P
    s = rr(k, "wset", k.nset)
    nf = fp // 128
    wg, wgr = k.wg[s]
    wu, wur = k.wu[s]
    wd, wdr = k.wd[s]
    P.dma("pool", wg[:, :, 0:fp], gu_src[:, f0:f0 + fp].rearrange("(j p) n -> p j n", p=128), wgr, writes=[wgr])
    P.dma("pool", wu[:, :, 0:fp], gu_src[:, ff + f0:ff + f0 + fp].rearrange("(j p) n -> p j n", p=128), wur, writes=[wur])
    P.dma("pool", wd[:, 0:nf, :], d_src[f0:f0 + fp, :].rearrange("(j p) n -> p j n", p=128), wdr, writes=[wdr])
    P.op("pool", lambda e: e.tensor_tensor(out=wd[:, 0:nf, :], in0=wd[:, 0:nf, :],
                                           in1=g2b[0][:].unsqueeze(1).broadcast_to([128, nf, 1024]), op=ALU.mult),
         reads=[wdr, g2b[1]], writes=[wdr])
    return s


def expert_compute(k, s, fp, hT, hT_r, xt, xt_r, comb):
    P = k.P
    nf = fp // 128
    wg, wgr = k.wg[s]
    wu, wur = k.wu[s]
    wd, wdr = k.wd[s]
    aT, aTr = k.aT[rr(k, "aT", 2)]
    for fc in range(nf):
        for half in range(2):
            i = rr(k, "gu", 2)
            pg, pgr = k.pb[i]
            pu, pur = k.pb[2 + i]
            sg, sgr = k.sg[i]
            P.op("pe", [lambda e, j=j: e.matmul(pg[:], lhsT=wg[:, j, fc * 128:(fc + 1) * 128], rhs=hT[:, j, half * 512:(half + 1) * 512],
                                                start=(j == 0), stop=(j == 7)) for j in range(8)],
                 reads=[wgr, hT_r], writes=[pgr])
            P.op("pe", [lambda e, j=j: e.matmul(pu[:], lhsT=wu[:, j, fc * 128:(fc + 1) * 128], rhs=hT[:, j, half * 512:(half + 1) * 512],
                                                start=(j == 0), stop=(j == 7)) for j in range(8)],
                 reads=[wur, hT_r], writes=[pur])
            P.op("act", lambda e: e.activation(out=sg[:], in_=pg[:], func=AF.Silu), reads=[pgr], writes=[sgr])
            P.op("dve", lambda e: e.tensor_tensor(out=aT[:, fc, half * 512:(half + 1) * 512], in0=sg[:], in1=pu[:], op=ALU.mult),
                 reads=[sgr, pur], writes=[aTr])
    for sub in range(8):
        for dh in range(2):
            pd, pdr = k.pb[4 + rr(k, "pd", 2)]
            P.op("pe", [lambda e, fc=fc: e.matmul(pd[:], lhsT=aT[:, fc, sub * 128:(sub + 1) * 128], rhs=wd[:, fc, dh * 512:(dh + 1) * 512],
                                                  start=(fc == 0), stop=(fc == nf - 1)) for fc in range(nf)],
                 reads=[aTr, wdr], writes=[pdr])
            xs = xt[:, sub, dh * 512:(dh + 1) * 512]
            if comb is None:
                P.op("dve", lambda e: e.tensor_tensor(out=xs, in0=pd[:], in1=xs, op=ALU.add), reads=[pdr, xt_r], writes=[xt_r])
            else:
                ct, cr, ei = comb
                P.op("dve", lambda e: e.scalar_tensor_tensor(out=xs, in0=pd[:], scalar=ct[:, sub, ei:ei + 1], in1=xs,
                                                             op0=ALU.mult, op1=ALU.add), reads=[pdr, xt_r, cr], writes=[xt_r])


FFN_PIECES = [(0, 512), (512, 512), (1024, 512), (1536, 512), (2048, 512), (2560, 256)]
MOE_PIECES = [(i * 512, 512) for i in range(7)]


def even_layer(k, io, x_in, x_halo, x_out):
    P, C = k.P, k.C
    C.push()
    bc = ada_phase(k, io, True)
    sh1, gain1, gs1, sh2, gain2, g2b = bc
    poolA, poolA_r = C.sb([128, 12, 128], F32, "poolA")
    P.dma("sp", poolA[:], io["poolA"], poolA_r, writes=[poolA_r])
    wp, wp_r = C.sb([128, 4, 2, 256], BF16, "wpool")
    P.dma("pool", wp[:], io["pool_w"].rearrange("g (c p) n -> p g c n", p=128), wp_r, writes=[wp_r])
    xt, xt_r = C.sb([128, 8, 1024], F32, "xt")
    hf = [C.sb([128, 1024], F32, "hf") for _ in range(3)]
    tmp = [C.sb([128, 1024], F32, "tmp") for _ in range(2)]
    junk = C.sb([128, 1024], F32, "junk")
    pT = [C.sb([128, 8, 128], BF16, "pT") for _ in range(2)]
    hb = [C.sb([128, 1024], BF16, "hb") for _ in range(2)]
    hT, hT_r = C.sb([128, 8, 1024], BF16, "hT")
    ss, ss_r = C.sb([128, 8], F32, "ss")
    alloc_expert_bufs(k, 2)

    xh, xh_r = tmp[0]
    P.dma("sp", xh[:], x_halo[THALO - 128:THALO, :], xh_r, writes=[xh_r])
    sumsq(k, xh[:], xh_r, ss, ss_r, 0, junk)
    rstd_from_ss(k, ss, ss_r, 1, 1.0 / D)
    hprev = hf[rr(k, "hf", 3)]
    mod_norm(k, xh[:], xh_r, ss[:, 0:1], ss_r, gain1, sh1, tmp[1], hprev[0][:], hprev[1])
    P.op("dve", lambda e: e.tensor_scalar(out=hprev[0][:], in0=hprev[0][:], scalar1=k.hv[:, 0:1], scalar2=None, op0=ALU.mult),
         reads=[hprev[1], k.hv_r], writes=[hprev[1]])

    work = [(t, pc) for t in range(4) for pc in FFN_PIECES]
    slots = {}
    slots[0] = expert_load(k, io["ffn_w_gu"], io["ffn_w_down"], work[0][1][0], work[0][1][1], DFF, g2b)
    wi = 0
    for t in range(4):
        t0 = t * 1024
        P.dma("sp", xt[:], x_in[t0:t0 + 1024, :].rearrange("(s p) d -> p s d", p=128), xt_r, writes=[xt_r])
        for s in range(8):
            sumsq(k, xt[:, s, :], xt_r, ss, ss_r, s, junk)
        rstd_from_ss(k, ss, ss_r, 8, 1.0 / D)
        for s in range(8):
            hcur = hf[rr(k, "hf", 3)]
            mod_norm(k, xt[:, s, :], xt_r, ss[:, s:s + 1], ss_r, gain1, sh1, tmp[rr(k, "tmp", 2)], hcur[0][:], hcur[1])
            first = (t == 0 and s == 0)
            pp0, pp0r = k.pb[0]
            pp1, pp1r = k.pb[1]
            fns = []
            for c in range(8):
                g = c // 2
                pp = pp0 if c < 4 else pp1
                o = pp[:].rearrange("p (c t) -> p c t", c=4)[:, c % 4, :]
                fns.append(lambda e, c=c, g=g, o=o: e.matmul(o, lhsT=hprev[0][:, c * 128:(c + 1) * 128], rhs=poolA[:, 4 + g, :],
                                                            start=True, stop=False))
                fns.append(lambda e, c=c, g=g, o=o: e.matmul(o, lhsT=hcur[0][:, c * 128:(c + 1) * 128],
                                                            rhs=poolA[:, (8 if first else 0) + g, :], start=False, stop=True))
            P.op("pe", fns, reads=[hprev[1], hcur[1], poolA_r], writes=[pp0r, pp1r])
            pt, ptr = pT[rr(k, "pT", 2)]
            P.op("act", lambda e: e.copy(out=pt[:, 0:4, :], in_=pp0[:].rearrange("p (c t) -> p c t", c=4)), reads=[pp0r], writes=[ptr])
            P.op("act", lambda e: e.copy(out=pt[:, 4:8, :], in_=pp1[:].rearrange("p (c t) -> p c t", c=4)), reads=[pp1r], writes=[ptr])
            py0, py0r = k.pb[4]
            py1, py1r = k.pb[5]
            fns = []
            for g in range(4):
                py = py0 if g < 2 else py1
                for cc in range(2):
                    fns.append(lambda e, g=g, cc=cc, py=py: e.matmul(py[:, (g % 2) * 256:(g % 2) * 256 + 256], lhsT=pt[:, 2 * g + cc, :],
                                                                   rhs=wp[:, g, cc, :], start=(cc == 0), stop=(cc == 1)))
            P.op("pe", fns, reads=[ptr, wp_r], writes=[py0r, py1r])
            for hh, (py, pyr) in enumerate(((py0, py0r), (py1, py1r))):
                tq, tqr = tmp[rr(k, "tmp", 2)]
                P.op("dve", lambda e: e.tensor_tensor(out=tq[:, 0:512], in0=py[:], in1=gs1[0][:, hh * 512:(hh + 1) * 512], op=ALU.mult),
                     reads=[pyr, gs1[1]], writes=[tqr])
                P.op("pool", lambda e: e.tensor_tensor(out=xt[:, s, hh * 512:(hh + 1) * 512], in0=tq[:, 0:512],
                                                       in1=xt[:, s, hh * 512:(hh + 1) * 512], op=ALU.add), reads=[tqr, xt_r], writes=[xt_r])
            hprev = hcur
        for s in range(8):
            sumsq(k, xt[:, s, :], xt_r, ss, ss_r, s, junk)
        rstd_from_ss(k, ss, ss_r, 8, 1.0 / D)
        for s in range(8):
            h2 = hb[rr(k, "hb", 2)]
            mod_norm(k, xt[:, s, :], xt_r, ss[:, s:s + 1], ss_r, gain2, sh2, tmp[rr(k, "tmp", 2)], h2[0][:], h2[1])
            transpose_bf16(k, h2[0], h2[1], hT, hT_r, s * 128)
        for (f0, fp) in FFN_PIECES:
            if wi + 1 < len(work):
                nf0, nfp = work[wi + 1][1]
                slots[wi + 1] = expert_load(k, io["ffn_w_gu"], io["ffn_w_down"], nf0, nfp, DFF, g2b)
            expert_compute(k, slots[wi], fp, hT, hT_r, xt, xt_r, None)
            wi += 1
        P.dma("sp", x_out[t0:t0 + 1024, :].rearrange("(s p) d -> p s d", p=128), xt[:], xt_r, reads=[xt_r])
    P.barrier()
    C.pop()


def make_poolA(first_special):
    A = np.zeros((128, 12, 128), np.float32)
    tp = np.arange(128)[:, None]
    t = np.arange(128)[None, :]
    eye = (tp == t).astype(np.float32)
    for g, w in enumerate((2, 4, 8, 16)):
        cur = ((t - tp >= 0) & (t - tp < w)).astype(np.float32)
        prv = ((t - (tp - 128) >= 0) & (t - (tp - 128) < w)).astype(np.float32)
        A[:, g, :] = cur / w - eye
        A[:, 4 + g, :] = prv / w
        cnt = np.minimum(t + 1, w).astype(np.float32)
        A[:, 8 + g, :] = (cur / cnt - eye) if first_special else A[:, g, :]
    return A


_COMMON_IO = [("x_own", [TOWN, D]), ("x_halo", [THALO, D]), ("c_col", [128, 8]), ("hv", [128, 1]), ("ident", [128, 128]),
              ("ada_w", [D, 6 * D]), ("ada_b", [1, 6 * D]), ("norm1_g", [1, D]), ("norm2_g", [1, D])]
_EVEN_IO = [("poolA", [128, 12, 128]), ("pool_scale", [1, D]), ("pool_w", [4, 256, 256]),
            ("ffn_w_gu", [D, 2 * DFF]), ("ffn_w_down", [DFF, D])]


def new_k(nc):
    k = K()
    k.nc = nc
    k.P = Prog(nc)
    k.C = Ctx(nc, k.P)
    return k


def build_even():
    nc = bass.Bass("TRN2", target_bir_lowering=False)
    io = {n: _dram_in(nc, n, s) for n, s in _COMMON_IO + _EVEN_IO}
    x_out = nc.dram_tensor("x_out", [TOWN, D], F32, kind="ExternalOutput").ap()
    k = new_k(nc)
    k.C.push()
    setup_common(k, io)
    even_layer(k, io, io["x_own"], io["x_halo"], x_out)
    k.P.barrier()
    k.C.pop()
    return nc


def core_common_inputs(x_full, c, core):
    b, half = core // 2, core % 2
    s0 = half * TOWN
    x_own = np.ascontiguousarray(x_full[b, s0:s0 + TOWN])
    if half == 0:
        x_halo = np.zeros((THALO, D), np.float32)
    else:
        x_halo = np.ascontiguousarray(x_full[b, s0 - THALO:s0])
    return {
        "x_own": x_own, "x_halo": x_halo,
        "c_col": np.ascontiguousarray(c[b].reshape(8, 128).T),
        "hv": np.full((128, 1), float(half), np.float32),
        "ident": np.eye(128, dtype=np.float32),
    }


def run_even(nc, x_full, inp, i):
    j = i // 2
    maps = []
    for core in range(NCORES):
        m = core_common_inputs(x_full, inp["c"], core)
        m.update({
            "ada_w": inp["ada_w"][i], "ada_b": inp["ada_b"][i][None, :], "norm1_g": inp["norm1_g"][i][None, :],
            "norm2_g": inp["norm2_g"][i][None, :], "poolA": make_poolA(core % 2 == 0),
            "pool_scale": inp["pool_scale"][j][None, :], "pool_w": inp["pool_w"][j],
            "ffn_w_gu": inp["ffn_w_gu"][j], "ffn_w_down": inp["ffn_w_down"][j],
        })
        maps.append(m)
    res = run_bass_kernel_spmd(nc, maps, core_ids=list(range(NCORES)))
    out = np.empty_like(x_full)
    for core in range(NCORES):
        b, half = core // 2, core % 2
        out[b, half * TOWN:(half + 1) * TOWN] = res.results[core]["x_out"]
    return out


def grp(g):
    d = DIL[g]
    halo = 128 * d
    T = halo + TOWN
    return d, halo, T, T // 128, (T // d) // 128


def attn_phase_a(k, io, g, bc, scr):
    P, C = k.P, k.C
    sh1, gain1 = bc[0], bc[1]
    d, halo, T, nblk, nbr = grp(g)
    C.push()
    hT, hT_r = C.sb([128, 8, T], BF16, "hTg")
    C.push()
    xt, xt_r = C.sb([128, 4, 1024], F32, "xa")
    tmp = [C.sb([128, 1024], F32, "tmpa") for _ in range(2)]
    hb = [C.sb([128, 1024], BF16, "hba") for _ in range(2)]
    junk = C.sb([128, 1024], F32, "junka")
    ss, ss_r = C.sb([128, 8], F32, "ssa")
    for t in range((T + 511) // 512):
        u0 = t * 512
        ns_ = min(4, (T - u0) // 128)
        for s in range(ns_):
            u = u0 + s * 128
            if u < halo:
                src = io["x_halo"][THALO - halo + u: THALO - halo + u + 128, :]
            else:
                src = k.x_in[u - halo: u - halo + 128, :]
            P.dma("sp", xt[:, s, :], src, xt_r, writes=[xt_r])
        for s in range(ns_):
            sumsq(k, xt[:, s, :], xt_r, ss, ss_r, s, junk)
        rstd_from_ss(k, ss, ss_r, 4, 1.0 / D)
        for s in range(ns_):
            h = hb[rr(k, "hba", 2)]
            mod_norm(k, xt[:, s, :], xt_r, ss[:, s:s + 1], ss_r, gain1, sh1, tmp[rr(k, "tmpa", 2)], h[0][:], h[1])
            tp, tpr = k.tp[rr(k, "tp", 2)]
            P.op("pe", [lambda e, j=j: e.transpose(out=tp[:, j, :], in_=h[0][:, j * 128:(j + 1) * 128], identity=k.identb[:])
                        for j in range(8)], reads=[h[1], k.identb_r], writes=[tpr])
            us = u0 + s * 128
            if d == 1:
                P.op("act", lambda e: e.copy(out=hT[:, :, us:us + 128], in_=tp[:]), reads=[tpr], writes=[hT_r])
            else:
                o = hT[:].rearrange("p k (r j) -> p k j r", r=d)[:, :, us // d: us // d + 128 // d, :]
                i_ = tp[:].rearrange("p k (j r) -> p k j r", r=d)
                for j4 in range(0, 8, 4):
                    P.op("act", lambda e: e.copy(out=o[:, j4:j4 + 4], in_=i_[:, j4:j4 + 4]), reads=[tpr], writes=[hT_r])
    P.barrier()
    C.pop()
    posi, posi_r = C.sb([128, nblk], I32, "posi")
    P.dma("sp", posi[:], io["pos%d" % g], posi_r, writes=[posi_r])
    posf, posf_r = C.sb([128, nblk], F32, "posf")
    P.op("dve", lambda e: e.tensor_copy(out=posf[:], in_=posi[:]), reads=[posi_r], writes=[posf_r])
    invf, invf_r = C.sb([128, 16], F32, "invf")
    P.dma("sp", invf[:], io["invf"], invf_r, writes=[invf_r])
    pi_t, pi_r = C.sb([128, 1], F32, "pi")
    P.op("pool", lambda e: e.memset(pi_t[:], float(np.pi)), writes=[pi_r])
    cosT, cos_r = C.sb([128, nblk, 16], F32, "cosT")
    sinT, sin_r = C.sb([128, nblk, 16], F32, "sinT")
    C.push()
    ang, ang_r = C.sb([128, nblk, 16], F32, "ang")
    P.op("dve", lambda e: e.tensor_tensor(out=ang[:], in0=posf[:].unsqueeze(2).broadcast_to([128, nblk, 16]),
                                          in1=invf[:].unsqueeze(1).broadcast_to([128, nblk, 16]), op=ALU.mult),
         reads=[posf_r, invf_r], writes=[ang_r])
    TWO_PI = float(2.0 * np.pi)
    ki, ki_r = C.sb([128, nblk, 16], I32, "ki")
    kf, kf_r = C.sb([128, nblk, 16], F32, "kf")

    def sin_of(dst, dst_r, shift):
        P.op("dve", lambda e: e.tensor_scalar(out=dst[:], in0=ang[:], scalar1=shift, scalar2=None, op0=ALU.add), reads=[ang_r], writes=[dst_r])
        P.op("dve", lambda e: e.tensor_scalar(out=kf[:], in0=dst[:], scalar1=1.0 / TWO_PI, scalar2=None, op0=ALU.mult), reads=[dst_r], writes=[kf_r])
        P.op("dve", lambda e: e.tensor_copy(out=ki[:], in_=kf[:]), reads=[kf_r], writes=[ki_r])
        P.op("dve", lambda e: e.tensor_copy(out=kf[:], in_=ki[:]), reads=[ki_r], writes=[kf_r])
        P.op("dve", lambda e: e.scalar_tensor_tensor(out=dst[:], in0=kf[:], scalar=-TWO_PI, in1=dst[:], op0=ALU.mult, op1=ALU.add),
             reads=[kf_r, dst_r], writes=[dst_r])
        P.op("dve", lambda e: e.tensor_scalar(out=kf[:], in0=dst[:], scalar1=float(np.pi), scalar2=-TWO_PI, op0=ALU.is_gt, op1=ALU.mult),
             reads=[dst_r], writes=[kf_r])
        P.op("dve", lambda e: e.tensor_tensor(out=dst[:], in0=dst[:], in1=kf[:], op=ALU.add), reads=[dst_r, kf_r], writes=[dst_r])
        P.op("dve", lambda e: e.tensor_scalar(out=kf[:], in0=dst[:], scalar1=float(-np.pi), scalar2=TWO_PI, op0=ALU.is_lt, op1=ALU.mult),
             reads=[dst_r], writes=[kf_r])
        P.op("dve", lambda e: e.tensor_tensor(out=dst[:], in0=dst[:], in1=kf[:], op=ALU.add), reads=[dst_r, kf_r], writes=[dst_r])
        P.op("dve", lambda e: e.tensor_scalar(out=dst[:], in0=dst[:], scalar1=3.1415925, scalar2=-3.1415925, op0=ALU.min, op1=ALU.max),
             reads=[dst_r], writes=[dst_r])
        P.op("act", lambda e: e.activation(out=dst[:], in_=dst[:], func=AF.Sin), reads=[dst_r], writes=[dst_r])

    sin_of(sinT, sin_r, 0.0)
    sin_of(cosT, cos_r, float(np.pi / 2))
    P.barrier()
    C.pop()
    grow, grow_r = C.sb([1, 256], F32, "qkg")
    P.dma("sp", grow[:, 0:128], io["q_norm_g"][g:g + 1, :], grow_r, writes=[grow_r])
    P.dma("sp", grow[:, 128:256], io["k_norm_g"][g:g + 1, :], grow_r, writes=[grow_r])
    gb, gb_r = C.sb([128, 2, 128], F32, "qkgb")
    pt, pr = k.pb[0]
    P.op("pe", lambda e: e.matmul(pt[:, 0:256], lhsT=k.ones[0:1, :], rhs=grow[0:1, :], start=True, stop=True), reads=[grow_r, k.ones_r], writes=[pr])
    P.op("act", lambda e: e.copy(out=gb[:].rearrange("p a b -> p (a b)"), in_=pt[:, 0:256]), reads=[pr], writes=[gb_r])
    NB = 8
    wq = [C.sb([128, 8, 512], BF16, "wqkv") for _ in range(2)]
    vst = [C.sb([128, NB, 512], BF16, "vst") for _ in range(2)]
    qst = [C.sb([128, 4, NB * 128], BF16, "qst") for _ in range(2)]
    sq = [C.sb([128, 512], F32, "sq") for _ in range(4)]
    qn = [C.sb([128, 512], F32, "qn") for _ in range(4)]
    qb = [C.sb([128, 512], BF16, "qb") for _ in range(4)]
    rt = [C.sb([128, 4, 4, 16], F32, "rt") for _ in range(4)]
    s4 = [C.sb([128, 4], F32, "s4") for _ in range(4)]
    def make_block(typ, hh, w, w_r, st, st_r, si, bi):
        ps, ps_r = k.pb[rr(k, "pqkv", 6)]
        is_halo = (bi % nbr == 0)
        stages = []
        stages.append(lambda: P.op("pe", [lambda e, j=j: e.matmul(ps[:], lhsT=hT[:, j, bi * 128:(bi + 1) * 128], rhs=w[:, j, :],
                                                                 start=(j == 0), stop=(j == 7)) for j in range(8)],
                                   reads=[hT_r, w_r], writes=[ps_r]))
        if typ == 2:
            if is_halo:
                stages.append(lambda: P.op("act", lambda e: e.activation(out=st[:, si, :], in_=ps[:], func=AF.Copy, scale=k.hv[:, 0:1]),
                                           reads=[ps_r, k.hv_r], writes=[st_r]))
            else:
                stages.append(lambda: P.op("act", lambda e: e.copy(out=st[:, si, :], in_=ps[:]), reads=[ps_r], writes=[st_r]))
            return stages
        sqt, sq_r = sq[rr(k, "sq", 4)]
        s4t, s4_r = s4[rr(k, "s4", 4)]
        qnt, qn_r = qn[rr(k, "qn", 4)]
        qbt, qb_r = qb[rr(k, "qb", 4)]
        rtt, rt_r = rt[rr(k, "rt", 4)]
        tp, tpr = k.tp[rr(k, "tp", 2)]
        q3 = qnt[:].rearrange("p (h e) -> p h e", h=4)
        b3 = qbt[:].rearrange("p (h e) -> p h e", h=4)
        cb_ = cosT[:, bi, :].unsqueeze(1).broadcast_to([128, 4, 16])
        sb_ = sinT[:, bi, :].unsqueeze(1).broadcast_to([128, 4, 16])
        x1, x2 = q3[:, :, 0:16], q3[:, :, 16:32]
        stages.append(lambda: P.op("act", lambda e: e.activation(out=sqt[:], in_=ps[:], func=AF.Square), reads=[ps_r], writes=[sq_r]))

        def s2():
            P.op("dve", lambda e: e.tensor_reduce(out=s4t[:], in_=sqt[:].rearrange("p (h e) -> p h e", h=4), axis=AX.X, op=ALU.add),
                 reads=[sq_r], writes=[s4_r])
            P.op("dve", lambda e: e.tensor_scalar(out=s4t[:], in0=s4t[:], scalar1=1.0 / 128, scalar2=EPS, op0=ALU.mult, op1=ALU.add),
                 reads=[s4_r], writes=[s4_r])
        stages.append(s2)
        stages.append(lambda: P.op("act", lambda e: e.activation(out=s4t[:], in_=s4t[:], func=AF.Sqrt), reads=[s4_r], writes=[s4_r]))

        def s4_():
            P.op("dve", lambda e: e.reciprocal(out=s4t[:], in_=s4t[:]), reads=[s4_r], writes=[s4_r])
            P.op("dve", lambda e: e.tensor_tensor(out=q3, in0=ps[:].rearrange("p (h e) -> p h e", h=4),
                                                  in1=s4t[:].unsqueeze(2).broadcast_to([128, 4, 128]), op=ALU.mult),
                 reads=[ps_r, s4_r], writes=[qn_r])
        stages.append(s4_)

        def s5():
            P.op("pool", lambda e: e.tensor_tensor(out=q3, in0=q3, in1=gb[:, typ, :].unsqueeze(1).broadcast_to([128, 4, 128]), op=ALU.mult),
                 reads=[qn_r, gb_r], writes=[qn_r])
            P.op("pool", [lambda e: e.tensor_tensor(out=rtt[:, 0], in0=x1, in1=cb_, op=ALU.mult),
                          lambda e: e.tensor_tensor(out=rtt[:, 1], in0=x2, in1=sb_, op=ALU.mult),
                          lambda e: e.tensor_tensor(out=rtt[:, 2], in0=x2, in1=cb_, op=ALU.mult),
                          lambda e: e.tensor_tensor(out=rtt[:, 3], in0=x1, in1=sb_, op=ALU.mult)],
                 reads=[qn_r, cos_r, sin_r], writes=[rt_r])
        stages.append(s5)

        def s6():
            P.op("dve", [lambda e: e.tensor_tensor(out=b3[:, :, 0:16], in0=rtt[:, 0], in1=rtt[:, 1], op=ALU.subtract),
                         lambda e: e.tensor_tensor(out=b3[:, :, 16:32], in0=rtt[:, 2], in1=rtt[:, 3], op=ALU.add)],
                 reads=[rt_r], writes=[qb_r])
            P.op("act", lambda e: e.copy(out=b3[:, :, 32:128], in_=q3[:, :, 32:128]), reads=[qn_r], writes=[qb_r])
        stages.append(s6)
        stages.append(lambda: P.op("pe", [lambda e, h_=h_: e.transpose(out=tp[:, h_, :], in_=qbt[:, h_ * 128:(h_ + 1) * 128], identity=k.identb[:])
                                          for h_ in range(4)], reads=[qb_r, k.identb_r], writes=[tpr]))
        stages.append(lambda: P.op("act", lambda e: e.copy(out=st[:, :, si * 128:(si + 1) * 128], in_=tp[:, 0:4, :]), reads=[tpr], writes=[st_r]))
        return stages

    def flush(typ, hh, st, st_r, chunk):
        runs = []
        for si, bi in enumerate(chunk):
            if runs and runs[-1][1] + runs[-1][2] == bi:
                runs[-1][2] += 1
            else:
                runs.append([si, bi, 1])
        for si0, bi0, n in runs:
            for hd in range(4):
                if typ == 2:
                    dst = scr["v"][g][hh * 4 + hd][bi0 * 128:(bi0 + n) * 128, :].rearrange("(b p) e -> p b e", p=128)
                    P.dma("sp", dst, st[:, si0:si0 + n, hd * 128:(hd + 1) * 128], st_r, reads=[st_r])
                else:
                    dst = scr["q" if typ == 0 else "k"][g][hh * 4 + hd][:, bi0 * 128:(bi0 + n) * 128]
                    P.dma("sp", dst, st[:, hd, si0 * 128:(si0 + n) * 128], st_r, reads=[st_r])

    items = []
    cbs = []
    for typ in range(3):
        for hh in range(2):
            w, w_r = wq[rr(k, "wqkv", 2)]
            c0 = g * 3072 + typ * 1024 + hh * 512
            blocks = [bi for bi in range(nblk) if (typ != 0 or bi % nbr != 0)]
            first = True
            for c_ in range(0, len(blocks), NB):
                chunk = blocks[c_:c_ + NB]
                if typ == 2:
                    st, st_r = vst[rr(k, "vst", 2)]
                else:
                    st, st_r = qst[rr(k, "qst", 2)]
                for si, bi in enumerate(chunk):
                    pre = None
                    if first:
                        pre = len(cbs)
                        cbs.append((w, w_r, c0))
                        first = False
                    fl = (typ, hh, st, st_r, chunk) if si == len(chunk) - 1 else None
                    items.append((pre, (typ, hh, w, w_r, st, st_r, si, bi), fl))
    live = []
    nxt = 0
    DEPTH = 3
    while nxt < len(items) or live:
        if nxt < len(items):
            pre, args, fl = items[nxt]
            nxt += 1
            if pre is not None:
                for n_ in ([0, 1] if pre == 0 else [pre + 1]):
                    if n_ < len(cbs):
                        w_, w_r_, c0_ = cbs[n_]
                        P.dma("pool", w_[:], io["attn_w_qkv"][:, c0_:c0_ + 512].rearrange("(j p) n -> p j n", p=128), w_r_, writes=[w_r_])
            live.append([make_block(*args), 0, fl])
        for ent in list(live):
            ent[0][ent[1]]()
            ent[1] += 1
            if ent[1] == len(ent[0]):
                if ent[2] is not None:
                    flush(*ent[2])
                live.remove(ent)
    P.barrier()
    C.pop()


def attn_phase_b(k, io, scr):
    P, C = k.P, k.C
    C.push()
    mask_f, mask_fr = C.sb([128, 256], F32, "maskf")
    P.dma("sp", mask_f[:], io["mask2"], mask_fr, writes=[mask_fr])
    mask, mask_r = C.sb([128, 256], BF16, "mask")
    P.op("dve", lambda e: e.tensor_copy(out=mask[:], in_=mask_f[:]), reads=[mask_fr], writes=[mask_r])
    gq, gq_r = C.sb([1, 768], F32, "gqk")
    P.dma("sp", gq[:, 0:384], io["q_norm_g"].rearrange("(o g) e -> o (g e)", o=1), gq_r, writes=[gq_r])
    P.dma("sp", gq[:, 384:768], io["k_norm_g"].rearrange("(o g) e -> o (g e)", o=1), gq_r, writes=[gq_r])
    mx, mx_r = C.sb([1, 4], F32, "mx")
    P.op("act", lambda e: e.activation(out=gq[:], in_=gq[:], func=AF.Abs), reads=[gq_r], writes=[gq_r])
    P.op("dve", lambda e: e.tensor_reduce(out=mx[:, 0:2], in_=gq[:].rearrange("o (a b) -> o a b", a=2), axis=AX.X, op=ALU.max),
         reads=[gq_r], writes=[mx_r])
    P.op("dve", lambda e: e.scalar_tensor_tensor(out=mx[:, 2:3], in0=mx[:, 0:1], scalar=-float(np.sqrt(128.0)), in1=mx[:, 1:2],
                                                 op0=ALU.mult, op1=ALU.mult), reads=[mx_r], writes=[mx_r])
    negc, negc_r = C.sb([128, 1], F32, "negc")
    pt, pr = k.pb[0]
    P.op("pe", lambda e: e.matmul(pt[:, 0:1], lhsT=k.ones[0:1, :], rhs=mx[0:1, 2:3], start=True, stop=True), reads=[mx_r, k.ones_r], writes=[pr])
    P.op("act", lambda e: e.copy(out=negc[:], in_=pt[:, 0:1]), reads=[pr], writes=[negc_r])
    TMAX = grp(2)[2]
    kT = [C.sb([128, TMAX], BF16, "kT") for _ in range(2)]
    qT = [C.sb([128, TMAX], BF16, "qT") for _ in range(2)]
    vv = [C.sb([128, TMAX // 128, 128], BF16, "vv") for _ in range(2)]
    acc, acc_r = C.sb([128, 2, TOWN], F32, "acc")
    Et = [C.sb([128, 256], BF16, "Et") for _ in range(4)]
    Pt = [C.sb([128, 256], BF16, "Pt") for _ in range(4)]
    oT, oT_r = C.sb([128, TOWN], BF16, "oT")
    scale = float(1.0 / np.sqrt(128.0))
    for hd in range(8):
        for g in range(3):
            d, halo, T, nblk, nbr = grp(g)
            i = rr(k, "kqv", 2)
            kt, kt_r = kT[i]
            qt, qt_r = qT[i]
            vt, vt_r = vv[i]
            P.dma("sp", kt[:, 0:T], scr["k"][g][hd], kt_r, writes=[kt_r])
            P.dma("sp", qt[:, 0:T], scr["q"][g][hd], qt_r, writes=[qt_r])
            P.dma("sp", vt[:, 0:nblk, :], scr["v"][g][hd].rearrange("(b p) e -> p b e", p=128), vt_r, writes=[vt_r])
            def make_qblock(r, jb, g=g, d=d, nbr=nbr, kt=kt, kt_r=kt_r, qt=qt, qt_r=qt_r, vt=vt, vt_r=vt_r):
                bq = r * nbr + jb
                sp_, sp_r = k.pb[rr(k, "ps", 3)]
                ei = rr(k, "Et", 4)
                et, et_r = Et[ei]
                pt_, pt_r = Pt[ei]
                up, up_r = k.pb[3 + rr(k, "pu", 3)]
                t0 = d * 128 * (jb - 1) + r
                dst = acc[:, :, t0: t0 + d * 127 + 1: d]
                src = up[:, 0:256].rearrange("p (a b) -> p a b", a=2)
                st = []
                st.append(lambda: P.op("pe", [lambda e: e.matmul(sp_[:, 0:128], lhsT=kt[:, (bq - 1) * 128:bq * 128], rhs=qt[:, bq * 128:(bq + 1) * 128], start=True, stop=True),
                                              lambda e: e.matmul(sp_[:, 128:256], lhsT=kt[:, bq * 128:(bq + 1) * 128], rhs=qt[:, bq * 128:(bq + 1) * 128], start=True, stop=True)],
                                       reads=[kt_r, qt_r], writes=[sp_r]))
                st.append(lambda: P.op("act", lambda e: e.activation(out=et[:], in_=sp_[:, 0:256], func=AF.Exp, bias=negc[:, 0:1], scale=scale),
                                       reads=[sp_r, negc_r], writes=[et_r]))

                def s2():
                    P.op("pool", lambda e: e.tensor_tensor(out=pt_[:], in0=et[:], in1=mask[:], op=ALU.mult), reads=[et_r, mask_r], writes=[pt_r])
                    if jb == 1:
                        P.op("dve", lambda e: e.tensor_scalar(out=pt_[:, 0:128], in0=pt_[:, 0:128], scalar1=k.hv[:, 0:1], scalar2=None, op0=ALU.mult),
                             reads=[pt_r, k.hv_r], writes=[pt_r])
                st.append(s2)
                st.append(lambda: P.op("pe", [lambda e: e.matmul(up[:, 0:128], lhsT=vt[:, bq - 1, :], rhs=pt_[:, 0:128], start=True, stop=False),
                                              lambda e: e.matmul(up[:, 0:128], lhsT=vt[:, bq, :], rhs=pt_[:, 128:256], start=False, stop=True),
                                              lambda e: e.matmul(up[:, 128:256], lhsT=k.onesb[:], rhs=pt_[:, 0:128], start=True, stop=False),
                                              lambda e: e.matmul(up[:, 128:256], lhsT=k.onesb[:], rhs=pt_[:, 128:256], start=False, stop=True)],
                                       reads=[vt_r, pt_r, k.onesb_r], writes=[up_r]))
                if g == 0:
                    st.append(lambda: P.op("dve", lambda e: e.tensor_copy(out=dst, in_=src), reads=[up_r], writes=[acc_r]))
                else:
                    st.append(lambda: P.op("dve", lambda e: e.tensor_tensor(out=dst, in0=src, in1=dst, op=ALU.add), reads=[up_r, acc_r], writes=[acc_r]))
                return st

            todo = [(r, jb) for r in range(d) for jb in range(1, nbr)]
            live = []
            nxt = 0
            while nxt < len(todo) or live:
                if nxt < len(todo):
                    live.append([make_qblock(*todo[nxt]), 0])
                    nxt += 1
                for ent in list(live):
                    ent[0][ent[1]]()
                    ent[1] += 1
                    if ent[1] == len(ent[0]):
                        live.remove(ent)
        P.op("dve", lambda e: e.reciprocal(out=acc[:, 1, :], in_=acc[:, 1, :]), reads=[acc_r], writes=[acc_r])
        P.op("pool", lambda e: e.tensor_tensor(out=oT[:], in0=acc[:, 0, :], in1=acc[:, 1, :], op=ALU.mult), reads=[acc_r], writes=[oT_r])
        P.dma("sp", scr["o"][hd], oT[:], oT_r, reads=[oT_r])
    P.barrier()
    C.pop()


def attn_phase_c(k, io, bc, scr, x1_dst):
    P, C = k.P, k.C
    g1b = bc[2]
    C.push()
    wo, wo_r = C.sb([128, 8, 1024], BF16, "wo")
    P.dma("pool", wo[:], io["attn_w_o"].rearrange("(h p) n -> p h n", p=128), wo_r, writes=[wo_r])
    ot = [C.sb([128, 8, 512], BF16, "ot") for _ in range(2)]
    xs = [C.sb([128, 4, 1024], F32, "xc") for _ in range(2)]
    tmp = [C.sb([128, 512], F32, "tmpc") for _ in range(2)]
    for t in range(TOWN // 512):
        t0 = t * 512
        o_, o_r = ot[t % 2]
        x_, x_r = xs[t % 2]
        for hd in range(8):
            P.dma("sp", o_[:, hd, :], scr["o"][hd][:, t0:t0 + 512], o_r, writes=[o_r])
        P.dma("sp", x_[:], k.x_in[t0:t0 + 512, :].rearrange("(s p) d -> p s d", p=128), x_r, writes=[x_r])
        for s in range(4):
            for half in range(2):
                pp, pp_r = k.pb[rr(k, "pc", 4)]
                P.op("pe", [lambda e, hd=hd: e.matmul(pp[:], lhsT=o_[:, hd, s * 128:(s + 1) * 128], rhs=wo[:, hd, half * 512:(half + 1) * 512],
                                                      start=(hd == 0), stop=(hd == 7)) for hd in range(8)], reads=[o_r, wo_r], writes=[pp_r])
                tq, tq_r = tmp[rr(k, "tmpc", 2)]
                P.op("dve", lambda e: e.tensor_tensor(out=tq[:], in0=pp[:], in1=g1b[0][:, half * 512:(half + 1) * 512], op=ALU.mult),
                     reads=[pp_r, g1b[1]], writes=[tq_r])
                P.op("pool", lambda e: e.tensor_tensor(out=x_[:, s, half * 512:(half + 1) * 512], in0=tq[:], in1=x_[:, s, half * 512:(half + 1) * 512],
                                                       op=ALU.add), reads=[tq_r, x_r], writes=[x_r])
        P.dma("sp", x1_dst[t0:t0 + 512, :].rearrange("(s p) d -> p s d", p=128), x_[:], x_r, reads=[x_r])
    P.barrier()
    C.pop()


def moe_phase(k, io, bc, x1_src, x_out):
    P, C = k.P, k.C
    sh2, gain2, g2b = bc[3], bc[4], bc[5]
    C.push()
    xt, xt_r = C.sb([128, 8, 1024], F32, "xm")
    hf = [C.sb([128, 1024], F32, "hfm") for _ in range(2)]
    tmp = [C.sb([128, 1024], F32, "tmpm") for _ in range(2)]
    junk = C.sb([128, 1024], F32, "junkm")
    hT, hT_r = C.sb([128, 8, 1024], BF16, "hTm")
    h32 = [C.sb([128, 8, 128], F32, "h32") for _ in range(2)]
    ss, ss_r = C.sb([128, 8], F32, "ssm")
    rw, rw_r = C.sb([128, 8, 8], F32, "rw")
    P.dma("sp", rw[:], io["router_w"].rearrange("(j p) e -> p j e", p=128), rw_r, writes=[rw_r])
    rbrow, rbrow_r = C.sb([1, 8], F32, "rbrow")
    P.dma("sp", rbrow[:], io["router_b"], rbrow_r, writes=[rbrow_r])
    rbb, rbb_r = C.sb([128, 8], F32, "rbb")
    pt, pr = k.pb[4]
    P.op("pe", lambda e: e.matmul(pt[:, 0:8], lhsT=k.ones[0:1, :], rhs=rbrow[0:1, :], start=True, stop=True), reads=[rbrow_r, k.ones_r], writes=[pr])
    P.op("act", lambda e: e.copy(out=rbb[:], in_=pt[:, 0:8]), reads=[pr], writes=[rbb_r])
    comb, comb_r = C.sb([128, 8, 8], F32, "comb")
    rt_ = [C.sb([128, 40], F32, "rtm") for _ in range(2)]
    alloc_expert_bufs(k, 2)
    import os
    NT_ = int(os.environ.get("MOE_TILES", "4"))
    NE_ = int(os.environ.get("MOE_EXP", str(NE)))
    work = [(t, e_, pc) for t in range(NT_) for e_ in range(NE_) for pc in MOE_PIECES]
    slots = {}
    slots[0] = expert_load(k, io["moe_w_gu0"], io["moe_w_down0"], 0, 512, DFFE, g2b)
    wi = 0
    for t in range(NT_):
        t0 = t * 1024
        P.dma("sp", xt[:], x1_src[t0:t0 + 1024, :].rearrange("(s p) d -> p s d", p=128), xt_r, writes=[xt_r])
        for s in range(8):
            sumsq(k, xt[:, s, :], xt_r, ss, ss_r, s, junk)
        rstd_from_ss(k, ss, ss_r, 8, 1.0 / D)
        for s in range(8):
            h = hf[rr(k, "hfm", 2)]
            mod_norm(k, xt[:, s, :], xt_r, ss[:, s:s + 1], ss_r, gain2, sh2, tmp[rr(k, "tmpm", 2)], h[0][:], h[1])
            p0, p0r = k.pb[0]
            p1, p1r = k.pb[1]
            fns = []
            for j in range(8):
                pp = p0 if j < 4 else p1
                fns.append(lambda e, j=j, pp=pp: e.matmul(pp[:, (j % 4) * 128:(j % 4) * 128 + 128], lhsT=h[0][:, j * 128:(j + 1) * 128], rhs=k.ident[:],
                                                          start=True, stop=True))
            P.op("pe", fns, reads=[h[1], k.ident_r], writes=[p0r, p1r])
            h3, h3r = h32[rr(k, "h32", 2)]
            for half, (pp, ppr) in enumerate(((p0, p0r), (p1, p1r))):
                src = pp[:].rearrange("p (j t) -> p j t", j=4)
                P.op("act", lambda e: e.copy(out=hT[:, half * 4:(half + 1) * 4, s * 128:(s + 1) * 128], in_=src), reads=[ppr], writes=[hT_r])
                P.op("dve", lambda e: e.tensor_copy(out=h3[:, half * 4:(half + 1) * 4, :], in_=src), reads=[ppr], writes=[h3r])
            pl, plr = k.pb[4 + rr(k, "pl", 2)]
            P.op("pe", [lambda e, j=j: e.matmul(pl[:, 0:8], lhsT=h3[:, j, :], rhs=rw[:, j, :], start=(j == 0), stop=(j == 7)) for j in range(8)],
                 reads=[h3r, rw_r], writes=[plr])
            r_, r_r = rt_[rr(k, "rtm", 2)]
            lg, eq1, l2, eq2, mm = r_[:, 0:8], r_[:, 8:16], r_[:, 16:24], r_[:, 24:32], r_[:, 32:40]
            P.op("dve", [lambda e: e.tensor_tensor(out=lg, in0=pl[:, 0:8], in1=rbb[:], op=ALU.add),
                         lambda e: e.tensor_reduce(out=mm[:, 0:1], in_=lg, axis=AX.X, op=ALU.max)], reads=[plr, rbb_r], writes=[r_r])
            P.op("dve", [lambda e: e.tensor_scalar(out=eq1, in0=lg, scalar1=mm[:, 0:1], scalar2=None, op0=ALU.is_equal),
                         ], reads=[r_r], writes=[r_r])
            P.op("dve", lambda e: e.scalar_tensor_tensor(out=l2, in0=eq1, scalar=-1.0e30, in1=lg, op0=ALU.mult, op1=ALU.add), reads=[r_r], writes=[r_r])
            P.op("dve", lambda e: e.tensor_reduce(out=mm[:, 1:2], in_=l2, axis=AX.X, op=ALU.max), reads=[r_r], writes=[r_r])
            P.op("dve", lambda e: e.tensor_scalar(out=eq2, in0=l2, scalar1=mm[:, 1:2], scalar2=None, op0=ALU.is_equal), reads=[r_r], writes=[r_r])
            P.op("dve", lambda e: e.tensor_tensor(out=mm[:, 2:3], in0=mm[:, 1:2], in1=mm[:, 0:1], op=ALU.subtract), reads=[r_r], writes=[r_r])
            P.op("act", lambda e: e.activation(out=mm[:, 3:4], in_=mm[:, 2:3], func=AF.Sigmoid), reads=[r_r], writes=[r_r])
            P.op("act", lambda e: e.activation(out=mm[:, 4:5], in_=mm[:, 2:3], func=AF.Sigmoid, scale=-1.0), reads=[r_r], writes=[r_r])
            P.op("dve", lambda e: e.tensor_scalar(out=eq1, in0=eq1, scalar1=mm[:, 4:5], scalar2=None, op0=ALU.mult), reads=[r_r], writes=[r_r])
            P.op("dve", lambda e: e.scalar_tensor_tensor(out=comb[:, s, :], in0=eq2, scalar=mm[:, 3:4], in1=eq1, op0=ALU.mult, op1=ALU.add),
                 reads=[r_r], writes=[comb_r])
        for e_ in range(NE_):
            for (f0, fp) in MOE_PIECES:
                if wi + 1 < len(work):
                    _, ne, (nf0, nfp) = work[wi + 1]
                    slots[wi + 1] = expert_load(k, io["moe_w_gu%d" % ne], io["moe_w_down%d" % ne], nf0, nfp, DFFE, g2b)
                expert_compute(k, slots[wi], fp, hT, hT_r, xt, xt_r, (comb, comb_r, e_))
                wi += 1
        P.dma("sp", x_out[t0:t0 + 1024, :].rearrange("(s p) d -> p s d", p=128), xt[:], xt_r, reads=[xt_r])
    P.barrier()
    C.pop()


def alloc_scratch(nc):
    scr = {"q": [], "k": [], "v": [], "o": []}
    for g in range(3):
        T = grp(g)[2]
        scr["q"].append([nc.dram_tensor("qT_%d_%d" % (g, h), [128, T], BF16, kind="Internal").ap() for h in range(8)])
        scr["k"].append([nc.dram_tensor("kT_%d_%d" % (g, h), [128, T], BF16, kind="Internal").ap() for h in range(8)])
        scr["v"].append([nc.dram_tensor("v_%d_%d" % (g, h), [T, 128], BF16, kind="Internal").ap() for h in range(8)])
    scr["o"] = [nc.dram_tensor("oT_%d" % h, [128, TOWN], BF16, kind="Internal").ap() for h in range(8)]
    scr["x1"] = nc.dram_tensor("x1_s", [TOWN, D], F32, kind="Internal").ap()
    return scr


def odd_layer(k, io, x_in, x_out, scr, phases="abcd"):
    P, C = k.P, k.C
    C.push()
    k.x_in = x_in
    bc = ada_phase(k, io, False)
    if "a" in phases:
        for g in range(3):
            attn_phase_a(k, io, g, bc, scr)
    if "b" in phases:
        attn_phase_b(k, io, scr)
    if "c" in phases:
        attn_phase_c(k, io, bc, scr, scr["x1"] if "d" in phases else x_out)
    if "d" in phases:
        moe_phase(k, io, bc, scr["x1"] if "c" in phases else x_in, x_out)
    P.barrier()
    C.pop()


_ODD_IO = [("attn_w_qkv", [D, 9216]), ("attn_w_o", [D, D]), ("q_norm_g", [3, 128]), ("k_norm_g", [3, 128]),
           ("router_w", [D, NE]), ("router_b", [1, NE]), ("mask2", [128, 256]), ("invf", [128, 16])]
_ODD_IO += [("moe_w_gu%d" % e_, [D, 2 * DFFE]) for e_ in range(NE)] + [("moe_w_down%d" % e_, [DFFE, D]) for e_ in range(NE)]


def build_odd(phases="abcd"):
    nc = bass.Bass("TRN2", target_bir_lowering=False)
    io = {n: _dram_in(nc, n, s) for n, s in _COMMON_IO + _ODD_IO}
    for g in range(3):
        io["pos%d" % g] = _dram_in(nc, "pos%d" % g, [128, grp(g)[3]], I32)
    x_out = nc.dram_tensor("x_out", [TOWN, D], F32, kind="ExternalOutput").ap()
    scr = alloc_scratch(nc)
    k = new_k(nc)
    k.C.push()
    setup_common(k, io)
    odd_layer(k, io, io["x_own"], x_out, scr, phases)
    k.P.barrier()
    k.C.pop()
    return nc


def pos_layout(positions, core, g):
    b, half = core // 2, core % 2
    d, halo, T, nblk, nbr = grp(g)
    s0 = half * TOWN
    i = np.arange(128)[:, None]
    bi = np.arange(nblk)[None, :]
    r, jb = bi // nbr, bi % nbr
    tok = s0 - halo + d * (128 * jb + i) + r
    out = np.where(tok >= 0, positions[b][np.clip(tok, 0, SEQ - 1)], 0)
    return np.ascontiguousarray(out.astype(np.int32))


def attn_consts():
    p = np.arange(128)[:, None]
    f = np.arange(128)[None, :]
    mask2 = np.concatenate([(p >= f), (p <= f)], axis=1).astype(np.float32)
    half = 16
    inv_freq = (500000.0 ** (-np.arange(half, dtype=np.float32) * 2.0 / 32)).astype(np.float32)
    return {"mask2": mask2, "invf": np.ascontiguousarray(np.broadcast_to(inv_freq[None, :], (128, 16)))}


def run_odd(nc, x_full, inp, i):
    j = i // 2
    ac = attn_consts()
    maps = []
    for core in range(NCORES):
        m = core_common_inputs(x_full, inp["c"], core)
        m.update(ac)
        m.update({
            "ada_w": inp["ada_w"][i], "ada_b": inp["ada_b"][i][None, :], "norm1_g": inp["norm1_g"][i][None, :],
            "norm2_g": inp["norm2_g"][i][None, :],
            "attn_w_qkv": inp["attn_w_qkv"][j], "attn_w_o": inp["attn_w_o"][j], "q_norm_g": inp["q_norm_g"][j],
            "k_norm_g": inp["k_norm_g"][j], "router_w": inp["router_w"][j], "router_b": inp["router_b"][j][None, :],
        })
        for e_ in range(NE):
            m["moe_w_gu%d" % e_] = inp["moe_w_gu"][j][e_]
            m["moe_w_down%d" % e_] = inp["moe_w_down"][j][e_]
        for g in range(3):
            m["pos%d" % g] = pos_layout(inp["positions"], core, g)
        maps.append(m)
    res = run_bass_kernel_spmd(nc, maps, core_ids=list(range(NCORES)))
    out = np.empty_like(x_full)
    for core in range(NCORES):
        b, half = core // 2, core % 2
        out[b, half * TOWN:(half + 1) * TOWN] = res.results[core]["x_out"]
    return out


_PROGS = {}


def kernel(**inp):
    inp = {k_: np.asarray(v) for k_, v in inp.items()}
    x = np.ascontiguousarray(inp["x"], dtype=np.float32)
    if "even" not in _PROGS:
        _PROGS["even"] = build_even()
        _PROGS["odd"] = build_odd()
    for i in range(4):
        if i % 2 == 0:
            x = run_even(_PROGS["even"], x, inp, i)
        else:
            x = run_odd(_PROGS["odd"], x, inp, i)
    return x


_MOEX_IO = [("x1", [TOWN, D]), ("xacc", [TOWN, D]), ("mod_in", [1, 6 * D]), ("esel", [128, NE]), ("ident", [128, 128]),
            ("hv", [128, 1]), ("router_w", [D, NE]), ("router_b", [1, NE]), ("w_gu", [D, 2 * DFFE]), ("w_down", [DFFE, D])]


def moe_expert_phase(k, io, x_out):
    P, C = k.P, k.C
    import os
    MODE = os.environ.get("MOEX_MODE", "full")
    C.push()
    row, row_r = C.sb([1, 3 * D], F32, "mrow")
    P.dma("sp", row[:], io["mod_in"][:, 3 * D:6 * D], row_r, writes=[row_r])
    bc = [C.sb([128, 1024], F32, "bcx") for _ in range(3)]
    for idx in range(3):
        for half in range(2):
            pt, pr = k.pb[(idx * 2 + half) % 2]
            P.op("pe", lambda e: e.matmul(pt[:], lhsT=k.ones[0:1, :], rhs=row[0:1, idx * 1024 + half * 512: idx * 1024 + half * 512 + 512],
                                          start=True, stop=True), reads=[row_r, k.ones_r], writes=[pr])
            P.op("act", lambda e: e.copy(out=bc[idx][0][:, half * 512:(half + 1) * 512], in_=pt[:]), reads=[pr], writes=[bc[idx][1]])
    sh2, gain2, g2b = bc
    xt, xt_r = C.sb([128, 8, 1024], F32, "xm")
    x1t, x1t_r = C.sb([128, 8, 1024], F32, "x1m")
    hf = [C.sb([128, 1024], F32, "hfm") for _ in range(2)]
    tmp = [C.sb([128, 1024], F32, "tmpm") for _ in range(2)]
    junk = C.sb([128, 1024], F32, "junkm")
    hT, hT_r = C.sb([128, 8, 1024], BF16, "hTm")
    hbh = [C.sb([128, 1024], BF16, "hbh") for _ in range(2)]
    hbl = [C.sb([128, 1024], BF16, "hbl") for _ in range(2)]
    hTlo = [C.sb([128, 8, 128], BF16, "hTlo") for _ in range(2)]
    ss, ss_r = C.sb([128, 8], F32, "ssm")
    rw, rw_r = C.sb([128, 8, 8], F32, "rw")
    P.dma("sp", rw[:], io["router_w"].rearrange("(j p) e -> p j e", p=128), rw_r, writes=[rw_r])
    rwh, rwh_r = C.sb([128, 8, 8], BF16, "rwh")
    rwl, rwl_r = C.sb([128, 8, 8], BF16, "rwl")
    P.op("dve", lambda e: e.tensor_copy(out=rwh[:], in_=rw[:]), reads=[rw_r], writes=[rwh_r])
    P.op("dve", lambda e: e.tensor_tensor(out=rwl[:], in0=rw[:], in1=rwh[:], op=ALU.subtract), reads=[rw_r, rwh_r], writes=[rwl_r])
    esel, esel_r = C.sb([128, 8], F32, "esel")
    P.dma("sp", esel[:], io["esel"], esel_r, writes=[esel_r])
    rbrow, rbrow_r = C.sb([1, 8], F32, "rbrow")
    P.dma("sp", rbrow[:], io["router_b"], rbrow_r, writes=[rbrow_r])
    rbb, rbb_r = C.sb([128, 8], F32, "rbb")
    pt, pr = k.pb[4]
    P.op("pe", lambda e: e.matmul(pt[:, 0:8], lhsT=k.ones[0:1, :], rhs=rbrow[0:1, :], start=True, stop=True), reads=[rbrow_r, k.ones_r], writes=[pr])
    P.op("act", lambda e: e.copy(out=rbb[:], in_=pt[:, 0:8]), reads=[pr], writes=[rbb_r])
    comb, comb_r = C.sb([128, 8, 8], F32, "comb")
    rt_ = [C.sb([128, 48], F32, "rtm") for _ in range(2)]
    alloc_expert_bufs(k, 2)
    work = [(t, pc) for t in range(4) for pc in MOE_PIECES]
    slots = {}
    slots[0] = expert_load(k, io["w_gu"], io["w_down"], 0, 512, DFFE, g2b)
    wi = 0
    for t in range(4):
        t0 = t * 1024
        if t == 0:
            P.dma("sp", x1t[:], io["x1"][t0:t0 + 1024, :].rearrange("(s p) d -> p s d", p=128), x1t_r, writes=[x1t_r])
        P.dma("sp", xt[:], io["xacc"][t0:t0 + 1024, :].rearrange("(s p) d -> p s d", p=128), xt_r, writes=[xt_r])
        for s in range(8):
            if MODE == "pro":
                break
            sumsq(k, x1t[:, s, :], x1t_r, ss, ss_r, s, junk)
        if MODE != "pro":
            rstd_from_ss(k, ss, ss_r, 8, 1.0 / D)
        for s in range(8):
            if MODE == "pro":
                break
            h = hf[rr(k, "hfm", 2)]
            mod_norm(k, x1t[:, s, :], x1t_r, ss[:, s:s + 1], ss_r, gain2, sh2, tmp[rr(k, "tmpm", 2)], h[0][:], h[1])
            if MODE == "norm":
                continue
            hh, hh_r = hbh[rr(k, "hbh", 2)]
            hl, hl_r = hbl[rr(k, "hbl", 2)]
            P.op("act", lambda e: e.copy(out=hh[:], in_=h[0][:]), reads=[h[1]], writes=[hh_r])
            P.op("dve", lambda e: e.tensor_tensor(out=hl[:], in0=h[0][:], in1=hh[:], op=ALU.subtract), reads=[h[1], hh_r], writes=[hl_r])
            transpose_bf16(k, hh, hh_r, hT, hT_r, s * 128)
            tp, tpr = k.tp[rr(k, "tp", 2)]
            P.op("pe", [lambda e, j=j: e.transpose(out=tp[:, j, :], in_=hl[:, j * 128:(j + 1) * 128], identity=k.identb[:]) for j in range(8)],
                 reads=[hl_r, k.identb_r], writes=[tpr])
            hTl, hTl_r = hTlo[rr(k, "hTlo", 2)]
            P.op("dve", lambda e: e.tensor_copy(out=hTl[:], in_=tp[:]), reads=[tpr], writes=[hTl_r])
            if MODE in ("norouter", "noexp2"):
                P.op("pool", lambda e: e.memset(comb[:, s, :], 0.25), writes=[comb_r])
                continue
            pl, plr = k.pb[4 + rr(k, "pl", 2)]
            fns = [lambda e, j=j: e.matmul(pl[:, 0:8], lhsT=hT[:, j, s * 128:(s + 1) * 128], rhs=rwh[:, j, :], start=(j == 0), stop=False) for j in range(8)]
            fns += [lambda e, j=j: e.matmul(pl[:, 0:8], lhsT=hT[:, j, s * 128:(s + 1) * 128], rhs=rwl[:, j, :], start=False, stop=False) for j in range(8)]
            fns += [lambda e, j=j: e.matmul(pl[:, 0:8], lhsT=hTl[:, j, :], rhs=rwh[:, j, :], start=False, stop=(j == 7)) for j in range(8)]
            P.op("pe", fns, reads=[hT_r, hTl_r, rwh_r, rwl_r], writes=[plr])
            r_, r_r = rt_[rr(k, "rtm", 2)]
            lg, eq1, l2, eq2, mm, cs = r_[:, 0:8], r_[:, 8:16], r_[:, 16:24], r_[:, 24:32], r_[:, 32:40], r_[:, 40:48]
            P.op("dve", lambda e: e.tensor_tensor(out=lg, in0=pl[:, 0:8], in1=rbb[:], op=ALU.add), reads=[plr, rbb_r], writes=[r_r])
            P.op("dve", lambda e: e.tensor_reduce(out=mm[:, 0:1], in_=lg, axis=AX.X, op=ALU.max), reads=[r_r], writes=[r_r])
            P.op("dve", lambda e: e.tensor_scalar(out=eq1, in0=lg, scalar1=mm[:, 0:1], scalar2=None, op0=ALU.is_equal), reads=[r_r], writes=[r_r])
            P.op("dve", lambda e: e.scalar_tensor_tensor(out=l2, in0=eq1, scalar=-1.0e30, in1=lg, op0=ALU.mult, op1=ALU.add), reads=[r_r], writes=[r_r])
            P.op("dve", lambda e: e.tensor_reduce(out=mm[:, 1:2], in_=l2, axis=AX.X, op=ALU.max), reads=[r_r], writes=[r_r])
            P.op("dve", lambda e: e.tensor_scalar(out=eq2, in0=l2, scalar1=mm[:, 1:2], scalar2=None, op0=ALU.is_equal), reads=[r_r], writes=[r_r])
            P.op("dve", lambda e: e.tensor_tensor(out=mm[:, 2:3], in0=mm[:, 1:2], in1=mm[:, 0:1], op=ALU.subtract), reads=[r_r], writes=[r_r])
            P.op("act", lambda e: e.activation(out=mm[:, 3:4], in_=mm[:, 2:3], func=AF.Sigmoid), reads=[r_r], writes=[r_r])
            P.op("act", lambda e: e.activation(out=mm[:, 4:5], in_=mm[:, 2:3], func=AF.Sigmoid, scale=-1.0), reads=[r_r], writes=[r_r])
            P.op("dve", lambda e: e.tensor_scalar(out=eq1, in0=eq1, scalar1=mm[:, 4:5], scalar2=None, op0=ALU.mult), reads=[r_r], writes=[r_r])
            P.op("dve", lambda e: e.scalar_tensor_tensor(out=cs, in0=eq2, scalar=mm[:, 3:4], in1=eq1, op0=ALU.mult, op1=ALU.add), reads=[r_r], writes=[r_r])
            P.op("dve", lambda e: e.tensor_tensor(out=cs, in0=cs, in1=esel[:], op=ALU.mult), reads=[r_r, esel_r], writes=[r_r])
            P.op("dve", lambda e: e.tensor_reduce(out=comb[:, s, 0:1], in_=cs, axis=AX.X, op=ALU.add), reads=[r_r], writes=[comb_r])
        if t + 1 < 4:
            P.dma("sp", x1t[:], io["x1"][t0 + 1024:t0 + 2048, :].rearrange("(s p) d -> p s d", p=128), x1t_r, writes=[x1t_r])
        for (f0, fp) in MOE_PIECES:
            if MODE in ("noexp", "noexp2", "pro", "norm"):
                break
            if wi + 1 < len(work):
                nf0, nfp = work[wi + 1][1]
                slots[wi + 1] = expert_load(k, io["w_gu"], io["w_down"], nf0, nfp, DFFE, g2b)
            expert_compute(k, slots[wi], fp, hT, hT_r, xt, xt_r, (comb, comb_r, 0))
            wi += 1
        P.dma("sp", x_out[t0:t0 + 1024, :].rearrange("(s p) d -> p s d", p=128), xt[:], xt_r, reads=[xt_r])
    P.barrier()
    C.pop()


def build_moex():
    nc = bass.Bass("TRN2", target_bir_lowering=False)
    io = {n: _dram_in(nc, n, s) for n, s in _MOEX_IO}
    x_out = nc.dram_tensor("x_out", [TOWN, D], F32, kind="ExternalOutput").ap()
    k = new_k(nc)
    k.C.push()
    setup_common(k, io)
    moe_expert_phase(k, io, x_out)
    k.P.barrier()
    k.C.pop()
    return nc


def build_attn(phases="abc"):
    nc = bass.Bass("TRN2", target_bir_lowering=False)
    io = {n: _dram_in(nc, n, s) for n, s in _COMMON_IO + _ODD_IO[:6] + [("mask2", [128, 256]), ("invf", [128, 16])]}
    for g in range(3):
        io["pos%d" % g] = _dram_in(nc, "pos%d" % g, [128, grp(g)[3]], I32)
    x_out = nc.dram_tensor("x_out", [TOWN, D], F32, kind="ExternalOutput").ap()
    io["mod_out"] = nc.dram_tensor("mod_out", [1, 6 * D], F32, kind="ExternalOutput").ap()
    scr = alloc_scratch(nc)
    k = new_k(nc)
    k.C.push()
    setup_common(k, io)
    odd_layer(k, io, io["x_own"], x_out, scr, phases)
    k.P.barrier()
    k.C.pop()
    return nc


def _gather(res, name="x_out"):
    out = np.empty((4, SEQ, D), np.float32)
    for core in range(NCORES):
        b, half = core // 2, core % 2
        out[b, half * TOWN:(half + 1) * TOWN] = res.results[core][name]
    return out


def run_attn(nc, x_full, inp, i):
    j = i // 2
    ac = attn_consts()
    maps = []
    for core in range(NCORES):
        m = core_common_inputs(x_full, inp["c"], core)
        m.update(ac)
        m.update({
            "ada_w": inp["ada_w"][i], "ada_b": inp["ada_b"][i][None, :], "norm1_g": inp["norm1_g"][i][None, :],
            "norm2_g": inp["norm2_g"][i][None, :],
            "attn_w_qkv": inp["attn_w_qkv"][j], "attn_w_o": inp["attn_w_o"][j], "q_norm_g": inp["q_norm_g"][j],
            "k_norm_g": inp["k_norm_g"][j], "router_w": inp["router_w"][j], "router_b": inp["router_b"][j][None, :],
        })
        for g in range(3):
            m["pos%d" % g] = pos_layout(inp["positions"], core, g)
        maps.append(m)
    res = run_bass_kernel_spmd(nc, maps, core_ids=list(range(NCORES)))
    return _gather(res), [res.results[c]["mod_out"] for c in range(NCORES)]


def run_moex(nc, x1_full, xacc_full, mods, inp, i, e_):
    j = i // 2
    esel = np.zeros((128, NE), np.float32)
    esel[:, e_] = 1.0
    maps = []
    for core in range(NCORES):
        b, half = core // 2, core % 2
        maps.append({
            "x1": np.ascontiguousarray(x1_full[b, half * TOWN:(half + 1) * TOWN]),
            "xacc": np.ascontiguousarray(xacc_full[b, half * TOWN:(half + 1) * TOWN]),
            "mod_in": mods[core], "esel": esel, "ident": np.eye(128, dtype=np.float32),
            "hv": np.full((128, 1), float(half), np.float32),
            "router_w": inp["router_w"][j], "router_b": inp["router_b"][j][None, :],
            "w_gu": inp["moe_w_gu"][j][e_], "w_down": inp["moe_w_down"][j][e_],
        })
    res = run_bass_kernel_spmd(nc, maps, core_ids=list(range(NCORES)))
    return _gather(res)


def kernel(**inp):
    inp = {k_: np.asarray(v) for k_, v in inp.items()}
    x = np.ascontiguousarray(inp["x"], dtype=np.float32)
    if "even" not in _PROGS:
        _PROGS["even"] = build_even()
        _PROGS["attn"] = build_attn()
        _PROGS["moex"] = build_moex()
    for i in range(4):
        if i % 2 == 0:
            x = run_even(_PROGS["even"], x, inp, i)
        else:
            x1, mods = run_attn(_PROGS["attn"], x, inp, i)
            acc = x1
            for e_ in range(NE):
                acc = run_moex(_PROGS["moex"], x1, acc, mods, inp, i, e_)
            x = acc
    return x
```
